# Optimizing a Trainium2 kernel written in Bass

```python
import math
import jax, jax.numpy as jnp
from jax import lax
import numpy as np

D_MODEL = 1024
BATCH = 16
SEQ = 2048
DEPTH = 1

CHUNK = 64
N_META = 16
Q_BLOCK = 128
EPS = 1e-6

MLA_HEADS = 8
MLA_Q_RANK = 384
MLA_KV_RANK = 256
MLA_NOPE = 128
MLA_ROPE = 64
MLA_V = 128
ROPE_HALF = MLA_ROPE // 2
ROPE_THETA = 10000.0

GLA_HEADS = 4
GLA_DK = D_MODEL // 2
GLA_DV = D_MODEL
GLA_HK = GLA_DK // GLA_HEADS
GLA_HV = GLA_DV // GLA_HEADS
GLA_GATE_RANK = 16
GLA_TAU = 16.0
GLA_CHUNK = 16

PEER_HEADS = 8
PEER_NKEYS = 128
PEER_EXPERTS = PEER_NKEYS * PEER_NKEYS
PEER_QDIM = 256
PEER_HALF = PEER_QDIM // 2
PEER_TOPK = 16
PEER_BLOCK = 16

SPLITS = (MLA_Q_RANK, MLA_KV_RANK, MLA_ROPE, GLA_DK, GLA_DK, GLA_DV, GLA_GATE_RANK, GLA_DV, D_MODEL, D_MODEL)
D_IN = MLA_Q_RANK + MLA_KV_RANK + MLA_ROPE + 2 * GLA_DK + GLA_DV + GLA_GATE_RANK + GLA_DV + 2 * D_MODEL

kernel_name = "hybrid_mla_gla_peer_meta_chunk_causal"


def rmsnorm(x, g):
    x32 = x.astype(jnp.float32)
    y = x32 * lax.rsqrt(jnp.mean(x32 * x32, axis=-1, keepdims=True) + EPS)
    return (y * g.astype(jnp.float32)).astype(x.dtype)


def rope_tables(n):
    inv_freq = ROPE_THETA ** (-jnp.arange(ROPE_HALF, dtype=jnp.float32) / ROPE_HALF)
    ang = jnp.arange(n, dtype=jnp.float32)[:, None] * inv_freq[None, :]
    return jnp.cos(ang), jnp.sin(ang)


def rope(x, cos, sin):
    x32 = x.astype(jnp.float32)
    x1, x2 = x32[..., :ROPE_HALF], x32[..., ROPE_HALF:]
    return jnp.concatenate([x1 * cos - x2 * sin, x2 * cos + x1 * sin], axis=-1).astype(x.dtype)


def mla_branch(c_q, c_kv, k_pe, q_norm, w_uq, kv_norm, w_ukv, qn_nope, qn_pe, kn_nope, kn_pe, cos, sin, cid):
    B, L, _ = c_q.shape
    q = (rmsnorm(c_q, q_norm) @ w_uq).reshape(B, L, MLA_HEADS, MLA_NOPE + MLA_ROPE)
    kv = (rmsnorm(c_kv, kv_norm) @ w_ukv).reshape(B, L, MLA_HEADS, MLA_NOPE + MLA_V)
    q_nope = rmsnorm(q[..., :MLA_NOPE], qn_nope)
    q_pe = rope(rmsnorm(q[..., MLA_NOPE:], qn_pe), cos[:, None, :], sin[:, None, :])
    k_nope = rmsnorm(kv[..., :MLA_NOPE], kn_nope)
    v = kv[..., MLA_NOPE:]
    k_pe = rope(rmsnorm(k_pe, kn_pe), cos, sin)
    scale = (MLA_NOPE + MLA_ROPE) ** -0.5
    lp = cid.shape[0]
    nb = lp // Q_BLOCK
    pad = ((0, 0), (0, lp - L), (0, 0), (0, 0))
    qn_b = jnp.pad(q_nope, pad).reshape(B, nb, Q_BLOCK, MLA_HEADS, MLA_NOPE).transpose(1, 0, 2, 3, 4)
    qp_b = jnp.pad(q_pe, pad).reshape(B, nb, Q_BLOCK, MLA_HEADS, MLA_ROPE).transpose(1, 0, 2, 3, 4)
    cq_b = cid.reshape(nb, Q_BLOCK)
    cid_k = cid[:L]

    def attend(blk):
        qn, qp, cq = blk
        s = (jnp.einsum('bqhd,bkhd->bhqk', qn, k_nope) + jnp.einsum('bqhd,bkd->bhqk', qp, k_pe)).astype(jnp.float32) * scale
        mask = cid_k[None, :] <= cq[:, None]
        s = jnp.where(mask, s, -jnp.inf)
        w = jax.nn.softmax(s, axis=-1).astype(v.dtype)
        return jnp.einsum('bhqk,bkhd->bqhd', w, v)

    out = lax.map(attend, (qn_b, qp_b, cq_b))
    out = out.transpose(1, 0, 2, 3, 4).reshape(B, lp, MLA_HEADS * MLA_V)
    return out[:, :L]


def gla_branch(gq, gk, gv, ga, gg, w_a2, b_a, g_norm):
    B, L, _ = gq.shape
    nc = L // GLA_CHUNK
    q = gq.reshape(B, L, GLA_HEADS, GLA_HK) * (GLA_HK ** -0.5)
    k = gk.reshape(B, L, GLA_HEADS, GLA_HK)
    v = gv.reshape(B, L, GLA_HEADS, GLA_HV)
    log_a = jax.nn.log_sigmoid((ga @ w_a2 + b_a).astype(jnp.float32)) / GLA_TAU
    log_a = log_a.reshape(B, L, GLA_HEADS, GLA_HK)

    def to_chunks(t):
        return t.reshape(B, nc, GLA_CHUNK, GLA_HEADS, t.shape[-1]).transpose(1, 0, 3, 2, 4).astype(jnp.float32)

    tri = jnp.tril(jnp.ones((GLA_CHUNK, GLA_CHUNK), dtype=bool))

    def step(S, inp):
        qc, kc, vc, lac = inp
        bcum = jnp.cumsum(lac, axis=2)
        o_inter = jnp.einsum('bhcd,bhde->bhce', qc * jnp.exp(bcum), S)
        diff = bcum[:, :, :, None, :] - bcum[:, :, None, :, :]
        decay = jnp.exp(jnp.where(tri[None, None, :, :, None], diff, -jnp.inf))
        A = jnp.einsum('bhid,bhjd,bhijd->bhij', qc, kc, decay)
        o_intra = jnp.einsum('bhij,bhje->bhie', A, vc)
        b_last = bcum[:, :, -1]
        k_dec = kc * jnp.exp(b_last[:, :, None, :] - bcum)
        S = jnp.exp(b_last)[..., None] * S + jnp.einsum('bhcd,bhce->bhde', k_dec, vc)
        return S, o_inter + o_intra

    S0 = jnp.zeros((B, GLA_HEADS, GLA_HK, GLA_HV), jnp.float32)
    _, o = lax.scan(step, S0, (to_chunks(q), to_chunks(k), to_chunks(v), to_chunks(log_a)))
    o = o.transpose(1, 0, 3, 2, 4).reshape(B, L, GLA_HEADS, GLA_HV).astype(gq.dtype)
    o = rmsnorm(o, g_norm).reshape(B, L, GLA_DV)
    return o * jax.nn.silu(gg)


def peer_ffn(h, w_q, sub_keys, u_tab, v_tab):
    B, L, D = h.shape
    q = (h @ w_q).reshape(B, L, PEER_HEADS, 2, PEER_HALF)
    s = jnp.einsum('blhpd,pnd->blhpn', q, sub_keys).astype(jnp.float32)
    top_s, top_i = lax.top_k(s, PEER_TOPK)
    cand_s = top_s[..., 0, :, None] + top_s[..., 1, None, :]
    cand_i = top_i[..., 0, :, None] * PEER_NKEYS + top_i[..., 1, None, :]
    best_s, best_pos = lax.top_k(cand_s.reshape(B, L, PEER_HEADS, PEER_TOPK * PEER_TOPK), PEER_TOPK)
    idx = jnp.take_along_axis(cand_i.reshape(B, L, PEER_HEADS, PEER_TOPK * PEER_TOPK), best_pos, axis=-1)
    gate = jax.nn.softmax(best_s, axis=-1)
    nb = L // PEER_BLOCK

    def blocks(t):
        return t.reshape((B, nb, PEER_BLOCK) + t.shape[2:]).swapaxes(0, 1)

    def expert_block(inp):
        hb, ib, gb = inp
        u = u_tab[ib]
        act = jax.nn.gelu(jnp.einsum('bpd,bphkd->bphk', hb, u).astype(jnp.float32), approximate=False)
        coef = (act * gb).astype(hb.dtype)
        return jnp.einsum('bphk,bphkd->bpd', coef, v_tab[ib])

    out = lax.map(expert_block, (blocks(h), blocks(idx), blocks(gate)))
    return out.swapaxes(0, 1).reshape(B, L, D)


def setup_inputs(seed: int = 0) -> dict:
    key = jax.random.key(seed)
    ks = jax.random.split(key, 24)
    f32 = jnp.float32

    def nrm(k, shape, fan_in):
        return jax.random.normal(k, shape, f32) * (fan_in ** -0.5)

    def gain(k, shape):
        return 1.0 + 0.02 * jax.random.normal(k, shape, f32)

    return {
        "x": jax.random.normal(ks[0], (BATCH, SEQ, D_MODEL), f32),
        "meta": jax.random.normal(ks[1], (N_META, D_MODEL), f32),
        "norm_mix": gain(ks[2], (DEPTH, D_MODEL)),
        "w_in": nrm(ks[3], (DEPTH, D_MODEL, D_IN), D_MODEL),
        "mla_q_norm": gain(ks[4], (DEPTH, MLA_Q_RANK)),
        "mla_w_uq": nrm(ks[5], (DEPTH, MLA_Q_RANK, MLA_HEADS * (MLA_NOPE + MLA_ROPE)), MLA_Q_RANK),
        "mla_kv_norm": gain(ks[6], (DEPTH, MLA_KV_RANK)),
        "mla_w_ukv": nrm(ks[7], (DEPTH, MLA_KV_RANK, MLA_HEADS * (MLA_NOPE + MLA_V)), MLA_KV_RANK),
        "qn_nope": gain(ks[8], (DEPTH, MLA_NOPE)),
        "qn_pe": gain(ks[9], (DEPTH, MLA_ROPE)),
        "kn_nope": gain(ks[10], (DEPTH, MLA_NOPE)),
        "kn_pe": gain(ks[11], (DEPTH, MLA_ROPE)),
        "gla_w_a2": nrm(ks[12], (DEPTH, GLA_GATE_RANK, GLA_DK), GLA_GATE_RANK),
        "gla_b_a": 0.1 * jax.random.normal(ks[13], (DEPTH, GLA_DK), f32),
        "gla_norm": gain(ks[14], (DEPTH, GLA_HV)),
        "w_o_mla": nrm(ks[15], (DEPTH, MLA_HEADS * MLA_V, D_MODEL), MLA_HEADS * MLA_V),
        "w_o_gla": nrm(ks[16], (DEPTH, GLA_DV, D_MODEL), GLA_DV),
        "w_out": nrm(ks[17], (DEPTH, D_MODEL, D_MODEL), D_MODEL),
        "norm_ffn": gain(ks[18], (DEPTH, D_MODEL)),
        "peer_w_q": nrm(ks[19], (DEPTH, D_MODEL, PEER_HEADS * PEER_QDIM), D_MODEL),
        "peer_keys": nrm(ks[20], (DEPTH, 2, PEER_NKEYS, PEER_HALF), PEER_HALF),
        "peer_u": nrm(ks[21], (DEPTH, PEER_EXPERTS, D_MODEL), D_MODEL),
        "peer_v": nrm(ks[22], (DEPTH, PEER_EXPERTS, D_MODEL), PEER_HEADS),
    }


def reference(x, meta, norm_mix, w_in, mla_q_norm, mla_w_uq, mla_kv_norm, mla_w_ukv, qn_nope, qn_pe, kn_nope, kn_pe, gla_w_a2, gla_b_a, gla_norm, w_o_mla, w_o_gla, w_out, norm_ffn, peer_w_q, peer_keys, peer_u, peer_v):
    B = x.shape[0]
    h = jnp.concatenate([jnp.broadcast_to(meta.astype(x.dtype)[None], (B, N_META, D_MODEL)), x], axis=1)
    L = h.shape[1]
    lp = -(-L // Q_BLOCK) * Q_BLOCK
    pos = jnp.arange(lp)
    cid = jnp.where(pos < N_META, 0, 1 + (pos - N_META) // CHUNK)
    cos, sin = rope_tables(L)
    offsets = [int(o) for o in np.cumsum(SPLITS)[:-1]]
    for l in range(DEPTH):
        n = rmsnorm(h, norm_mix[l])
        parts = jnp.split(n @ w_in[l], offsets, axis=-1)
        c_q, c_kv, k_pe, gq, gk, gv, ga, gg, gate_a, gate_b = parts
        y_a = mla_branch(c_q, c_kv, k_pe, mla_q_norm[l], mla_w_uq[l], mla_kv_norm[l], mla_w_ukv[l], qn_nope[l], qn_pe[l], kn_nope[l], kn_pe[l], cos, sin, cid)
        y_b = gla_branch(gq, gk, gv, ga, gg, gla_w_a2[l], gla_b_a[l], gla_norm[l])
        mix = jax.nn.sigmoid(gate_a) * (y_a @ w_o_mla[l]) + jax.nn.sigmoid(gate_b) * (y_b @ w_o_gla[l])
        h = h + mix @ w_out[l]
        h = h + peer_ffn(rmsnorm(h, norm_ffn[l]), peer_w_q[l], peer_keys[l], peer_u[l], peer_v[l])
    return h[:, N_META:]
```

```python
import numpy as np
from contextlib import ExitStack
import concourse.bass as bass
import concourse.mybir as mybir
from concourse.bass_utils import run_bass_kernel_spmd

F32 = mybir.dt.float32
BF16 = mybir.dt.bfloat16
I32 = mybir.dt.int32
U32 = mybir.dt.uint32
AF = mybir.ActivationFunctionType
ALU = mybir.AluOpType
AX = mybir.AxisListType

EPS = 1e-6
NCORES = 8
ENGS = ("pe", "dve", "act", "pool", "sp")
EPOCH = 30000


class Prog:
    def __init__(self, nc, stack):
        self.nc = nc
        self.stack = stack
        self.q = {e: [] for e in ENGS}
        self.cnt = {e: 0 for e in ENGS}
        self.known = {e: {} for e in ENGS}
        self.sems = {}
        self.st = {}
        self.ring = {"sp": [("dma", "sp", i) for i in range(12)], "pool": [("dma", "pool", i) for i in range(8)],
                     "act": [("dma", "act", i) for i in range(4)]}
        self.ring_pos = {k: 0 for k in self.ring}
        self.ring_ev = {}
        self.ring_uses = {}
        self.final = []
        self.alias = {}

    def _x(self, keys):
        out = []
        for k in keys:
            out.extend(self.alias.get(k, (k,)))
        return out

    def _sem(self, key):
        if key not in self.sems:
            name = "s_" + "_".join(str(k) for k in key)
            self.sems[key] = self.stack.enter_context(self.nc.semaphore(name))
        return self.sems[key]

    def _deps(self, r, w):
        r = self._x(r)
        w = self._x(w)
        deps = []
        for k in r:
            s = self.st.get(k)
            if s and s[0]:
                deps.append(s[0])
        for k in w:
            s = self.st.get(k)
            if s:
                if s[0]:
                    deps.append(s[0])
                deps.extend(s[1].values())
        return deps

    def _wait(self, eng, deps):
        for (sk, val) in deps:
            if eng == "pe" and sk[0] == "pe":
                continue
            if self.known[eng].get(sk, 0) >= val:
                continue
            self.known[eng][sk] = val
            self._sem(sk)
            self.q[eng].append(("wait", sk, val))

    def _record(self, ev, r, w):
        r = self._x(r)
        w = self._x(w)
        for k in w:
            self.st[k] = [ev, {}]
        for k in r:
            s = self.st.setdefault(k, [None, {}])
            s[1][ev[0]] = ev

    def op(self, eng, fn, r=(), w=()):
        self._wait(eng, self._deps(r, w))
        self.cnt[eng] += 1
        c = self.cnt[eng]
        ep = (c - 1) // EPOCH
        ev = ((eng, ep), c - ep * EPOCH)
        self._sem(ev[0])
        self.q[eng].append(("op", fn, ev[0], 1))
        self._record(ev, r, w)
        return ev

    def dma(self, eng, fn, r=(), w=(), final=False):
        ring = self.ring[eng]
        sk = ring[self.ring_pos[eng] % len(ring)]
        self.ring_pos[eng] += 1
        deps = self._deps(r, w)
        if sk in self.ring_ev:
            deps.append(self.ring_ev[sk])
        self._wait(eng, deps)
        self.ring_uses[sk] = self.ring_uses.get(sk, 0) + 1
        ev = (sk, 16 * self.ring_uses[sk])
        self.ring_ev[sk] = ev
        self._sem(sk)
        self.q[eng].append(("op", fn, sk, 16))
        self._record(ev, r, w)
        if final:
            self.final.append(ev)
        return ev

    def flush(self):
        nc = self.nc
        self._wait("sp", self.final)
        self._wait("sp", list(self.ring_ev.values()))
        print("PROG counts", self.cnt, {k: len(v) for k, v in self.q.items()}, flush=True)
        q = self.q
        sems = self.sems

        def run(name, e):
            for it in q[name]:
                if it[0] == "wait":
                    e.wait_ge(sems[it[1]], it[2])
                else:
                    ins = it[1](e)
                    ins.then_inc(sems[it[2]], it[3])

        with nc.Block() as block:
            @block.tensor
            def _(e):
                run("pe", e)

            @block.vector
            def _(e):
                run("dve", e)

            @block.scalar
            def _(e):
                run("act", e)

            @block.gpsimd
            def _(e):
                run("pool", e)

            @block.sync
            def _(e):
                run("sp", e)


C_CQ, C_CKV, C_KPE, C_GQ, C_GK, C_GV, C_GA, C_GG, C_GTA, C_GTB, C_END = (
    0, 384, 640, 704, 1216, 1728, 2752, 2768, 3792, 4816, 5840)
NKT = 17


def build(nseq, ntile, stop=None):
    nc = bass.Bass("TRN2", target_bir_lowering=False)
    stack = ExitStack()
    P = Prog(nc, stack)

    def din(name, shape, dt=F32):
        return nc.dram_tensor(name, list(shape), dt, kind="ExternalInput").ap()

    x = din("x", [nseq, 2048, 1024])
    xm = din("xm", [128, 1024])
    Wd = {
        "in": (din("w_in", [1024, 5840]), 8, 5840, 0),
        "uq": (din("w_uq", [384, 1536]), 3, 1536, 8),
        "ukv": (din("w_ukv", [256, 2048]), 2, 2048, 11),
        "oa": (din("w_oa", [1024, 1024]), 8, 1024, None),
        "ob": (din("w_ob", [1024, 1024]), 8, 1024, None),
        "out": (din("w_out", [1024, 1024]), 8, 1024, None),
        "pq": (din("w_pq", [1024, 2048]), 8, 2048, None),
    }
    gcol_d = din("gcol", [128, 13])
    gb_d = din("gb", [128, 1664])
    wa2_d = din("wa2", [32, 512])
    keysT_d = din("keysT", [128, 256])
    NEXP = 16384 if stop in (None, "H") else 128
    uv = din("uv", [NEXP, 2048])
    rope_d = din("rope", [NKT, 128, 64])
    cst_d = din("cst", [128, 1152])
    y = nc.dram_tensor("y", [nseq, 2048, 1024], F32, kind="ExternalOutput").ap()
    ws = {k: nc.dram_tensor("ws_" + k, [128, v[1], v[2]], BF16, kind="Internal").ap() for k, v in Wd.items()}

    def sb(name, shape, dt=F32):
        return stack.enter_context(nc.sbuf_tensor("sb_" + name, list(shape), dt))

    def ps(name, shape, dt=F32):
        return stack.enter_context(nc.psum_tensor("ps_" + name, list(shape), dt))

    cst = sb("cst", [128, 1152])
    IDENT = cst[:, 0:128]
    TRI = cst[:, 128:256]
    SUT = cst[:, 256:384]
    TRIM = cst[:, 384:512]
    SUTM = cst[:, 512:640]
    MASKUT = cst[:, 640:1152]
    identb = sb("identb", [128, 128], BF16)
    gcol = sb("gcol", [128, 13])
    gbt = sb("gbt", [128, 1664])
    G_QN, G_QP, G_KN, G_KP, G_GLA, G_FFN = (gbt[:, 0:128], gbt[:, 128:192], gbt[:, 192:320], gbt[:, 320:384],
                                          gbt[:, 384:640], gbt[:, 640:1664])
    wa2 = sb("wa2", [32, 512])
    keysT = sb("keysT", [128, 256])
    ropet = sb("ropet", [128, NKT, 64])

    KT = sb("KT", [128, 8, NKT * 128], BF16)
    KPT = sb("KPT", [128, NKT * 128], BF16)
    VC = sb("VC", [128, NKT, 8, 132], BF16)
    S = sb("S", [128, 4, 256])
    S0 = sb("S0", [128, 4, 256])
    Sb = sb("Sb", [128, 4, 256], BF16)

    NWS = 2
    wslot = [sb(f"wslot{i}", [128, 8, 512], BF16) for i in range(NWS)]
    NUV = 3
    ND = 4

    AW = 7168
    arena = sb("arena", [128, AW])

    def carve(name, off, shape, dt=F32):
        per = 1
        for d in shape[1:]:
            per *= d
        words = per if dt in (F32, I32, U32) else per // 2
        assert off + words <= AW, name
        v = arena[:, off:off + words]
        if dt != F32:
            v = v.bitcast(dt)
        if len(shape) == 3:
            v = v.rearrange("p (a b) -> p a b", a=shape[1])
        P.alias[name] = tuple(("ar", g) for g in range(off // 128, (off + words - 1) // 128 + 1))
        return v

    lat = carve("lat", 0, [128, 704])
    qsb = carve("qsb", 704, [128, 8, 192])
    kvs = carve("kvs", 2240, [128, 8, 128])
    knn = carve("knn", 3264, [128, 8, 128], BF16)
    qnn = carve("qnn", 3776, [128, 8, 128], BF16)
    qpe = carve("qpe", 4288, [128, 8, 64])
    qpr = carve("qpr", 4800, [128, 8, 64], BF16)
    cqn = carve("cqn", 5056, [128, 384], BF16)
    ckn = carve("ckn", 5248, [128, 256], BF16)
    cqT = carve("cqT", 5376, [128, 3, 128], BF16)
    ckT = carve("ckT", 5568, [128, 2, 128], BF16)
    kp = carve("kp", 5696, [128, 64])
    kp2 = carve("kp2", 5760, [128, 128], BF16)
    rt = carve("rt", 5824, [128, 8, 32])
    rt2 = carve("rt2", 6080, [128, 8, 32])
    e1 = carve("e1", 0, [128, 512])
    lsp = carve("lsp", 512, [128, 512])
    Ep = carve("Ep", 1024, [128, 512])
    En = carve("En", 1536, [128, 512])
    QtT = carve("QtT", 2048, [128, 4, 128], BF16)
    KtT = carve("KtT", 2304, [128, 4, 128], BF16)
    Khat = carve("Khat", 2560, [128, 512], BF16)
    At = carve("At", 2816, [128, 512], BF16)
    n2b = carve("n2b", 0, [128, 1024], BF16)
    pqT = carve("pqT", 512, [128, 4, 128])
    ss2 = carve("ss2", 1024, [128, 4, 128])
    tsv = carve("tsv", 1536, [128, 16, 16])
    tiu = carve("tiu", 1792, [128, 16, 16], U32)
    tif = carve("tif", 2048, [128, 16, 16])
    cands = carve("cands", 2304, [128, 256])
    cands2 = carve("cands2", 2560, [128, 256])
    candi = carve("candi", 2816, [128, 256])
    dgb = [carve(f"dgb{i}", 3072 + 64 * i, [128, 128], BF16) for i in range(ND)]
    uvb = [carve(f"uvb{i}", 3328 + 1024 * i, [128, 2048], BF16) for i in range(NUV)]
    stg = [carve(f"stg{i}", 512 * i, [128, 512]) for i in range(2)]
    stgb = [carve(f"stgb{i}", 1024 + 256 * i, [128, 512], BF16) for i in range(2)]

    xt = [sb("xt0", [128, 1024])]
    junk = sb("junk", [128, 1024], BF16)
    junk2 = sb("junk2", [128, 1024], BF16)
    nb = sb("nb", [128, 1024], BF16)
    nT = sb("nT", [128, 8, 128], BF16)
    st1 = sb("st1", [128, 64])
    gqT = sb("gqT", [128, 4, 128])
    gkT = sb("gkT", [128, 4, 128])
    gktm = sb("gktm", [128, 512])
    gv = sb("gv", [128, 1024], BF16)
    gaT = sb("gaT", [32, 128])
    sg = sb("sg", [128, 1024], BF16)
    gas = sb("gas", [128, 1024], BF16)
    gbs = sb("gbs", [128, 1024], BF16)
    qnT = sb("qnT", [128, 8, 128], BF16)
    qpT = sb("qpT", [128, 4, 128], BF16)
    pb = [sb(f"pb{i}", [128, 512], BF16) for i in range(3)]
    pm = [sb(f"pm{i}", [16, 128], BF16) for i in range(2)]
    ya = sb("ya", [128, 1024], BF16)
    yb = sb("yb", [128, 1024], BF16)
    yT = sb("yT", [128, 8, 128], BF16)
    MG = sb("MG", [128, 2048])
    mix = MG[:, 0:1024]
    gsg = MG[:, 1024:2048]
    sq = MG[:, 0:1536].rearrange("p (a b) -> p a b", a=8)
    for nm in ("mix", "gsg", "sq"):
        P.alias[nm] = ("MG",)
    mixb = sb("mixb", [128, 1024], BF16)
    bsv = sb("bsv", [128, 8, 16])
    idxf = sb("idxf", [128, 128])
    idxi = sb("idxi", [128, 128], I32)
    gate = sb("gate", [128, 128])
    actv = sb("actv", [128, 128])
    glv = sb("glv", [128, 128])

    pf = [ps(f"pf{i}", [128, 512]) for i in range(6)]
    pt = [ps(f"pt{i}", [128, 1024], BF16) for i in range(2)]
    cnt = {"mm": 0, "s": 0, "t": 0, "w": 0, "pb": 0, "pm": 0, "stg": 0, "uv": 0, "dg": 0}

    def rot(kind, n):
        v = cnt[kind] % n
        cnt[kind] += 1
        return v

    def bank_mm():
        i = rot("mm", 2)
        return pf[i], f"pf{i}"

    def bank_s():
        i = 2 + rot("s", 2)
        return pf[i], f"pf{i}"

    def bank_t():
        i = rot("t", 2)
        return pt[i], f"pt{i}"

    def dma_sp(out, in_, r, w, final=False):
        P.dma("sp", lambda e: e.dma_start(out=out, in_=in_), r, w, final)

    def mm(out, lhsT, rhs, start, stop, r, w):
        P.op("pe", lambda e: e.matmul(out, lhsT, rhs, start=start, stop=stop), r, w)

    def tr(out, in_, idn, r, w):
        P.op("pe", lambda e: e.transpose(out, in_, idn), r, w)

    def act(out, in_, func, r, w, bias=None, scale=None, accum_out=None):
        kw = {}
        if bias is not None:
            kw["bias"] = bias
        if scale is not None:
            kw["scale"] = scale
        if accum_out is not None:
            kw["accum_out"] = accum_out
        P.op("act", lambda e: e.activation(out=out, in_=in_, func=func, **kw), r, w)

    def tt(out, in0, in1, op, r, w, eng="dve"):
        P.op(eng, lambda e: e.tensor_tensor(out=out, in0=in0, in1=in1, op=op), r, w)

    def tsc(out, in0, s1, s2, op0, op1, r, w, eng="dve", accum_out=None):
        if op1 is None:
            P.op(eng, lambda e: e.tensor_scalar(out=out, in0=in0, scalar1=s1, scalar2=None, op0=op0), r, w)
        else:
            P.op(eng, lambda e: e.tensor_scalar(out=out, in0=in0, scalar1=s1, scalar2=s2, op0=op0, op1=op1,
                                                accum_out=accum_out), r, w)

    def stt(out, in0, scalar, in1, op0, op1, r, w, accum_out=None):
        P.op("dve", lambda e: e.scalar_tensor_tensor(out=out, in0=in0, scalar=scalar, in1=in1, op0=op0, op1=op1,
                                                     accum_out=accum_out), r, w)

    def cp(out, in_, r, w, eng="dve"):
        if eng == "act":
            P.op("act", lambda e: e.copy(out=out, in_=in_), r, w)
        else:
            P.op(eng, lambda e: e.tensor_copy(out=out, in_=in_), r, w)

    def recip(out, in_, r, w):
        P.op("dve", lambda e: e.reciprocal(out=out, in_=in_), r, w)

    def red(out, in_, op, r, w):
        P.op("dve", lambda e: e.tensor_reduce(out=out, in_=in_, axis=AX.X, op=op), r, w)

    def rstd_from_ss(dst, ssum, n, r, w):
        act(dst, ssum, AF.Sqrt, tuple(r) + ("epsb",), w, bias=epsb[:, 0:1], scale=1.0 / n)
        recip(dst, dst, w, w)

    epsb = sb("epsb", [128, 1])
    P.op("dve", lambda e: e.memset(epsb[:], EPS), (), ("epsb",))

    import os
    SK = os.environ.get("DBGSKIP", "").split(",")
    if "cst" not in SK:
        dma_sp(cst[:], cst_d, (), ("cst",))
    if "gcol" not in SK:
        dma_sp(gcol[:], gcol_d, (), ("gcol",))
    if "gbt" not in SK:
        dma_sp(gbt[:], gb_d, (), ("gbt",))
    if "wa2" not in SK:
        dma_sp(wa2[:], wa2_d, (), ("wa2",))
    if "keysT" not in SK:
        dma_sp(keysT[:], keysT_d, (), ("keysT",))
    if "rope" not in SK:
        dma_sp(ropet[:], rope_d.rearrange("t p c -> p t c"), (), ("ropet",))
    if "ms" not in SK:
        cp(identb[:], IDENT, ("cst",), ("identb",))
        P.op("dve", lambda e: e.memset(gaT[:], 1.0), (), ("gaT",))
        P.op("dve", lambda e: e.memset(VC[:], 1.0), (), tuple(f"V{t}" for t in range(NKT)))
        P.op("dve", lambda e: e.memset(S0[:], 0.0), (), ("S0",))

    for name, (wd, kc_n, ncol, gc) in Wd.items():
        if stop == "P1" or (stop == "P2" and name != "ukv"):
            continue
        for kc in range(kc_n):
            for c0 in range(0, ncol, 512):
                c1 = min(ncol, c0 + 512)
                i = rot("stg", 2)
                dma_sp(stg[i][:, 0:c1 - c0], wd[kc * 128:(kc + 1) * 128, c0:c1], (), (f"stg{i}",))
                if gc is None:
                    cp(stgb[i][:, 0:c1 - c0], stg[i][:, 0:c1 - c0], (f"stg{i}",), (f"stgb{i}",),
                       eng=("act" if (cnt["stg"] % 4 < 2) else "dve"))
                else:
                    tsc(stgb[i][:, 0:c1 - c0], stg[i][:, 0:c1 - c0], gcol[:, gc + kc:gc + kc + 1], None, ALU.mult,
                        None, (f"stg{i}", "gcol"), (f"stgb{i}",))
                dma_sp(ws[name][:, kc, c0:c1], stgb[i][:, 0:c1 - c0], (f"stgb{i}",), (f"ws_{name}",))

    def load_w(name, c0, c1):
        kc_n = Wd[name][1]
        i = rot("w", NWS)
        dma_sp(wslot[i][:, 0:kc_n, 0:c1 - c0], ws[name][:, :, c0:c1], (f"ws_{name}",), (f"wslot{i}",))
        return wslot[i], f"wslot{i}", kc_n

    def proj_tm(name, c0, c1, lhs, lhs_key):
        wsl, wk, kc_n = load_w(name, c0, c1)
        bk, bkey = bank_mm()
        for kc in range(kc_n):
            mm(bk[:, 0:c1 - c0], lhs[:, kc, :], wsl[:, kc, 0:c1 - c0], kc == 0, kc == kc_n - 1, (lhs_key, wk), (bkey,))
        return bk, bkey

    def transposes(dst, dst_key, src, src_key, nblk, eng="dve"):
        bk, bkey = bank_t()
        for j in range(nblk):
            tr(bk[:, j * 128:(j + 1) * 128], src[:, j * 128:(j + 1) * 128], identb[:], (src_key, "identb"), (bkey,))
        cp(dst, bk[:, 0:nblk * 128], (bkey,), (dst_key,), eng=eng)

    SCALE_ATT = 192.0 ** -0.5
    SCALE_GLA = 128.0 ** -0.5

    def rope_apply(dst, src, t, nh, r, w):
        cos = ropet[:, t, 0:32].unsqueeze(1).to_broadcast([128, nh, 32])
        sin = ropet[:, t, 32:64].unsqueeze(1).to_broadcast([128, nh, 32])
        x1 = src[:, :, 0:32]
        x2 = src[:, :, 32:64]
        a = rt[:, 0:nh, :]
        b = rt2[:, 0:nh, :]
        tt(a, x1, cos, ALU.mult, r + ("ropet",), ("rt",))
        tt(b, x2, sin, ALU.mult, r + ("ropet",), ("rt2",))
        tt(dst[:, :, 0:32], a, b, ALU.subtract, ("rt", "rt2"), w)
        tt(a, x2, cos, ALU.mult, r + ("ropet",), ("rt",))
        tt(b, x1, sin, ALU.mult, r + ("ropet",), ("rt2",))
        tt(dst[:, :, 32:64], a, b, ALU.add, ("rt", "rt2"), w)

    def dump(s, t, items):
        X = xt[0]
        o = 0
        for ap, key, n in items:
            cp(X[:ap.shape[0], o:o + n], ap, (key,), ("xt0",))
            o += n
        dma_sp(y[s, (t - 1) * 128:t * 128, :], X[:], ("xt0",), ("y",), final=True)

    def tile(s, t, par):
        is_meta = s is None
        X = xt[0]
        xk = "xt0"
        if is_meta:
            dma_sp(X[:], xm, (), (xk,))
        else:
            dma_sp(X[:], x[s, (t - 1) * 128:t * 128, :], (), (xk,))
        act(junk[:], X[:], AF.Square, (xk,), ("st1a",), accum_out=st1[:, 0:1])
        rstd_from_ss(st1[:, 0:1], st1[:, 0:1], 1024.0, ("st1a",), ("st1a",))
        tsc(nb[:], X[:], st1[:, 0:1], None, ALU.mult, None, (xk, "st1a"), ("nb",))
        transposes(nT[:].rearrange("p a b -> p (a b)"), "nT", nb, "nb", 8)

        if stop == "A":
            if not is_meta:
                dump(s, t, [(nb[:], "nb", 1024)])
            return
        bk, bkey = proj_tm("in", 0, 512, nT, "nT")
        cp(lat[:, 0:512], bk[:, 0:512], (bkey,), ("lat",), eng="act")
        bk, bkey = proj_tm("in", 512, 704, nT, "nT")
        cp(lat[:, 512:704], bk[:, 0:192], (bkey,), ("lat",), eng="act")
        wsl, wk, _ = load_w("in", C_GQ, C_GQ + 512)
        bk, bkey = bank_mm()
        for h in range(4):
            for kc in range(8):
                mm(bk[:, h * 128:(h + 1) * 128], wsl[:, kc, h * 128:(h + 1) * 128], nT[:, kc, :], kc == 0, kc == 7,
                   ("nT", wk), (bkey,))
        cp(gqT[:].rearrange("p a b -> p (a b)"), bk[:, :], (bkey,), ("gqT",), eng="act")
        wsl, wk, _ = load_w("in", C_GK, C_GK + 512)
        bk, bkey = bank_mm()
        for h in range(4):
            for kc in range(8):
                mm(bk[:, h * 128:(h + 1) * 128], wsl[:, kc, h * 128:(h + 1) * 128], nT[:, kc, :], kc == 0, kc == 7,
                   ("nT", wk), (bkey,))
        cp(gkT[:].rearrange("p a b -> p (a b)"), bk[:, :], (bkey,), ("gkT",), eng="act")
        bk, bkey = bank_mm()
        for kc in range(8):
            mm(bk[:, :], nT[:, kc, :], wsl[:, kc, :], kc == 0, kc == 7, ("nT", wk), (bkey,))
        cp(gktm[:], bk[:, :], (bkey,), ("gktm",), eng="dve")
        for j in range(2):
            bk, bkey = proj_tm("in", C_GV + j * 512, C_GV + (j + 1) * 512, nT, "nT")
            cp(gv[:, j * 512:(j + 1) * 512], bk[:, :], (bkey,), ("gv",), eng=("act" if j == 0 else "dve"))
        wsl, wk, _ = load_w("in", C_GA, C_GA + 16)
        bk, bkey = bank_mm()
        for kc in range(8):
            mm(bk[0:16, 0:128], wsl[:, kc, 0:16], nT[:, kc, :], kc == 0, kc == 7, ("nT", wk), (bkey,))
        cp(gaT[0:16, :], bk[0:16, 0:128], (bkey,), ("gaT",), eng="dve")
        if not is_meta:
            for j in range(2):
                bk, bkey = proj_tm("in", C_GG + j * 512, C_GG + (j + 1) * 512, nT, "nT")
                act(sg[:, j * 512:(j + 1) * 512], bk[:, :], AF.Silu, (bkey,), ("sg",))
            for j in range(2):
                bk, bkey = proj_tm("in", C_GTA + j * 512, C_GTA + (j + 1) * 512, nT, "nT")
                act(gas[:, j * 512:(j + 1) * 512], bk[:, :], AF.Sigmoid, (bkey,), ("gas",))
            for j in range(2):
                bk, bkey = proj_tm("in", C_GTB + j * 512, C_GTB + (j + 1) * 512, nT, "nT")
                act(gbs[:, j * 512:(j + 1) * 512], bk[:, :], AF.Sigmoid, (bkey,), ("gbs",))

        if stop == "B":
            if not is_meta:
                dump(s, t, [(lat[:, :], "lat", 704), (gv[:, 0:320], "gv", 320)])
            return
        tt(sq[:].rearrange("p a b -> p (a b)")[:, 0:704], lat[:, :], lat[:, :], ALU.mult, ("lat",), ("sq",))
        sqf = sq[:].rearrange("p a b -> p (a b)")
        red(st1[:, 1:2], sqf[:, 0:384], ALU.add, ("sq",), ("st1b",))
        red(st1[:, 2:3], sqf[:, 384:640], ALU.add, ("sq",), ("st1b",))
        red(st1[:, 3:4], sqf[:, 640:704], ALU.add, ("sq",), ("st1b",))
        rstd_from_ss(st1[:, 1:2], st1[:, 1:2], 384.0, ("st1b",), ("st1b",))
        rstd_from_ss(st1[:, 2:3], st1[:, 2:3], 256.0, ("st1b",), ("st1b",))
        rstd_from_ss(st1[:, 3:4], st1[:, 3:4], 64.0, ("st1b",), ("st1b",))
        tsc(ckn[:], lat[:, 384:640], st1[:, 2:3], None, ALU.mult, None, ("lat", "st1b"), ("ckn",))
        transposes(ckT[:].rearrange("p a b -> p (a b)"), "ckT", ckn, "ckn", 2)
        stt(kp[:], lat[:, 640:704], st1[:, 3:4], G_KP, ALU.mult, ALU.mult, ("lat", "st1b", "gbt"), ("kp",))
        kpv = kp[:].unsqueeze(1)
        kp2v = kp2[:].rearrange("p (a b) -> p a b", a=2)
        rope_apply(kp2v[:, 0:1, :], kpv, t, 1, ("kp",), ("kp2",))
        cp(kp2[:, 64:128], kp2[:, 0:64], ("kp2",), ("kp2",))
        bk, bkey = bank_t()
        tr(bk[:, 0:128], kp2[:], identb[:], ("kp2", "identb"), (bkey,))
        cp(KPT[:, t * 128:(t + 1) * 128], bk[:, 0:128], (bkey,), (f"KP{t}",))
        if stop == "C1":
            if not is_meta:
                dump(s, t, [])
            return
        for j in range(4):
            bk, bkey = proj_tm("ukv", j * 512, (j + 1) * 512, ckT, "ckT")
            bv = bk[:, :].rearrange("p (h c) -> p h c", h=2)
            ee = "act" if j % 2 == 0 else "dve"
            cp(kvs[:, 2 * j:2 * j + 2, :], bv[:, :, 0:128], (bkey,), ("kvs",), eng=ee)
            cp(VC[:, t, 2 * j:2 * j + 2, 0:128], bv[:, :, 128:256], (bkey,), (f"V{t}",), eng=ee)
        if stop == "C1a":
            if not is_meta:
                dump(s, t, [])
            return
        tt(sq[:, :, 0:128], kvs[:], kvs[:], ALU.mult, ("kvs",), ("sq",))
        red(st1[:, 8:16], sq[:, :, 0:128], ALU.add, ("sq",), ("st1c",))
        rstd_from_ss(st1[:, 8:16], st1[:, 8:16], 128.0, ("st1c",), ("st1c",))
        if stop == "C1b":
            if not is_meta:
                dump(s, t, [])
            return
        tt(kvs[:], kvs[:], st1[:, 8:16].unsqueeze(2).to_broadcast([128, 8, 128]), ALU.mult, ("kvs", "st1c"), ("kvs",))
        tt(knn[:], kvs[:], G_KN.unsqueeze(1).to_broadcast([128, 8, 128]), ALU.mult, ("kvs", "gbt"), ("knn",))
        if stop == "C1c":
            if not is_meta:
                dump(s, t, [])
            return
        bk, bkey = bank_t()
        for h in range(8):
            tr(bk[:, h * 128:(h + 1) * 128], knn[:, h, :], identb[:], ("knn", "identb"), (bkey,))
        cp(KT[:, :, t * 128:(t + 1) * 128], bk[:, :].rearrange("p (h c) -> p h c", h=8), (bkey,), (f"K{t}",), eng="act")

        if stop == "C2":
            if not is_meta:
                dump(s, t, [])
            return
        if not is_meta:
            tsc(cqn[:], lat[:, 0:384], st1[:, 1:2], None, ALU.mult, None, ("lat", "st1b"), ("cqn",))
            transposes(cqT[:].rearrange("p a b -> p (a b)"), "cqT", cqn, "cqn", 3)
            qf = qsb[:].rearrange("p a b -> p (a b)")
            for j in range(3):
                bk, bkey = proj_tm("uq", j * 512, (j + 1) * 512, cqT, "cqT")
                cp(qf[:, j * 512:(j + 1) * 512], bk[:, :], (bkey,), ("qsb",), eng=("act" if j != 1 else "dve"))
            tt(sq[:], qsb[:], qsb[:], ALU.mult, ("qsb",), ("sq",))
            red(st1[:, 16:24], sq[:, :, 0:128], ALU.add, ("sq",), ("st1d",))
            red(st1[:, 24:32], sq[:, :, 128:192], ALU.add, ("sq",), ("st1d",))
            rstd_from_ss(st1[:, 16:24], st1[:, 16:24], 128.0, ("st1d",), ("st1d",))
            rstd_from_ss(st1[:, 24:32], st1[:, 24:32], 64.0, ("st1d",), ("st1d",))
            tt(qsb[:, :, 0:128], qsb[:, :, 0:128], st1[:, 16:24].unsqueeze(2).to_broadcast([128, 8, 128]), ALU.mult,
               ("qsb", "st1d"), ("qsb",))
            tt(qnn[:], qsb[:, :, 0:128], G_QN.unsqueeze(1).to_broadcast([128, 8, 128]), ALU.mult, ("qsb", "gbt"),
               ("qnn",))
            tt(qsb[:, :, 128:192], qsb[:, :, 128:192], st1[:, 24:32].unsqueeze(2).to_broadcast([128, 8, 64]), ALU.mult,
               ("qsb", "st1d"), ("qsb",))
            tt(qpe[:], qsb[:, :, 128:192], G_QP.unsqueeze(1).to_broadcast([128, 8, 64]), ALU.mult, ("qsb", "gbt"),
               ("qpe",))
            rope_apply(qpr[:], qpe[:], t, 8, ("qpe",), ("qpr",))
            transposes(qnT[:].rearrange("p a b -> p (a b)"), "qnT", qnn[:].rearrange("p a b -> p (a b)"), "qnn", 8,
                       eng="act")
            transposes(qpT[:].rearrange("p a b -> p (a b)"), "qpT", qpr[:].rearrange("p a b -> p (a b)"), "qpr", 4,
                       eng="act")

            if stop == "C":
                dump(s, t, [(qnn[:].rearrange("p a b -> p (a b)")[:, 0:512], "qnn", 512),
                            (qpr[:].rearrange("p a b -> p (a b)"), "qpr", 512)])
                return
            kts = list(range(1, t + 1))
            for h in range(8):
                hp0 = (h % 2) * 64
                acc = pf[4 + (h % 2)]
                akey = f"pf{4 + (h % 2)}"
                bk, bkey = bank_s()
                mm(bk[0:16, 0:128], KT[:, h, 0:16], qnT[:, h, :], True, False, ("K0", "qnT"), (bkey,))
                mm(bk[0:16, 0:128], KPT[hp0:hp0 + 64, 0:16], qpT[hp0:hp0 + 64, h // 2, :], False, True,
                   ("KP0", "qpT"), (bkey,))
                mi = rot("pm", 2)
                act(pm[mi][:], bk[0:16, 0:128], AF.Exp, (bkey,), (f"pm{mi}",), scale=SCALE_ATT)
                mm(acc[:, 0:129], pm[mi][:], VC[0:16, 0, h, 0:129], True, False, (f"pm{mi}", "V0"), (akey,))
                for g0 in range(0, len(kts), 4):
                    grp = kts[g0:g0 + 4]
                    bk, bkey = bank_s()
                    for j, kt in enumerate(grp):
                        mm(bk[:, j * 128:(j + 1) * 128], KT[:, h, kt * 128:(kt + 1) * 128], qnT[:, h, :], True, False,
                           (f"K{kt}", "qnT"), (bkey,))
                        mm(bk[:, j * 128:(j + 1) * 128], KPT[hp0:hp0 + 64, kt * 128:(kt + 1) * 128],
                           qpT[hp0:hp0 + 64, h // 2, :], False, True, (f"KP{kt}", "qpT"), (bkey,))
                    pi = rot("pb", 3)
                    n = len(grp) * 128
                    act(pb[pi][:, 0:n], bk[:, 0:n], AF.Exp, (bkey,), (f"pb{pi}",), scale=SCALE_ATT)
                    if grp[-1] == t:
                        j = len(grp) - 1
                        P.op("dve", (lambda pp, jj: (lambda e: e.memset(pp[64:128, jj * 128:jj * 128 + 64], 0.0)))(
                            pb[pi], j), (), (f"pb{pi}",))
                    for j, kt in enumerate(grp):
                        mm(acc[:, 0:129], pb[pi][:, j * 128:(j + 1) * 128], VC[:, kt, h, 0:129], False, kt == t,
                           (f"pb{pi}", f"V{kt}"), (akey,))
                recip(st1[:, 32 + h:33 + h], acc[:, 128:129], (akey,), (f"st1e{h}",))
                tsc(ya[:, h * 128:(h + 1) * 128], acc[:, 0:128], st1[:, 32 + h:33 + h], None, ALU.mult, None,
                    (akey, f"st1e{h}"), ("ya",))

        if stop == "D" and not is_meta:
            dump(s, t, [(ya[:], "ya", 1024)])
            return
        tri = TRIM if is_meta else TRI
        sut = SUTM if is_meta else SUT
        bk, bkey = bank_s()
        mm(bk[:, :], gaT[:, :], wa2[:, :], True, True, ("gaT", "wa2"), (bkey,))
        act(e1[:], bk[:, :], AF.Exp, (bkey,), ("e1",), scale=-1.0)
        act(lsp[:], e1[:], AF.Ln, ("e1", "oneb"), ("lsp",), bias=oneb[:, 0:1])
        bk, bkey = bank_s()
        for h in range(4):
            mm(bk[:, h * 128:(h + 1) * 128], lsp[:, h * 128:(h + 1) * 128], tri, True, True, ("lsp", "cst"), (bkey,))
        act(Ep[:], bk[:, :], AF.Exp, (bkey,), ("Ep",))
        act(En[:], bk[:, :], AF.Exp, (bkey,), ("En",), scale=-1.0)
        bk, bkey = bank_s()
        mm(bk[:, :], sut, lsp[:], True, True, ("lsp", "cst"), (bkey,))
        act(e1[:], bk[:, :], AF.Exp, (bkey,), ("e1",))
        tt(Khat[:], gktm[:], e1[:], ALU.mult, ("gktm", "e1"), ("Khat",))
        tt(KtT[:].rearrange("p a b -> p (a b)"), gkT[:].rearrange("p a b -> p (a b)"), En[:], ALU.mult, ("gkT", "En"),
           ("KtT",))
        if not is_meta:
            stt(QtT[:].rearrange("p a b -> p (a b)"), gqT[:].rearrange("p a b -> p (a b)"), SCALE_GLA, Ep[:], ALU.mult,
                ALU.mult, ("gqT", "Ep"), ("QtT",))
            bk, bkey = bank_s()
            for h in range(4):
                mm(bk[:, h * 128:(h + 1) * 128], KtT[:, h, :], QtT[:, h, :], True, True, ("KtT", "QtT"), (bkey,))
            tt(At[:], bk[:, :], MASKUT, ALU.mult, (bkey, "cst"), ("At",))
            for h in range(4):
                acc = pf[4 + h // 2]
                akey = f"pf{4 + h // 2}"
                o = (h % 2) * 256
                mm(acc[:, o:o + 256], At[:, h * 128:(h + 1) * 128], gv[:, h * 256:(h + 1) * 256], True, False,
                   ("At", "gv"), (akey,))
                mm(acc[:, o:o + 256], QtT[:, h, :], Sb[:, h, :], False, True, ("QtT", "Sb"), (akey,))
            for h in range(4):
                acc = pf[4 + h // 2]
                akey = f"pf{4 + h // 2}"
                o = (h % 2) * 256
                act(junk[:, 0:256], acc[:, o:o + 256], AF.Square, (akey,), ("st1f",),
                    accum_out=st1[:, 40 + h:41 + h])
            rstd_from_ss(st1[:, 40:44], st1[:, 40:44], 256.0, ("st1f",), ("st1f",))
            tt(gsg[:].rearrange("p (h c) -> p h c", h=4), sg[:].rearrange("p (h c) -> p h c", h=4),
               G_GLA.unsqueeze(1).to_broadcast([128, 4, 256]), ALU.mult, ("sg", "gbt"), ("gsg",))
            for h in range(4):
                acc = pf[4 + h // 2]
                akey = f"pf{4 + h // 2}"
                o = (h % 2) * 256
                stt(yb[:, h * 256:(h + 1) * 256], acc[:, o:o + 256], st1[:, 40 + h:41 + h],
                    gsg[:, h * 256:(h + 1) * 256], ALU.mult, ALU.mult, (akey, "st1f", "gsg"), ("yb",))
        for h in range(4):
            bk, bkey = bank_s()
            mm(bk[:, 0:256], Khat[:, h * 128:(h + 1) * 128], gv[:, h * 256:(h + 1) * 256], True, True, ("Khat", "gv"),
               (bkey,))
            if is_meta:
                cp(S0[:, h, :], bk[:, 0:256], (bkey,), ("S0",))
            else:
                stt(S[:, h, :], S[:, h, :], Ep[:, h * 128 + 127:h * 128 + 128], bk[:, 0:256], ALU.mult, ALU.add,
                    ("S", "Ep", bkey), ("S",))
        if is_meta:
            return
        cp(Sb[:], S[:], ("S",), ("Sb",), eng="act")

        if stop == "E":
            dump(s, t, [(yb[:], "yb", 1024)])
            return
        transposes(yT[:].rearrange("p a b -> p (a b)"), "yT", ya, "ya", 8)
        for j in range(2):
            bk, bkey = proj_tm("oa", j * 512, (j + 1) * 512, yT, "yT")
            tt(mix[:, j * 512:(j + 1) * 512], bk[:, :], gas[:, j * 512:(j + 1) * 512], ALU.mult, (bkey, "gas"),
               ("mix",))
        transposes(yT[:].rearrange("p a b -> p (a b)"), "yT", yb, "yb", 8)
        for j in range(2):
            bk, bkey = proj_tm("ob", j * 512, (j + 1) * 512, yT, "yT")
            tt(gsg[:, j * 512:(j + 1) * 512], bk[:, :], gbs[:, j * 512:(j + 1) * 512], ALU.mult, (bkey, "gbs"),
               ("gsg",))
        tt(mixb[:], mix[:], gsg[:], ALU.add, ("mix", "gsg"), ("mixb",))
        transposes(yT[:].rearrange("p a b -> p (a b)"), "yT", mixb, "mixb", 8)
        for j in range(2):
            bk, bkey = proj_tm("out", j * 512, (j + 1) * 512, yT, "yT")
            tt(X[:, j * 512:(j + 1) * 512], bk[:, :], X[:, j * 512:(j + 1) * 512], ALU.add, (bkey, xk), (xk,))

        if stop == "F":
            dump(s, t, [(X[:], "xt0", 1024)])
            return
        act(junk[:], X[:], AF.Square, (xk,), ("st1g",), accum_out=st1[:, 48:49])
        rstd_from_ss(st1[:, 48:49], st1[:, 48:49], 1024.0, ("st1g",), ("st1g",))
        stt(n2b[:], X[:], st1[:, 48:49], G_FFN, ALU.mult, ALU.mult, (xk, "st1g", "gbt"), ("n2b",))
        transposes(yT[:].rearrange("p a b -> p (a b)"), "yT", n2b, "n2b", 8)
        for j in range(4):
            wsl, wk, _ = load_w("pq", j * 512, (j + 1) * 512)
            bk, bkey = bank_mm()
            for c in range(4):
                for kc in range(8):
                    mm(bk[:, c * 128:(c + 1) * 128], wsl[:, kc, c * 128:(c + 1) * 128], yT[:, kc, :], kc == 0, kc == 7,
                       ("yT", wk), (bkey,))
            cp(pqT[:].rearrange("p a b -> p (a b)"), bk[:, :], (bkey,), ("pqT",), eng="act")
            bk, bkey = bank_mm()
            for c in range(4):
                hp = 4 * j + c
                mm(bk[:, c * 128:(c + 1) * 128], pqT[:, c, :], keysT[:, (hp % 2) * 128:(hp % 2 + 1) * 128], True, True,
                   ("pqT", "keysT"), (bkey,))
            for c in range(4):
                hp = 4 * j + c
                sv = bk[:, c * 128:(c + 1) * 128]
                P.op("dve", (lambda hh, vv: (lambda e: e.max(out=tsv[:, hh, 0:8], in_=vv)))(hp, sv), (bkey,), ("tsv",))
                P.op("dve", (lambda hh, vv: (lambda e: e.max_index(out=tiu[:, hh, 0:8], in_max=tsv[:, hh, 0:8],
                                                                   in_values=vv)))(hp, sv), (bkey, "tsv"), ("tiu",))
                P.op("dve", (lambda hh, vv, cc: (lambda e: e.match_replace(
                    out=ss2[:, cc, :], in_to_replace=tsv[:, hh, 0:8], in_values=vv, imm_value=-1e30)))(hp, sv, c),
                    (bkey, "tsv"), ("ss2",))
                P.op("dve", (lambda hh, cc: (lambda e: e.max(out=tsv[:, hh, 8:16], in_=ss2[:, cc, :])))(hp, c),
                     ("ss2",), ("tsv",))
                P.op("dve", (lambda hh, cc: (lambda e: e.max_index(out=tiu[:, hh, 8:16], in_max=tsv[:, hh, 8:16],
                                                                   in_values=ss2[:, cc, :])))(hp, c), ("ss2", "tsv"),
                     ("tiu",))
        cp(tif[:], tiu[:], ("tiu",), ("tif",))
        ts4 = tsv[:].rearrange("p (h two) k -> p h two k", two=2)
        ti4 = tif[:].rearrange("p (h two) k -> p h two k", two=2)
        tsc(ti4[:, :, 0, :], ti4[:, :, 0, :], 128.0, None, ALU.mult, None, ("tif",), ("tif",))
        for h in range(8):
            cs = cands[:].rearrange("p (a b) -> p a b", a=16)
            ci = candi[:].rearrange("p (a b) -> p a b", a=16)
            tt(cs, ts4[:, h, 0, :].unsqueeze(2).to_broadcast([128, 16, 16]),
               ts4[:, h, 1, :].unsqueeze(1).to_broadcast([128, 16, 16]), ALU.add, ("tsv",), ("cands",))
            tt(ci, ti4[:, h, 0, :].unsqueeze(2).to_broadcast([128, 16, 16]),
               ti4[:, h, 1, :].unsqueeze(1).to_broadcast([128, 16, 16]), ALU.add, ("tif",), ("candi",))
            P.op("dve", (lambda hh: (lambda e: e.max(out=bsv[:, hh, 0:8], in_=cands[:])))(h), ("cands",), ("bsv",))
            P.op("dve", (lambda hh: (lambda e: e.match_replace(out=cands2[:], in_to_replace=bsv[:, hh, 0:8],
                                                               in_values=cands[:], imm_value=-1e30)))(h),
                 ("cands", "bsv"), ("cands2",))
            P.op("dve", (lambda hh: (lambda e: e.max(out=bsv[:, hh, 8:16], in_=cands2[:])))(h), ("cands2",), ("bsv",))
            for k in range(16):
                stt(junk2[:, 0:256], cands[:], bsv[:, h, k:k + 1], candi[:], ALU.is_equal, ALU.mult,
                    ("cands", "bsv", "candi"), ("idxf",), accum_out=idxf[:, h * 16 + k:h * 16 + k + 1])
        tsc(idxf[:], idxf[:], 16383.0, 0.0, ALU.min, ALU.max, ("idxf",), ("idxf",))
        cp(idxi[:], idxf[:], ("idxf",), ("idxi",))
        b3 = bsv[:]
        g3 = gate[:].rearrange("p (h k) -> p h k", h=8)
        tt(g3, b3, b3[:, :, 0:1].to_broadcast([128, 8, 16]), ALU.subtract, ("bsv",), ("gate",))
        act(gate[:], gate[:], AF.Exp, ("gate",), ("gate",))
        red(st1[:, 50:58], g3, ALU.add, ("gate",), ("st1h",))
        recip(st1[:, 50:58], st1[:, 50:58], ("st1h",), ("st1h",))
        tt(g3, g3, st1[:, 50:58].unsqueeze(2).to_broadcast([128, 8, 16]), ALU.mult, ("gate", "st1h"), ("gate",))
        if stop == "G":
            dump(s, t, [(idxf[:], "idxf", 128), (gate[:], "gate", 128)])
            return
        for j in range(128):
            ui = rot("uv", NUV)
            P.dma("pool", (lambda uu, jj: (lambda e: e.indirect_dma_start(
                out=uvb[uu][:, :], out_offset=None, in_=uv,
                in_offset=bass.IndirectOffsetOnAxis(ap=idxi[:, jj:jj + 1], axis=0))))(ui, j),
                ("idxi",), (f"uvb{ui}",))
            stt(junk2[:], uvb[ui][:, 0:1024], 1.0, n2b[:], ALU.mult, ALU.mult, (f"uvb{ui}", "n2b"), (f"actv{j}",),
                accum_out=actv[:, j:j + 1])
            act(glv[:, j:j + 1], actv[:, j:j + 1], AF.Gelu, (f"actv{j}",), (f"glv{j}",))
            di = rot("dg", ND)
            tsc(dgb[di][:], identb[:], glv[:, j:j + 1], gate[:, j:j + 1], ALU.mult, ALU.mult,
                ("identb", f"glv{j}", "gate"), (f"dgb{di}",))
            mm(pf[4][:, :], dgb[di][:], uvb[ui][:, 1024:1536], j == 0, j == 127, (f"dgb{di}", f"uvb{ui}"), ("pf4",))
            mm(pf[5][:, :], dgb[di][:], uvb[ui][:, 1536:2048], j == 0, j == 127, (f"dgb{di}", f"uvb{ui}"), ("pf5",))
        tt(X[:, 0:512], pf[4][:, :], X[:, 0:512], ALU.add, ("pf4", xk), (xk,))
        tt(X[:, 512:1024], pf[5][:, :], X[:, 512:1024], ALU.add, ("pf5", xk), (xk,))
        dma_sp(y[s, (t - 1) * 128:t * 128, :], X[:], (xk,), ("y",), final=True)

    oneb = sb("oneb", [128, 1])
    P.op("dve", lambda e: e.memset(oneb[:], 1.0), (), ("oneb",))

    if stop not in ("M0", "P1", "P2"):
        tile(None, 0, 0)
    par = 1
    for s in range(nseq):
        cp(S[:], S0[:], ("S0",), ("S",))
        cp(Sb[:], S0[:], ("S0",), ("Sb",), eng="act")
        for t in range(1, ntile + 1):
            if stop in ("M0", "P1", "P2"):
                dump(s, t, [])
                continue
            tile(s, t, par)
            par ^= 1
    P.flush()
    stack.close()
    return nc


def _host_consts():
    idx = np.arange(128)
    ident = np.eye(128, dtype=np.float32)
    s_ = idx[:, None]
    t_ = idx[None, :]
    tri = np.where(s_ <= t_, -1.0 / 16.0, 0.0).astype(np.float32)
    sut = np.where(s_ > t_, -1.0 / 16.0, 0.0).astype(np.float32)
    trim = np.where((s_ <= t_) & (s_ < 16), -1.0 / 16.0, 0.0).astype(np.float32)
    sutm = np.where((s_ > t_) & (s_ < 16), -1.0 / 16.0, 0.0).astype(np.float32)
    mut = np.where(s_ <= t_, 1.0, 0.0).astype(np.float32)
    cst = np.concatenate([ident, tri, sut, trim, sutm, mut, mut, mut, mut], axis=1)
    inv_freq = (10000.0 ** (-np.arange(32, dtype=np.float32) / 32.0)).astype(np.float32)
    rope = np.zeros((NKT, 128, 64), np.float32)
    for t in range(NKT):
        pos = (np.arange(128) if t == 0 else 16 + 128 * (t - 1) + np.arange(128)).astype(np.float32)
        ang = pos[:, None] * inv_freq[None, :]
        rope[t, :, 0:32] = np.cos(ang)
        rope[t, :, 32:64] = np.sin(ang)
    return np.ascontiguousarray(cst), rope


_CACHE = {}


def _run(inputs, nseq, ntile, ncores, stop=None):
    f = lambda a: np.ascontiguousarray(np.asarray(a, dtype=np.float32))
    x = f(inputs["x"])
    cst, rope = _host_consts()
    xm = np.zeros((128, 1024), np.float32)
    xm[0:16] = f(inputs["meta"])
    gcol = np.concatenate([f(inputs["norm_mix"])[0].reshape(8, 128).T, f(inputs["mla_q_norm"])[0].reshape(3, 128).T,
                           f(inputs["mla_kv_norm"])[0].reshape(2, 128).T], axis=1)
    gvec = np.concatenate([f(inputs["qn_nope"])[0], f(inputs["qn_pe"])[0], f(inputs["kn_nope"])[0],
                           f(inputs["kn_pe"])[0], f(inputs["gla_norm"])[0], f(inputs["norm_ffn"])[0]])
    gb = np.ascontiguousarray(np.broadcast_to(gvec[None, :], (128, gvec.shape[0])))
    wa2 = np.concatenate([f(inputs["gla_w_a2"])[0], f(inputs["gla_b_a"]), np.zeros((15, 512), np.float32)], axis=0)
    keys = f(inputs["peer_keys"])[0]
    keysT = np.ascontiguousarray(keys.transpose(2, 0, 1).reshape(128, 256))
    uvt = np.ascontiguousarray(np.concatenate([f(inputs["peer_u"])[0], f(inputs["peer_v"])[0]], axis=1))
    common = {
        "xm": xm, "w_in": f(inputs["w_in"])[0], "w_uq": f(inputs["mla_w_uq"])[0], "w_ukv": f(inputs["mla_w_ukv"])[0],
        "w_oa": f(inputs["w_o_mla"])[0], "w_ob": f(inputs["w_o_gla"])[0], "w_out": f(inputs["w_out"])[0],
        "w_pq": f(inputs["peer_w_q"])[0], "gcol": np.ascontiguousarray(gcol), "gb": gb, "wa2": np.ascontiguousarray(wa2),
        "keysT": keysT, "uv": (uvt if stop in (None, "H") else uvt[:128]), "rope": rope, "cst": cst,
    }
    key = (nseq, ntile, stop)
    if key not in _CACHE:
        _CACHE[key] = build(nseq, ntile, stop)
    nc = _CACHE[key]
    in_maps = []
    for c in range(ncores):
        m = dict(common)
        m["x"] = np.ascontiguousarray(x[c * nseq:(c + 1) * nseq])
        in_maps.append(m)
    res = run_bass_kernel_spmd(nc, in_maps, core_ids=list(range(ncores)))
    return np.concatenate([np.asarray(r["y"]) for r in res.results], axis=0)


def kernel(**inputs):
    out = _run(inputs, 2, 16, NCORES)
    return out.astype(np.float32)
```

```python
import numpy as np
from contextlib import ExitStack
import concourse.bass as bass
import concourse.mybir as mybir
from concourse.bass_utils import run_bass_kernel_spmd

F32 = mybir.dt.float32
BF16 = mybir.dt.bfloat16
I32 = mybir.dt.int32
U32 = mybir.dt.uint32
AF = mybir.ActivationFunctionType
ALU = mybir.AluOpType
AX = mybir.AxisListType

EPS = 1e-6
NCORES = 8
ENGS = ("pe", "dve", "act", "pool", "sp")
EPOCH = 30000


class Prog:
    def __init__(self, nc, stack):
        self.nc = nc
        self.stack = stack
        self.q = {e: [] for e in ENGS}
        self.cnt = {e: 0 for e in ENGS}
        self.known = {e: {} for e in ENGS}
        self.sems = {}
        self.st = {}
        self.ring = {"sp": [("dma", "sp", i) for i in range(12)], "pool": [("dma", "pool", i) for i in range(8)],
                     "act": [("dma", "act", i) for i in range(4)]}
        self.ring_pos = {k: 0 for k in self.ring}
        self.ring_ev = {}
        self.ring_uses = {}
        self.final = []
        self.alias = {}

    def _x(self, keys):
        out = []
        for k in keys:
            out.extend(self.alias.get(k, (k,)))
        return out

    def _sem(self, key):
        if key not in self.sems:
            name = "s_" + "_".join(str(k) for k in key)
            self.sems[key] = self.stack.enter_context(self.nc.semaphore(name))
        return self.sems[key]

    def _deps(self, r, w):
        r = self._x(r)
        w = self._x(w)
        deps = []
        for k in r:
            s = self.st.get(k)
            if s and s[0]:
                deps.append(s[0])
        for k in w:
            s = self.st.get(k)
            if s:
                if s[0]:
                    deps.append(s[0])
                deps.extend(s[1].values())
        return deps

    def _wait(self, eng, deps):
        for (sk, val) in deps:
            if eng == "pe" and sk[0] == "pe":
                continue
            if self.known[eng].get(sk, 0) >= val:
                continue
            self.known[eng][sk] = val
            self._sem(sk)
            self.q[eng].append(("wait", sk, val))

    def _record(self, ev, r, w):
        r = self._x(r)
        w = self._x(w)
        for k in w:
            self.st[k] = [ev, {}]
        for k in r:
            s = self.st.setdefault(k, [None, {}])
            s[1][ev[0]] = ev

    def op(self, eng, fn, r=(), w=()):
        self._wait(eng, self._deps(r, w))
        self.cnt[eng] += 1
        c = self.cnt[eng]
        ep = (c - 1) // EPOCH
        ev = ((eng, ep), c - ep * EPOCH)
        self._sem(ev[0])
        self.q[eng].append(("op", fn, ev[0], 1))
        self._record(ev, r, w)
        return ev

    def dma(self, eng, fn, r=(), w=(), final=False):
        ring = self.ring[eng]
        sk = ring[self.ring_pos[eng] % len(ring)]
        self.ring_pos[eng] += 1
        deps = self._deps(r, w)
        if sk in self.ring_ev:
            deps.append(self.ring_ev[sk])
        self._wait(eng, deps)
        self.ring_uses[sk] = self.ring_uses.get(sk, 0) + 1
        ev = (sk, 16 * self.ring_uses[sk])
        self.ring_ev[sk] = ev
        self._sem(sk)
        self.q[eng].append(("op", fn, sk, 16))
        self._record(ev, r, w)
        if final:
            self.final.append(ev)
        return ev

    def flush(self):
        nc = self.nc
        self._wait("sp", self.final)
        self._wait("sp", list(self.ring_ev.values()))
        print("PROG counts", self.cnt, {k: len(v) for k, v in self.q.items()}, flush=True)
        q = self.q
        sems = self.sems

        def run(name, e):
            for it in q[name]:
                if it[0] == "wait":
                    e.wait_ge(sems[it[1]], it[2])
                else:
                    ins = it[1](e)
                    ins.then_inc(sems[it[2]], it[3])

        with nc.Block() as block:
            @block.tensor
            def _(e):
                run("pe", e)

            @block.vector
            def _(e):
                run("dve", e)

            @block.scalar
            def _(e):
                run("act", e)

            @block.gpsimd
            def _(e):
                run("pool", e)

            @block.sync
            def _(e):
                run("sp", e)


C_CQ, C_CKV, C_KPE, C_GQ, C_GK, C_GV, C_GA, C_GG, C_GTA, C_GTB, C_END = (
    0, 384, 640, 704, 1216, 1728, 2752, 2768, 3792, 4816, 5840)
NKT = 17


def build(nseq, ntile, stop=None):
    nc = bass.Bass("TRN2", target_bir_lowering=False)
    stack = ExitStack()
    P = Prog(nc, stack)

    def din(name, shape, dt=F32):
        return nc.dram_tensor(name, list(shape), dt, kind="ExternalInput").ap()

    x = din("x", [nseq, 2048, 1024])
    xm = din("xm", [128, 1024])
    Wd = {
        "in": (din("w_in", [1024, 5840]), 8, 5840, 0),
        "uq": (din("w_uq", [384, 1536]), 3, 1536, 8),
        "ukv": (din("w_ukv", [256, 2048]), 2, 2048, 11),
        "oa": (din("w_oa", [1024, 1024]), 8, 1024, None),
        "ob": (din("w_ob", [1024, 1024]), 8, 1024, None),
        "out": (din("w_out", [1024, 1024]), 8, 1024, None),
        "pq": (din("w_pq", [1024, 2048]), 8, 2048, None),
    }
    gcol_d = din("gcol", [128, 13])
    gb_d = din("gb", [128, 1664])
    wa2_d = din("wa2", [32, 512])
    keysT_d = din("keysT", [128, 256])
    NEXP = 16384 if stop in (None, "H") else 128
    uv = din("uv", [NEXP, 2048])
    rope_d = din("rope", [NKT, 128, 64])
    cst_d = din("cst", [128, 1152])
    y = nc.dram_tensor("y", [nseq, 2048, 1024], F32, kind="ExternalOutput").ap()
    uvs = nc.dram_tensor("uvs", [NEXP, 2048], BF16, kind="Internal").ap()
    ws = {k: nc.dram_tensor("ws_" + k, [128, v[1], v[2]], BF16, kind="Internal").ap() for k, v in Wd.items()}

    def sb(name, shape, dt=F32):
        return stack.enter_context(nc.sbuf_tensor("sb_" + name, list(shape), dt))

    def ps(name, shape, dt=F32):
        return stack.enter_context(nc.psum_tensor("ps_" + name, list(shape), dt))

    cst = sb("cst", [128, 1152])
    IDENT = cst[:, 0:128]
    TRI = cst[:, 128:256]
    SUT = cst[:, 256:384]
    TRIM = cst[:, 384:512]
    SUTM = cst[:, 512:640]
    MASKUT = cst[:, 640:1152]
    identb = sb("identb", [128, 128], BF16)
    gcol = sb("gcol", [128, 13])
    gbt = sb("gbt", [128, 1664])
    G_QN, G_QP, G_KN, G_KP, G_GLA, G_FFN = (gbt[:, 0:128], gbt[:, 128:192], gbt[:, 192:320], gbt[:, 320:384],
                                          gbt[:, 384:640], gbt[:, 640:1664])
    wa2 = sb("wa2", [32, 512])
    keysT = sb("keysT", [128, 256])
    ropet = sb("ropet", [128, NKT, 64])

    KT = sb("KT", [128, 8, NKT * 128], BF16)
    KPT = sb("KPT", [128, NKT * 128], BF16)
    VC = sb("VC", [128, NKT, 8, 132], BF16)
    S = sb("S", [128, 4, 256])
    S0 = sb("S0", [128, 4, 256])
    Sb = sb("Sb", [128, 4, 256], BF16)

    NWS = 2
    wslot = [sb(f"wslot{i}", [128, 8, 512], BF16) for i in range(NWS)]
    NUV = 3
    ND = 4

    AW = 7168
    arena = sb("arena", [128, AW])

    def carve(name, off, shape, dt=F32):
        per = 1
        for d in shape[1:]:
            per *= d
        words = per if dt in (F32, I32, U32) else per // 2
        assert off + words <= AW, name
        v = arena[:, off:off + words]
        if dt != F32:
            v = v.bitcast(dt)
        if len(shape) == 3:
            v = v.rearrange("p (a b) -> p a b", a=shape[1])
        P.alias[name] = tuple(("ar", g) for g in range(off // 128, (off + words - 1) // 128 + 1))
        return v

    lat = carve("lat", 0, [128, 704])
    qsb = carve("qsb", 704, [128, 8, 192])
    kvs = carve("kvs", 2240, [128, 8, 128])
    knn = carve("knn", 3264, [128, 8, 128], BF16)
    qnn = carve("qnn", 3776, [128, 8, 128], BF16)
    qpe = carve("qpe", 4288, [128, 8, 64])
    qpr = carve("qpr", 4800, [128, 8, 64], BF16)
    cqn = carve("cqn", 5056, [128, 384], BF16)
    ckn = carve("ckn", 5248, [128, 256], BF16)
    cqT = carve("cqT", 5376, [128, 3, 128], BF16)
    ckT = carve("ckT", 5568, [128, 2, 128], BF16)
    kp = carve("kp", 5696, [128, 64])
    kp2 = carve("kp2", 5760, [128, 128], BF16)
    rt = carve("rt", 5824, [128, 8, 32])
    rt2 = carve("rt2", 6080, [128, 8, 32])
    e1 = carve("e1", 0, [128, 512])
    lsp = carve("lsp", 512, [128, 512])
    Ep = carve("Ep", 1024, [128, 512])
    En = carve("En", 1536, [128, 512])
    QtT = carve("QtT", 2048, [128, 4, 128], BF16)
    KtT = carve("KtT", 2304, [128, 4, 128], BF16)
    Khat = carve("Khat", 2560, [128, 512], BF16)
    At = carve("At", 2816, [128, 512], BF16)
    n2b = carve("n2b", 0, [128, 1024], BF16)
    pqT = carve("pqT", 512, [128, 4, 128])
    ss2 = carve("ss2", 1024, [128, 4, 128])
    tsv = carve("tsv", 1536, [128, 16, 16])
    tiu = carve("tiu", 1792, [128, 16, 16], U32)
    tif = carve("tif", 2048, [128, 16, 16])
    cands = carve("cands", 2304, [128, 256])
    cands2 = carve("cands2", 2560, [128, 256])
    candi = carve("candi", 2816, [128, 256])
    dgb = [carve(f"dgb{i}", 3072 + 64 * i, [128, 128], BF16) for i in range(ND)]
    uvb = [carve(f"uvb{i}", 3328 + 1024 * i, [128, 2048], BF16) for i in range(NUV)]
    stg = [carve(f"stg{i}", 512 * i, [128, 512]) for i in range(2)]
    stgb = [carve(f"stgb{i}", 1024 + 256 * i, [128, 512], BF16) for i in range(2)]

    ustg = [carve(f"ustg{i}", 2048 * i, [128, 2048]) for i in range(2)]
    ustgb = [carve(f"ustgb{i}", 4096 + 1024 * i, [128, 2048], BF16) for i in range(2)]

    xt = [sb("xt0", [128, 1024])]
    junk = sb("junk", [128, 1024], BF16)
    junk2 = sb("junk2", [128, 1024], BF16)
    nb = sb("nb", [128, 1024], BF16)
    nT = sb("nT", [128, 8, 128], BF16)
    st1 = sb("st1", [128, 64])
    gqT = sb("gqT", [128, 4, 128])
    gkT = sb("gkT", [128, 4, 128])
    gktm = sb("gktm", [128, 512])
    gv = sb("gv", [128, 1024], BF16)
    gaT = sb("gaT", [32, 128])
    sg = sb("sg", [128, 1024], BF16)
    gas = sb("gas", [128, 1024], BF16)
    gbs = sb("gbs", [128, 1024], BF16)
    qnT = sb("qnT", [128, 8, 128], BF16)
    qpT = sb("qpT", [128, 4, 128], BF16)
    pb = [sb(f"pb{i}", [128, 512], BF16) for i in range(3)]
    pm = [sb(f"pm{i}", [16, 128], BF16) for i in range(2)]
    ya = sb("ya", [128, 1024], BF16)
    yb = sb("yb", [128, 1024], BF16)
    yT = sb("yT", [128, 8, 128], BF16)
    MG = sb("MG", [128, 2048])
    mix = MG[:, 0:1024]
    gsg = MG[:, 1024:2048]
    sq = MG[:, 0:1536].rearrange("p (a b) -> p a b", a=8)
    for nm in ("mix", "gsg", "sq"):
        P.alias[nm] = ("MG",)
    mixb = sb("mixb", [128, 1024], BF16)
    bsv = sb("bsv", [128, 8, 16])
    idxf = sb("idxf", [128, 128])
    idxi = sb("idxi", [128, 128], I32)
    gate = sb("gate", [128, 128])
    actv = sb("actv", [128, 128])
    glv = sb("glv", [128, 128])

    pf = [ps(f"pf{i}", [128, 512]) for i in range(6)]
    pt = [ps(f"pt{i}", [128, 1024], BF16) for i in range(2)]
    cnt = {"mm": 0, "s": 0, "t": 0, "w": 0, "pb": 0, "pm": 0, "stg": 0, "uv": 0, "dg": 0}

    def rot(kind, n):
        v = cnt[kind] % n
        cnt[kind] += 1
        return v

    def bank_mm():
        i = rot("mm", 2)
        return pf[i], f"pf{i}"

    def bank_s():
        i = 2 + rot("s", 2)
        return pf[i], f"pf{i}"

    def bank_t():
        i = rot("t", 2)
        return pt[i], f"pt{i}"

    def dma_sp(out, in_, r, w, final=False):
        P.dma("sp", lambda e: e.dma_start(out=out, in_=in_), r, w, final)

    def mm(out, lhsT, rhs, start, stop, r, w):
        P.op("pe", lambda e: e.matmul(out, lhsT, rhs, start=start, stop=stop), r, w)

    def tr(out, in_, idn, r, w):
        P.op("pe", lambda e: e.transpose(out, in_, idn), r, w)

    def act(out, in_, func, r, w, bias=None, scale=None, accum_out=None):
        kw = {}
        if bias is not None:
            kw["bias"] = bias
        if scale is not None:
            kw["scale"] = scale
        if accum_out is not None:
            kw["accum_out"] = accum_out
        P.op("act", lambda e: e.activation(out=out, in_=in_, func=func, **kw), r, w)

    def tt(out, in0, in1, op, r, w, eng="dve"):
        P.op(eng, lambda e: e.tensor_tensor(out=out, in0=in0, in1=in1, op=op), r, w)

    def tsc(out, in0, s1, s2, op0, op1, r, w, eng="dve", accum_out=None):
        if op1 is None:
            P.op(eng, lambda e: e.tensor_scalar(out=out, in0=in0, scalar1=s1, scalar2=None, op0=op0), r, w)
        else:
            P.op(eng, lambda e: e.tensor_scalar(out=out, in0=in0, scalar1=s1, scalar2=s2, op0=op0, op1=op1,
                                                accum_out=accum_out), r, w)

    def stt(out, in0, scalar, in1, op0, op1, r, w, accum_out=None):
        P.op("dve", lambda e: e.scalar_tensor_tensor(out=out, in0=in0, scalar=scalar, in1=in1, op0=op0, op1=op1,
                                                     accum_out=accum_out), r, w)

    def cp(out, in_, r, w, eng="dve"):
        if eng == "act":
            P.op("act", lambda e: e.copy(out=out, in_=in_), r, w)
        else:
            P.op(eng, lambda e: e.tensor_copy(out=out, in_=in_), r, w)

    def recip(out, in_, r, w):
        P.op("dve", lambda e: e.reciprocal(out=out, in_=in_), r, w)

    def red(out, in_, op, r, w):
        P.op("dve", lambda e: e.tensor_reduce(out=out, in_=in_, axis=AX.X, op=op), r, w)

    def rstd_from_ss(dst, ssum, n, r, w):
        act(dst, ssum, AF.Sqrt, tuple(r) + ("epsb",), w, bias=epsb[:, 0:1], scale=1.0 / n)
        recip(dst, dst, w, w)

    epsb = sb("epsb", [128, 1])
    P.op("dve", lambda e: e.memset(epsb[:], EPS), (), ("epsb",))

    import os
    SK = os.environ.get("DBGSKIP", "").split(",")
    if "cst" not in SK:
        dma_sp(cst[:], cst_d, (), ("cst",))
    if "gcol" not in SK:
        dma_sp(gcol[:], gcol_d, (), ("gcol",))
    if "gbt" not in SK:
        dma_sp(gbt[:], gb_d, (), ("gbt",))
    if "wa2" not in SK:
        dma_sp(wa2[:], wa2_d, (), ("wa2",))
    if "keysT" not in SK:
        dma_sp(keysT[:], keysT_d, (), ("keysT",))
    if "rope" not in SK:
        dma_sp(ropet[:], rope_d.rearrange("t p c -> p t c"), (), ("ropet",))
    if "ms" not in SK:
        cp(identb[:], IDENT, ("cst",), ("identb",))
        P.op("dve", lambda e: e.memset(gaT[:], 1.0), (), ("gaT",))
        P.op("dve", lambda e: e.memset(VC[:], 1.0), (), tuple(f"V{t}" for t in range(NKT)))
        P.op("dve", lambda e: e.memset(S0[:], 0.0), (), ("S0",))

    for name, (wd, kc_n, ncol, gc) in Wd.items():
        if stop == "P1" or (stop == "P2" and name != "ukv"):
            continue
        for kc in range(kc_n):
            for c0 in range(0, ncol, 512):
                c1 = min(ncol, c0 + 512)
                i = rot("stg", 2)
                dma_sp(stg[i][:, 0:c1 - c0], wd[kc * 128:(kc + 1) * 128, c0:c1], (), (f"stg{i}",))
                if gc is None:
                    cp(stgb[i][:, 0:c1 - c0], stg[i][:, 0:c1 - c0], (f"stg{i}",), (f"stgb{i}",),
                       eng=("act" if (cnt["stg"] % 4 < 2) else "dve"))
                else:
                    tsc(stgb[i][:, 0:c1 - c0], stg[i][:, 0:c1 - c0], gcol[:, gc + kc:gc + kc + 1], None, ALU.mult,
                        None, (f"stg{i}", "gcol"), (f"stgb{i}",))
                dma_sp(ws[name][:, kc, c0:c1], stgb[i][:, 0:c1 - c0], (f"stgb{i}",), (f"ws_{name}",))

    if stop in (None, "H"):
        for i in range(NEXP // 128):
            k = i % 2
            dma_sp(ustg[k][:], uv[i * 128:(i + 1) * 128, :], (), (f"ustg{k}",))
            cp(ustgb[k][:], ustg[k][:], (f"ustg{k}",), (f"ustgb{k}",), eng=("act", "dve", "pool")[i % 3])
            dma_sp(uvs[i * 128:(i + 1) * 128, :], ustgb[k][:], (f"ustgb{k}",), ("uvs",))

    def load_w(name, c0, c1):
        kc_n = Wd[name][1]
        i = rot("w", NWS)
        dma_sp(wslot[i][:, 0:kc_n, 0:c1 - c0], ws[name][:, :, c0:c1], (f"ws_{name}",), (f"wslot{i}",))
        return wslot[i], f"wslot{i}", kc_n

    def proj_tm(name, c0, c1, lhs, lhs_key):
        wsl, wk, kc_n = load_w(name, c0, c1)
        bk, bkey = bank_mm()
        for kc in range(kc_n):
            mm(bk[:, 0:c1 - c0], lhs[:, kc, :], wsl[:, kc, 0:c1 - c0], kc == 0, kc == kc_n - 1, (lhs_key, wk), (bkey,))
        return bk, bkey

    def transposes(dst, dst_key, src, src_key, nblk, eng="dve"):
        bk, bkey = bank_t()
        for j in range(nblk):
            tr(bk[:, j * 128:(j + 1) * 128], src[:, j * 128:(j + 1) * 128], identb[:], (src_key, "identb"), (bkey,))
        cp(dst, bk[:, 0:nblk * 128], (bkey,), (dst_key,), eng=eng)

    SCALE_ATT = 192.0 ** -0.5
    SCALE_GLA = 128.0 ** -0.5

    def rope_apply(dst, src, t, nh, r, w):
        cos = ropet[:, t, 0:32].unsqueeze(1).to_broadcast([128, nh, 32])
        sin = ropet[:, t, 32:64].unsqueeze(1).to_broadcast([128, nh, 32])
        x1 = src[:, :, 0:32]
        x2 = src[:, :, 32:64]
        a = rt[:, 0:nh, :]
        b = rt2[:, 0:nh, :]
        tt(a, x1, cos, ALU.mult, r + ("ropet",), ("rt",))
        tt(b, x2, sin, ALU.mult, r + ("ropet",), ("rt2",))
        tt(dst[:, :, 0:32], a, b, ALU.subtract, ("rt", "rt2"), w)
        tt(a, x2, cos, ALU.mult, r + ("ropet",), ("rt",))
        tt(b, x1, sin, ALU.mult, r + ("ropet",), ("rt2",))
        tt(dst[:, :, 32:64], a, b, ALU.add, ("rt", "rt2"), w)

    def dump(s, t, items):
        X = xt[0]
        o = 0
        for ap, key, n in items:
            cp(X[:ap.shape[0], o:o + n], ap, (key,), ("xt0",))
            o += n
        dma_sp(y[s, (t - 1) * 128:t * 128, :], X[:], ("xt0",), ("y",), final=True)

    def tile(s, t, par):
        is_meta = s is None
        X = xt[0]
        xk = "xt0"
        if is_meta:
            dma_sp(X[:], xm, (), (xk,))
        else:
            dma_sp(X[:], x[s, (t - 1) * 128:t * 128, :], (), (xk,))
        act(junk[:], X[:], AF.Square, (xk,), ("st1a",), accum_out=st1[:, 0:1])
        rstd_from_ss(st1[:, 0:1], st1[:, 0:1], 1024.0, ("st1a",), ("st1a",))
        tsc(nb[:], X[:], st1[:, 0:1], None, ALU.mult, None, (xk, "st1a"), ("nb",))
        transposes(nT[:].rearrange("p a b -> p (a b)"), "nT", nb, "nb", 8)

        if stop == "A":
            if not is_meta:
                dump(s, t, [(nb[:], "nb", 1024)])
            return
        bk, bkey = proj_tm("in", 0, 512, nT, "nT")
        cp(lat[:, 0:512], bk[:, 0:512], (bkey,), ("lat",), eng="act")
        bk, bkey = proj_tm("in", 512, 704, nT, "nT")
        cp(lat[:, 512:704], bk[:, 0:192], (bkey,), ("lat",), eng="act")
        wsl, wk, _ = load_w("in", C_GQ, C_GQ + 512)
        bk, bkey = bank_mm()
        for h in range(4):
            for kc in range(8):
                mm(bk[:, h * 128:(h + 1) * 128], wsl[:, kc, h * 128:(h + 1) * 128], nT[:, kc, :], kc == 0, kc == 7,
                   ("nT", wk), (bkey,))
        cp(gqT[:].rearrange("p a b -> p (a b)"), bk[:, :], (bkey,), ("gqT",), eng="act")
        wsl, wk, _ = load_w("in", C_GK, C_GK + 512)
        bk, bkey = bank_mm()
        for h in range(4):
            for kc in range(8):
                mm(bk[:, h * 128:(h + 1) * 128], wsl[:, kc, h * 128:(h + 1) * 128], nT[:, kc, :], kc == 0, kc == 7,
                   ("nT", wk), (bkey,))
        cp(gkT[:].rearrange("p a b -> p (a b)"), bk[:, :], (bkey,), ("gkT",), eng="act")
        bk, bkey = bank_mm()
        for kc in range(8):
            mm(bk[:, :], nT[:, kc, :], wsl[:, kc, :], kc == 0, kc == 7, ("nT", wk), (bkey,))
        cp(gktm[:], bk[:, :], (bkey,), ("gktm",), eng="dve")
        for j in range(2):
            bk, bkey = proj_tm("in", C_GV + j * 512, C_GV + (j + 1) * 512, nT, "nT")
            cp(gv[:, j * 512:(j + 1) * 512], bk[:, :], (bkey,), ("gv",), eng=("act" if j == 0 else "dve"))
        wsl, wk, _ = load_w("in", C_GA, C_GA + 16)
        bk, bkey = bank_mm()
        for kc in range(8):
            mm(bk[0:16, 0:128], wsl[:, kc, 0:16], nT[:, kc, :], kc == 0, kc == 7, ("nT", wk), (bkey,))
        cp(gaT[0:16, :], bk[0:16, 0:128], (bkey,), ("gaT",), eng="dve")
        if not is_meta:
            for j in range(2):
                bk, bkey = proj_tm("in", C_GG + j * 512, C_GG + (j + 1) * 512, nT, "nT")
                act(sg[:, j * 512:(j + 1) * 512], bk[:, :], AF.Silu, (bkey,), ("sg",))
            for j in range(2):
                bk, bkey = proj_tm("in", C_GTA + j * 512, C_GTA + (j + 1) * 512, nT, "nT")
                act(gas[:, j * 512:(j + 1) * 512], bk[:, :], AF.Sigmoid, (bkey,), ("gas",))
            for j in range(2):
                bk, bkey = proj_tm("in", C_GTB + j * 512, C_GTB + (j + 1) * 512, nT, "nT")
                act(gbs[:, j * 512:(j + 1) * 512], bk[:, :], AF.Sigmoid, (bkey,), ("gbs",))

        if stop == "B":
            if not is_meta:
                dump(s, t, [(lat[:, :], "lat", 704), (gv[:, 0:320], "gv", 320)])
            return
        tt(sq[:].rearrange("p a b -> p (a b)")[:, 0:704], lat[:, :], lat[:, :], ALU.mult, ("lat",), ("sq",))
        sqf = sq[:].rearrange("p a b -> p (a b)")
        red(st1[:, 1:2], sqf[:, 0:384], ALU.add, ("sq",), ("st1b",))
        red(st1[:, 2:3], sqf[:, 384:640], ALU.add, ("sq",), ("st1b",))
        red(st1[:, 3:4], sqf[:, 640:704], ALU.add, ("sq",), ("st1b",))
        rstd_from_ss(st1[:, 1:2], st1[:, 1:2], 384.0, ("st1b",), ("st1b",))
        rstd_from_ss(st1[:, 2:3], st1[:, 2:3], 256.0, ("st1b",), ("st1b",))
        rstd_from_ss(st1[:, 3:4], st1[:, 3:4], 64.0, ("st1b",), ("st1b",))
        tsc(ckn[:], lat[:, 384:640], st1[:, 2:3], None, ALU.mult, None, ("lat", "st1b"), ("ckn",))
        transposes(ckT[:].rearrange("p a b -> p (a b)"), "ckT", ckn, "ckn", 2)
        stt(kp[:], lat[:, 640:704], st1[:, 3:4], G_KP, ALU.mult, ALU.mult, ("lat", "st1b", "gbt"), ("kp",))
        kpv = kp[:].unsqueeze(1)
        kp2v = kp2[:].rearrange("p (a b) -> p a b", a=2)
        rope_apply(kp2v[:, 0:1, :], kpv, t, 1, ("kp",), ("kp2",))
        cp(kp2[:, 64:128], kp2[:, 0:64], ("kp2",), ("kp2",))
        bk, bkey = bank_t()
        tr(bk[:, 0:128], kp2[:], identb[:], ("kp2", "identb"), (bkey,))
        cp(KPT[:, t * 128:(t + 1) * 128], bk[:, 0:128], (bkey,), (f"KP{t}",))
        if stop == "C1":
            if not is_meta:
                dump(s, t, [])
            return
        for j in range(4):
            bk, bkey = proj_tm("ukv", j * 512, (j + 1) * 512, ckT, "ckT")
            bv = bk[:, :].rearrange("p (h c) -> p h c", h=2)
            ee = "act" if j % 2 == 0 else "dve"
            cp(kvs[:, 2 * j:2 * j + 2, :], bv[:, :, 0:128], (bkey,), ("kvs",), eng=ee)
            cp(VC[:, t, 2 * j:2 * j + 2, 0:128], bv[:, :, 128:256], (bkey,), (f"V{t}",), eng=ee)
        if stop == "C1a":
            if not is_meta:
                dump(s, t, [])
            return
        tt(sq[:, :, 0:128], kvs[:], kvs[:], ALU.mult, ("kvs",), ("sq",))
        red(st1[:, 8:16], sq[:, :, 0:128], ALU.add, ("sq",), ("st1c",))
        rstd_from_ss(st1[:, 8:16], st1[:, 8:16], 128.0, ("st1c",), ("st1c",))
        if stop == "C1b":
            if not is_meta:
                dump(s, t, [])
            return
        tt(kvs[:], kvs[:], st1[:, 8:16].unsqueeze(2).to_broadcast([128, 8, 128]), ALU.mult, ("kvs", "st1c"), ("kvs",))
        tt(knn[:], kvs[:], G_KN.unsqueeze(1).to_broadcast([128, 8, 128]), ALU.mult, ("kvs", "gbt"), ("knn",))
        if stop == "C1c":
            if not is_meta:
                dump(s, t, [])
            return
        bk, bkey = bank_t()
        for h in range(8):
            tr(bk[:, h * 128:(h + 1) * 128], knn[:, h, :], identb[:], ("knn", "identb"), (bkey,))
        cp(KT[:, :, t * 128:(t + 1) * 128], bk[:, :].rearrange("p (h c) -> p h c", h=8), (bkey,), (f"K{t}",), eng="act")

        if stop == "C2":
            if not is_meta:
                dump(s, t, [])
            return
        if not is_meta:
            tsc(cqn[:], lat[:, 0:384], st1[:, 1:2], None, ALU.mult, None, ("lat", "st1b"), ("cqn",))
            transposes(cqT[:].rearrange("p a b -> p (a b)"), "cqT", cqn, "cqn", 3)
            qf = qsb[:].rearrange("p a b -> p (a b)")
            for j in range(3):
                bk, bkey = proj_tm("uq", j * 512, (j + 1) * 512, cqT, "cqT")
                cp(qf[:, j * 512:(j + 1) * 512], bk[:, :], (bkey,), ("qsb",), eng=("act" if j != 1 else "dve"))
            tt(sq[:], qsb[:], qsb[:], ALU.mult, ("qsb",), ("sq",))
            red(st1[:, 16:24], sq[:, :, 0:128], ALU.add, ("sq",), ("st1d",))
            red(st1[:, 24:32], sq[:, :, 128:192], ALU.add, ("sq",), ("st1d",))
            rstd_from_ss(st1[:, 16:24], st1[:, 16:24], 128.0, ("st1d",), ("st1d",))
            rstd_from_ss(st1[:, 24:32], st1[:, 24:32], 64.0, ("st1d",), ("st1d",))
            tt(qsb[:, :, 0:128], qsb[:, :, 0:128], st1[:, 16:24].unsqueeze(2).to_broadcast([128, 8, 128]), ALU.mult,
               ("qsb", "st1d"), ("qsb",))
            tt(qnn[:], qsb[:, :, 0:128], G_QN.unsqueeze(1).to_broadcast([128, 8, 128]), ALU.mult, ("qsb", "gbt"),
               ("qnn",))
            tt(qsb[:, :, 128:192], qsb[:, :, 128:192], st1[:, 24:32].unsqueeze(2).to_broadcast([128, 8, 64]), ALU.mult,
               ("qsb", "st1d"), ("qsb",))
            tt(qpe[:], qsb[:, :, 128:192], G_QP.unsqueeze(1).to_broadcast([128, 8, 64]), ALU.mult, ("qsb", "gbt"),
               ("qpe",))
            rope_apply(qpr[:], qpe[:], t, 8, ("qpe",), ("qpr",))
            transposes(qnT[:].rearrange("p a b -> p (a b)"), "qnT", qnn[:].rearrange("p a b -> p (a b)"), "qnn", 8,
                       eng="act")
            transposes(qpT[:].rearrange("p a b -> p (a b)"), "qpT", qpr[:].rearrange("p a b -> p (a b)"), "qpr", 4,
                       eng="act")

            if stop == "C":
                dump(s, t, [(qnn[:].rearrange("p a b -> p (a b)")[:, 0:512], "qnn", 512),
                            (qpr[:].rearrange("p a b -> p (a b)"), "qpr", 512)])
                return
            kts = list(range(1, t + 1))
            for h in range(8):
                hp0 = (h % 2) * 64
                acc = pf[4 + (h % 2)]
                akey = f"pf{4 + (h % 2)}"
                bk, bkey = bank_s()
                mm(bk[0:16, 0:128], KT[:, h, 0:16], qnT[:, h, :], True, False, ("K0", "qnT"), (bkey,))
                mm(bk[0:16, 0:128], KPT[hp0:hp0 + 64, 0:16], qpT[hp0:hp0 + 64, h // 2, :], False, True,
                   ("KP0", "qpT"), (bkey,))
                mi = rot("pm", 2)
                act(pm[mi][:], bk[0:16, 0:128], AF.Exp, (bkey,), (f"pm{mi}",), scale=SCALE_ATT)
                mm(acc[:, 0:129], pm[mi][:], VC[0:16, 0, h, 0:129], True, False, (f"pm{mi}", "V0"), (akey,))
                for g0 in range(0, len(kts), 4):
                    grp = kts[g0:g0 + 4]
                    bk, bkey = bank_s()
                    for j, kt in enumerate(grp):
                        mm(bk[:, j * 128:(j + 1) * 128], KT[:, h, kt * 128:(kt + 1) * 128], qnT[:, h, :], True, False,
                           (f"K{kt}", "qnT"), (bkey,))
                        mm(bk[:, j * 128:(j + 1) * 128], KPT[hp0:hp0 + 64, kt * 128:(kt + 1) * 128],
                           qpT[hp0:hp0 + 64, h // 2, :], False, True, (f"KP{kt}", "qpT"), (bkey,))
                    pi = rot("pb", 3)
                    n = len(grp) * 128
                    act(pb[pi][:, 0:n], bk[:, 0:n], AF.Exp, (bkey,), (f"pb{pi}",), scale=SCALE_ATT)
                    if grp[-1] == t:
                        j = len(grp) - 1
                        P.op("dve", (lambda pp, jj: (lambda e: e.memset(pp[64:128, jj * 128:jj * 128 + 64], 0.0)))(
                            pb[pi], j), (), (f"pb{pi}",))
                    for j, kt in enumerate(grp):
                        mm(acc[:, 0:129], pb[pi][:, j * 128:(j + 1) * 128], VC[:, kt, h, 0:129], False, kt == t,
                           (f"pb{pi}", f"V{kt}"), (akey,))
                recip(st1[:, 32 + h:33 + h], acc[:, 128:129], (akey,), (f"st1e{h}",))
                tsc(ya[:, h * 128:(h + 1) * 128], acc[:, 0:128], st1[:, 32 + h:33 + h], None, ALU.mult, None,
                    (akey, f"st1e{h}"), ("ya",))

        if stop == "D" and not is_meta:
            dump(s, t, [(ya[:], "ya", 1024)])
            return
        tri = TRIM if is_meta else TRI
        sut = SUTM if is_meta else SUT
        bk, bkey = bank_s()
        mm(bk[:, :], gaT[:, :], wa2[:, :], True, True, ("gaT", "wa2"), (bkey,))
        act(e1[:], bk[:, :], AF.Exp, (bkey,), ("e1",), scale=-1.0)
        act(lsp[:], e1[:], AF.Ln, ("e1", "oneb"), ("lsp",), bias=oneb[:, 0:1])
        bk, bkey = bank_s()
        for h in range(4):
            mm(bk[:, h * 128:(h + 1) * 128], lsp[:, h * 128:(h + 1) * 128], tri, True, True, ("lsp", "cst"), (bkey,))
        act(Ep[:], bk[:, :], AF.Exp, (bkey,), ("Ep",))
        act(En[:], bk[:, :], AF.Exp, (bkey,), ("En",), scale=-1.0)
        bk, bkey = bank_s()
        mm(bk[:, :], sut, lsp[:], True, True, ("lsp", "cst"), (bkey,))
        act(e1[:], bk[:, :], AF.Exp, (bkey,), ("e1",))
        tt(Khat[:], gktm[:], e1[:], ALU.mult, ("gktm", "e1"), ("Khat",))
        tt(KtT[:].rearrange("p a b -> p (a b)"), gkT[:].rearrange("p a b -> p (a b)"), En[:], ALU.mult, ("gkT", "En"),
           ("KtT",))
        if not is_meta:
            stt(QtT[:].rearrange("p a b -> p (a b)"), gqT[:].rearrange("p a b -> p (a b)"), SCALE_GLA, Ep[:], ALU.mult,
                ALU.mult, ("gqT", "Ep"), ("QtT",))
            bk, bkey = bank_s()
            for h in range(4):
                mm(bk[:, h * 128:(h + 1) * 128], KtT[:, h, :], QtT[:, h, :], True, True, ("KtT", "QtT"), (bkey,))
            tt(At[:], bk[:, :], MASKUT, ALU.mult, (bkey, "cst"), ("At",))
            for h in range(4):
                acc = pf[4 + h // 2]
                akey = f"pf{4 + h // 2}"
                o = (h % 2) * 256
                mm(acc[:, o:o + 256], At[:, h * 128:(h + 1) * 128], gv[:, h * 256:(h + 1) * 256], True, False,
                   ("At", "gv"), (akey,))
                mm(acc[:, o:o + 256], QtT[:, h, :], Sb[:, h, :], False, True, ("QtT", "Sb"), (akey,))
            for h in range(4):
                acc = pf[4 + h // 2]
                akey = f"pf{4 + h // 2}"
                o = (h % 2) * 256
                act(junk[:, 0:256], acc[:, o:o + 256], AF.Square, (akey,), ("st1f",),
                    accum_out=st1[:, 40 + h:41 + h])
            rstd_from_ss(st1[:, 40:44], st1[:, 40:44], 256.0, ("st1f",), ("st1f",))
            tt(gsg[:].rearrange("p (h c) -> p h c", h=4), sg[:].rearrange("p (h c) -> p h c", h=4),
               G_GLA.unsqueeze(1).to_broadcast([128, 4, 256]), ALU.mult, ("sg", "gbt"), ("gsg",))
            for h in range(4):
                acc = pf[4 + h // 2]
                akey = f"pf{4 + h // 2}"
                o = (h % 2) * 256
                stt(yb[:, h * 256:(h + 1) * 256], acc[:, o:o + 256], st1[:, 40 + h:41 + h],
                    gsg[:, h * 256:(h + 1) * 256], ALU.mult, ALU.mult, (akey, "st1f", "gsg"), ("yb",))
        for h in range(4):
            bk, bkey = bank_s()
            mm(bk[:, 0:256], Khat[:, h * 128:(h + 1) * 128], gv[:, h * 256:(h + 1) * 256], True, True, ("Khat", "gv"),
               (bkey,))
            if is_meta:
                cp(S0[:, h, :], bk[:, 0:256], (bkey,), ("S0",))
            else:
                stt(S[:, h, :], S[:, h, :], Ep[:, h * 128 + 127:h * 128 + 128], bk[:, 0:256], ALU.mult, ALU.add,
                    ("S", "Ep", bkey), ("S",))
        if is_meta:
            return
        cp(Sb[:], S[:], ("S",), ("Sb",), eng="act")

        if stop == "E":
            dump(s, t, [(yb[:], "yb", 1024)])
            return
        transposes(yT[:].rearrange("p a b -> p (a b)"), "yT", ya, "ya", 8)
        for j in range(2):
            bk, bkey = proj_tm("oa", j * 512, (j + 1) * 512, yT, "yT")
            tt(mix[:, j * 512:(j + 1) * 512], bk[:, :], gas[:, j * 512:(j + 1) * 512], ALU.mult, (bkey, "gas"),
               ("mix",))
        transposes(yT[:].rearrange("p a b -> p (a b)"), "yT", yb, "yb", 8)
        for j in range(2):
            bk, bkey = proj_tm("ob", j * 512, (j + 1) * 512, yT, "yT")
            tt(gsg[:, j * 512:(j + 1) * 512], bk[:, :], gbs[:, j * 512:(j + 1) * 512], ALU.mult, (bkey, "gbs"),
               ("gsg",))
        tt(mixb[:], mix[:], gsg[:], ALU.add, ("mix", "gsg"), ("mixb",))
        transposes(yT[:].rearrange("p a b -> p (a b)"), "yT", mixb, "mixb", 8)
        for j in range(2):
            bk, bkey = proj_tm("out", j * 512, (j + 1) * 512, yT, "yT")
            tt(X[:, j * 512:(j + 1) * 512], bk[:, :], X[:, j * 512:(j + 1) * 512], ALU.add, (bkey, xk), (xk,))

        if stop == "F":
            dump(s, t, [(X[:], "xt0", 1024)])
            return
        act(junk[:], X[:], AF.Square, (xk,), ("st1g",), accum_out=st1[:, 48:49])
        rstd_from_ss(st1[:, 48:49], st1[:, 48:49], 1024.0, ("st1g",), ("st1g",))
        stt(n2b[:], X[:], st1[:, 48:49], G_FFN, ALU.mult, ALU.mult, (xk, "st1g", "gbt"), ("n2b",))
        transposes(yT[:].rearrange("p a b -> p (a b)"), "yT", n2b, "n2b", 8)
        for j in range(4):
            wsl, wk, _ = load_w("pq", j * 512, (j + 1) * 512)
            bk, bkey = bank_mm()
            for c in range(4):
                for kc in range(8):
                    mm(bk[:, c * 128:(c + 1) * 128], wsl[:, kc, c * 128:(c + 1) * 128], yT[:, kc, :], kc == 0, kc == 7,
                       ("yT", wk), (bkey,))
            cp(pqT[:].rearrange("p a b -> p (a b)"), bk[:, :], (bkey,), ("pqT",), eng="act")
            bk, bkey = bank_mm()
            for c in range(4):
                hp = 4 * j + c
                mm(bk[:, c * 128:(c + 1) * 128], pqT[:, c, :], keysT[:, (hp % 2) * 128:(hp % 2 + 1) * 128], True, True,
                   ("pqT", "keysT"), (bkey,))
            for c in range(4):
                hp = 4 * j + c
                sv = bk[:, c * 128:(c + 1) * 128]
                P.op("dve", (lambda hh, vv: (lambda e: e.max(out=tsv[:, hh, 0:8], in_=vv)))(hp, sv), (bkey,), ("tsv",))
                P.op("dve", (lambda hh, vv: (lambda e: e.max_index(out=tiu[:, hh, 0:8], in_max=tsv[:, hh, 0:8],
                                                                   in_values=vv)))(hp, sv), (bkey, "tsv"), ("tiu",))
                P.op("dve", (lambda hh, vv, cc: (lambda e: e.match_replace(
                    out=ss2[:, cc, :], in_to_replace=tsv[:, hh, 0:8], in_values=vv, imm_value=-1e30)))(hp, sv, c),
                    (bkey, "tsv"), ("ss2",))
                P.op("dve", (lambda hh, cc: (lambda e: e.max(out=tsv[:, hh, 8:16], in_=ss2[:, cc, :])))(hp, c),
                     ("ss2",), ("tsv",))
                P.op("dve", (lambda hh, cc: (lambda e: e.max_index(out=tiu[:, hh, 8:16], in_max=tsv[:, hh, 8:16],
                                                                   in_values=ss2[:, cc, :])))(hp, c), ("ss2", "tsv"),
                     ("tiu",))
        cp(tif[:], tiu[:], ("tiu",), ("tif",))
        ts4 = tsv[:].rearrange("p (h two) k -> p h two k", two=2)
        ti4 = tif[:].rearrange("p (h two) k -> p h two k", two=2)
        tsc(ti4[:, :, 0, :], ti4[:, :, 0, :], 128.0, None, ALU.mult, None, ("tif",), ("tif",))
        for h in range(8):
            cs = cands[:].rearrange("p (a b) -> p a b", a=16)
            ci = candi[:].rearrange("p (a b) -> p a b", a=16)
            tt(cs, ts4[:, h, 0, :].unsqueeze(2).to_broadcast([128, 16, 16]),
               ts4[:, h, 1, :].unsqueeze(1).to_broadcast([128, 16, 16]), ALU.add, ("tsv",), ("cands",))
            tt(ci, ti4[:, h, 0, :].unsqueeze(2).to_broadcast([128, 16, 16]),
               ti4[:, h, 1, :].unsqueeze(1).to_broadcast([128, 16, 16]), ALU.add, ("tif",), ("candi",))
            P.op("dve", (lambda hh: (lambda e: e.max(out=bsv[:, hh, 0:8], in_=cands[:])))(h), ("cands",), ("bsv",))
            P.op("dve", (lambda hh: (lambda e: e.match_replace(out=cands2[:], in_to_replace=bsv[:, hh, 0:8],
                                                               in_values=cands[:], imm_value=-1e30)))(h),
                 ("cands", "bsv"), ("cands2",))
            P.op("dve", (lambda hh: (lambda e: e.max(out=bsv[:, hh, 8:16], in_=cands2[:])))(h), ("cands2",), ("bsv",))
            for k in range(16):
                stt(junk2[:, 0:256], cands[:], bsv[:, h, k:k + 1], candi[:], ALU.is_equal, ALU.mult,
                    ("cands", "bsv", "candi"), ("idxf",), accum_out=idxf[:, h * 16 + k:h * 16 + k + 1])
        tsc(idxf[:], idxf[:], 16383.0, 0.0, ALU.min, ALU.max, ("idxf",), ("idxf",))
        cp(idxi[:], idxf[:], ("idxf",), ("idxi",))
        b3 = bsv[:]
        g3 = gate[:].rearrange("p (h k) -> p h k", h=8)
        tt(g3, b3, b3[:, :, 0:1].to_broadcast([128, 8, 16]), ALU.subtract, ("bsv",), ("gate",))
        act(gate[:], gate[:], AF.Exp, ("gate",), ("gate",))
        red(st1[:, 50:58], g3, ALU.add, ("gate",), ("st1h",))
        recip(st1[:, 50:58], st1[:, 50:58], ("st1h",), ("st1h",))
        tt(g3, g3, st1[:, 50:58].unsqueeze(2).to_broadcast([128, 8, 16]), ALU.mult, ("gate", "st1h"), ("gate",))
        if stop == "G":
            dump(s, t, [(idxf[:], "idxf", 128), (gate[:], "gate", 128)])
            return
        for j in range(128):
            ui = rot("uv", NUV)
            P.dma("pool", (lambda uu, jj: (lambda e: e.indirect_dma_start(
                out=uvb[uu][:, :], out_offset=None, in_=uvs,
                in_offset=bass.IndirectOffsetOnAxis(ap=idxi[:, jj:jj + 1], axis=0))))(ui, j),
                ("idxi", "uvs"), (f"uvb{ui}",))
            stt(junk2[:], uvb[ui][:, 0:1024], 1.0, n2b[:], ALU.mult, ALU.mult, (f"uvb{ui}", "n2b"), (f"actv{j}",),
                accum_out=actv[:, j:j + 1])
            act(glv[:, j:j + 1], actv[:, j:j + 1], AF.Gelu, (f"actv{j}",), (f"glv{j}",))
            di = rot("dg", ND)
            tsc(dgb[di][:], identb[:], glv[:, j:j + 1], gate[:, j:j + 1], ALU.mult, ALU.mult,
                ("identb", f"glv{j}", "gate"), (f"dgb{di}",))
            mm(pf[4][:, :], dgb[di][:], uvb[ui][:, 1024:1536], j == 0, j == 127, (f"dgb{di}", f"uvb{ui}"), ("pf4",))
            mm(pf[5][:, :], dgb[di][:], uvb[ui][:, 1536:2048], j == 0, j == 127, (f"dgb{di}", f"uvb{ui}"), ("pf5",))
        tt(X[:, 0:512], pf[4][:, :], X[:, 0:512], ALU.add, ("pf4", xk), (xk,))
        tt(X[:, 512:1024], pf[5][:, :], X[:, 512:1024], ALU.add, ("pf5", xk), (xk,))
        dma_sp(y[s, (t - 1) * 128:t * 128, :], X[:], (xk,), ("y",), final=True)

    oneb = sb("oneb", [128, 1])
    P.op("dve", lambda e: e.memset(oneb[:], 1.0), (), ("oneb",))

    if stop not in ("M0", "P1", "P2"):
        tile(None, 0, 0)
    par = 1
    for s in range(nseq):
        cp(S[:], S0[:], ("S0",), ("S",))
        cp(Sb[:], S0[:], ("S0",), ("Sb",), eng="act")
        for t in range(1, ntile + 1):
            if stop in ("M0", "P1", "P2"):
                dump(s, t, [])
                continue
            tile(s, t, par)
            par ^= 1
    P.flush()
    stack.close()
    return nc


def _host_consts():
    idx = np.arange(128)
    ident = np.eye(128, dtype=np.float32)
    s_ = idx[:, None]
    t_ = idx[None, :]
    tri = np.where(s_ <= t_, -1.0 / 16.0, 0.0).astype(np.float32)
    sut = np.where(s_ > t_, -1.0 / 16.0, 0.0).astype(np.float32)
    trim = np.where((s_ <= t_) & (s_ < 16), -1.0 / 16.0, 0.0).astype(np.float32)
    sutm = np.where((s_ > t_) & (s_ < 16), -1.0 / 16.0, 0.0).astype(np.float32)
    mut = np.where(s_ <= t_, 1.0, 0.0).astype(np.float32)
    cst = np.concatenate([ident, tri, sut, trim, sutm, mut, mut, mut, mut], axis=1)
    inv_freq = (10000.0 ** (-np.arange(32, dtype=np.float32) / 32.0)).astype(np.float32)
    rope = np.zeros((NKT, 128, 64), np.float32)
    for t in range(NKT):
        pos = (np.arange(128) if t == 0 else 16 + 128 * (t - 1) + np.arange(128)).astype(np.float32)
        ang = pos[:, None] * inv_freq[None, :]
        rope[t, :, 0:32] = np.cos(ang)
        rope[t, :, 32:64] = np.sin(ang)
    return np.ascontiguousarray(cst), rope


_CACHE = {}


def _run(inputs, nseq, ntile, ncores, stop=None):
    f = lambda a: np.ascontiguousarray(np.asarray(a, dtype=np.float32))
    x = f(inputs["x"])
    cst, rope = _host_consts()
    xm = np.zeros((128, 1024), np.float32)
    xm[0:16] = f(inputs["meta"])
    gcol = np.concatenate([f(inputs["norm_mix"])[0].reshape(8, 128).T, f(inputs["mla_q_norm"])[0].reshape(3, 128).T,
                           f(inputs["mla_kv_norm"])[0].reshape(2, 128).T], axis=1)
    gvec = np.concatenate([f(inputs["qn_nope"])[0], f(inputs["qn_pe"])[0], f(inputs["kn_nope"])[0],
                           f(inputs["kn_pe"])[0], f(inputs["gla_norm"])[0], f(inputs["norm_ffn"])[0]])
    gb = np.ascontiguousarray(np.broadcast_to(gvec[None, :], (128, gvec.shape[0])))
    wa2 = np.concatenate([f(inputs["gla_w_a2"])[0], f(inputs["gla_b_a"]), np.zeros((15, 512), np.float32)], axis=0)
    keys = f(inputs["peer_keys"])[0]
    keysT = np.ascontiguousarray(keys.transpose(2, 0, 1).reshape(128, 256))
    uvt = np.ascontiguousarray(np.concatenate([f(inputs["peer_u"])[0], f(inputs["peer_v"])[0]], axis=1))
    common = {
        "xm": xm, "w_in": f(inputs["w_in"])[0], "w_uq": f(inputs["mla_w_uq"])[0], "w_ukv": f(inputs["mla_w_ukv"])[0],
        "w_oa": f(inputs["w_o_mla"])[0], "w_ob": f(inputs["w_o_gla"])[0], "w_out": f(inputs["w_out"])[0],
        "w_pq": f(inputs["peer_w_q"])[0], "gcol": np.ascontiguousarray(gcol), "gb": gb, "wa2": np.ascontiguousarray(wa2),
        "keysT": keysT, "uv": (uvt if stop in (None, "H") else uvt[:128]), "rope": rope, "cst": cst,
    }
    key = (nseq, ntile, stop)
    if key not in _CACHE:
        _CACHE[key] = build(nseq, ntile, stop)
    nc = _CACHE[key]
    in_maps = []
    for c in range(ncores):
        m = dict(common)
        m["x"] = np.ascontiguousarray(x[c * nseq:(c + 1) * nseq])
        in_maps.append(m)
    import os
    res = run_bass_kernel_spmd(nc, in_maps, core_ids=list(range(ncores)), trace=bool(os.environ.get("DBGTRACE")))
    if os.environ.get("DBGTRACE"):
        print("EXEC_NS", res.exec_time_ns)
    return np.concatenate([np.asarray(r["y"]) for r in res.results], axis=0)


def kernel(**inputs):
    out = _run(inputs, 2, 16, NCORES)
    return out.astype(np.float32)
```

```python
import numpy as np
from contextlib import ExitStack
import concourse.bass as bass
import concourse.mybir as mybir
from concourse.bass_utils import run_bass_kernel_spmd

F32 = mybir.dt.float32
BF16 = mybir.dt.bfloat16
I32 = mybir.dt.int32
U32 = mybir.dt.uint32
AF = mybir.ActivationFunctionType
ALU = mybir.AluOpType
AX = mybir.AxisListType

EPS = 1e-6
NCORES = 8
ENGS = ("pe", "dve", "act", "pool", "sp")
EPOCH = 30000


class Prog:
    def __init__(self, nc, stack):
        self.nc = nc
        self.stack = stack
        self.q = {e: [] for e in ENGS}
        self.cnt = {e: 0 for e in ENGS}
        self.known = {e: {} for e in ENGS}
        self.sems = {}
        self.st = {}
        self.ring = {"sp": [("dma", "sp", i) for i in range(12)], "pool": [("dma", "pool", i) for i in range(16)],
                     "act": [("dma", "act", i) for i in range(4)]}
        self.ring_pos = {k: 0 for k in self.ring}
        self.ring_ev = {}
        self.ring_uses = {}
        self.final = []
        self.alias = {}

    def _x(self, keys):
        out = []
        for k in keys:
            out.extend(self.alias.get(k, (k,)))
        return out

    def _sem(self, key):
        if key not in self.sems:
            name = "s_" + "_".join(str(k) for k in key)
            self.sems[key] = self.stack.enter_context(self.nc.semaphore(name))
        return self.sems[key]

    def _deps(self, r, w):
        r = self._x(r)
        w = self._x(w)
        deps = []
        for k in r:
            s = self.st.get(k)
            if s and s[0]:
                deps.append(s[0])
        for k in w:
            s = self.st.get(k)
            if s:
                if s[0]:
                    deps.append(s[0])
                deps.extend(s[1].values())
        return deps

    def _wait(self, eng, deps):
        for (sk, val) in deps:
            if eng == "pe" and sk[0] == "pe":
                continue
            if self.known[eng].get(sk, 0) >= val:
                continue
            self.known[eng][sk] = val
            self._sem(sk)
            self.q[eng].append(("wait", sk, val))

    def _record(self, ev, r, w):
        r = self._x(r)
        w = self._x(w)
        for k in w:
            self.st[k] = [ev, {}]
        for k in r:
            s = self.st.setdefault(k, [None, {}])
            s[1][ev[0]] = ev

    def op(self, eng, fn, r=(), w=()):
        self._wait(eng, self._deps(r, w))
        self.cnt[eng] += 1
        c = self.cnt[eng]
        ep = (c - 1) // EPOCH
        ev = ((eng, ep), c - ep * EPOCH)
        self._sem(ev[0])
        self.q[eng].append(("op", fn, ev[0], 1))
        self._record(ev, r, w)
        return ev

    def dma(self, eng, fn, r=(), w=(), final=False):
        ring = self.ring[eng]
        sk = ring[self.ring_pos[eng] % len(ring)]
        self.ring_pos[eng] += 1
        deps = self._deps(r, w)
        if sk in self.ring_ev:
            deps.append(self.ring_ev[sk])
        self._wait(eng, deps)
        self.ring_uses[sk] = self.ring_uses.get(sk, 0) + 1
        ev = (sk, 16 * self.ring_uses[sk])
        self.ring_ev[sk] = ev
        self._sem(sk)
        self.q[eng].append(("op", fn, sk, 16))
        self._record(ev, r, w)
        if final:
            self.final.append(ev)
        return ev

    def flush(self):
        nc = self.nc
        self._wait("sp", self.final)
        self._wait("sp", list(self.ring_ev.values()))
        print("PROG counts", self.cnt, {k: len(v) for k, v in self.q.items()}, flush=True)
        q = self.q
        sems = self.sems

        def run(name, e):
            for it in q[name]:
                if it[0] == "wait":
                    e.wait_ge(sems[it[1]], it[2])
                else:
                    ins = it[1](e)
                    ins.then_inc(sems[it[2]], it[3])

        with nc.Block() as block:
            @block.tensor
            def _(e):
                run("pe", e)

            @block.vector
            def _(e):
                run("dve", e)

            @block.scalar
            def _(e):
                run("act", e)

            @block.gpsimd
            def _(e):
                run("pool", e)

            @block.sync
            def _(e):
                run("sp", e)


C_CQ, C_CKV, C_KPE, C_GQ, C_GK, C_GV, C_GA, C_GG, C_GTA, C_GTB, C_END = (
    0, 384, 640, 704, 1216, 1728, 2752, 2768, 3792, 4816, 5840)
NKT = 17


def build(nseq, ntile, stop=None):
    nc = bass.Bass("TRN2", target_bir_lowering=False)
    stack = ExitStack()
    P = Prog(nc, stack)

    def din(name, shape, dt=F32):
        return nc.dram_tensor(name, list(shape), dt, kind="ExternalInput").ap()

    x = din("x", [nseq, 2048, 1024])
    xm = din("xm", [128, 1024])
    Wd = {
        "in": (din("w_in", [1024, 5840]), 8, 5840, 0),
        "uq": (din("w_uq", [384, 1536]), 3, 1536, 8),
        "ukv": (din("w_ukv", [256, 2048]), 2, 2048, 11),
        "oa": (din("w_oa", [1024, 1024]), 8, 1024, None),
        "ob": (din("w_ob", [1024, 1024]), 8, 1024, None),
        "out": (din("w_out", [1024, 1024]), 8, 1024, None),
        "pq": (din("w_pq", [1024, 2048]), 8, 2048, None),
    }
    gcol_d = din("gcol", [128, 13])
    gb_d = din("gb", [128, 1664])
    wa2_d = din("wa2", [32, 512])
    keysT_d = din("keysT", [128, 256])
    NEXP = 16384 if stop in (None, "H") else 128
    uv = din("uv", [NEXP, 2048])
    rope_d = din("rope", [NKT, 128, 64])
    cst_d = din("cst", [128, 1152])
    y = nc.dram_tensor("y", [nseq, 2048, 1024], F32, kind="ExternalOutput").ap()
    uvs = nc.dram_tensor("uvs", [NEXP, 2048], BF16, kind="Internal").ap()
    ws = {k: nc.dram_tensor("ws_" + k, [128, v[1], v[2]], BF16, kind="Internal").ap() for k, v in Wd.items()}

    def sb(name, shape, dt=F32):
        return stack.enter_context(nc.sbuf_tensor("sb_" + name, list(shape), dt))

    def ps(name, shape, dt=F32):
        return stack.enter_context(nc.psum_tensor("ps_" + name, list(shape), dt))

    cst = sb("cst", [128, 1152])
    IDENT = cst[:, 0:128]
    TRI = cst[:, 128:256]
    SUT = cst[:, 256:384]
    TRIM = cst[:, 384:512]
    SUTM = cst[:, 512:640]
    MASKUT = cst[:, 640:1152]
    identb = sb("identb", [128, 128], BF16)
    gcol = sb("gcol", [128, 13])
    gbt = sb("gbt", [128, 1664])
    G_QN, G_QP, G_KN, G_KP, G_GLA, G_FFN = (gbt[:, 0:128], gbt[:, 128:192], gbt[:, 192:320], gbt[:, 320:384],
                                          gbt[:, 384:640], gbt[:, 640:1664])
    wa2 = sb("wa2", [32, 512])
    keysT = sb("keysT", [128, 256])
    ropet = sb("ropet", [128, NKT, 64])

    KT = sb("KT", [128, 8, NKT * 128], BF16)
    KPT = sb("KPT", [128, NKT * 128], BF16)
    VC = sb("VC", [128, NKT, 8, 132], BF16)
    S = sb("S", [128, 4, 256])
    S0 = sb("S0", [128, 4, 256])
    Sb = sb("Sb", [128, 4, 256], BF16)

    NWS = 2
    wslot = [sb(f"wslot{i}", [128, 8, 512], BF16) for i in range(NWS)]
    NUV = 12
    ND = 8

    AW = 16896
    arena = sb("arena", [128, AW])

    def carve(name, off, shape, dt=F32):
        per = 1
        for d in shape[1:]:
            per *= d
        words = per if dt in (F32, I32, U32) else per // 2
        assert off + words <= AW, name
        v = arena[:, off:off + words]
        if dt != F32:
            v = v.bitcast(dt)
        if len(shape) == 3:
            v = v.rearrange("p (a b) -> p a b", a=shape[1])
        P.alias[name] = tuple(("ar", g) for g in range(off // 128, (off + words - 1) // 128 + 1))
        return v

    lat = carve("lat", 0, [128, 704])
    qsb = carve("qsb", 704, [128, 8, 192])
    kvs = carve("kvs", 2240, [128, 8, 128])
    knn = carve("knn", 3264, [128, 8, 128], BF16)
    qnn = carve("qnn", 3776, [128, 8, 128], BF16)
    qpe = carve("qpe", 4288, [128, 8, 64])
    qpr = carve("qpr", 4800, [128, 8, 64], BF16)
    cqn = carve("cqn", 5056, [128, 384], BF16)
    ckn = carve("ckn", 5248, [128, 256], BF16)
    cqT = carve("cqT", 5376, [128, 3, 128], BF16)
    ckT = carve("ckT", 5568, [128, 2, 128], BF16)
    kp = carve("kp", 5696, [128, 64])
    kp2 = carve("kp2", 5760, [128, 128], BF16)
    rt = carve("rt", 5824, [128, 8, 32])
    rt2 = carve("rt2", 6080, [128, 8, 32])
    e1 = carve("e1", 0, [128, 512])
    lsp = carve("lsp", 512, [128, 512])
    Ep = carve("Ep", 1024, [128, 512])
    En = carve("En", 1536, [128, 512])
    QtT = carve("QtT", 2048, [128, 4, 128], BF16)
    KtT = carve("KtT", 2304, [128, 4, 128], BF16)
    Khat = carve("Khat", 2560, [128, 512], BF16)
    At = carve("At", 2816, [128, 512], BF16)
    n2b = carve("n2b", 0, [128, 1024], BF16)
    pqT = carve("pqT", 512, [128, 4, 128])
    ss2 = carve("ss2", 1024, [128, 4, 128])
    tsv = carve("tsv", 1536, [128, 16, 16])
    tiu = carve("tiu", 1792, [128, 16, 16], U32)
    tif = carve("tif", 2048, [128, 16, 16])
    cands = carve("cands", 2304, [128, 256])
    cands2 = carve("cands2", 2560, [128, 256])
    candi = carve("candi", 2816, [128, 256])
    dgb = [carve(f"dgb{i}", 3072 + 64 * i, [128, 128], BF16) for i in range(ND)]
    uvb = [carve(f"uvb{i}", 3584 + 1024 * i, [128, 2048], BF16) for i in range(NUV)]
    stg = [carve(f"stg{i}", 512 * i, [128, 512]) for i in range(2)]
    stgb = [carve(f"stgb{i}", 1024 + 256 * i, [128, 512], BF16) for i in range(2)]

    ustg = [carve(f"ustg{i}", 2048 * i, [128, 2048]) for i in range(2)]
    ustgb = [carve(f"ustgb{i}", 4096 + 1024 * i, [128, 2048], BF16) for i in range(2)]

    xt = [sb("xt0", [128, 1024])]
    junk = sb("junk", [128, 1024], BF16)
    junk2 = sb("junk2", [128, 1024], BF16)
    st1 = sb("st1", [128, 64])
    gaT = sb("gaT", [32, 128])
    pm = [sb(f"pm{i}", [16, 128], BF16) for i in range(2)]
    yT = sb("yT", [128, 8, 128], BF16)
    o2 = 7168
    nb = carve("nb", o2, [128, 1024], BF16)
    nT = carve("nT", o2 + 512, [128, 8, 128], BF16)
    gqT = carve("gqT", o2 + 1024, [128, 4, 128])
    gkT = carve("gkT", o2 + 1536, [128, 4, 128])
    gktm = carve("gktm", o2 + 2048, [128, 512])
    gv = carve("gv", o2 + 2560, [128, 1024], BF16)
    sg = carve("sg", o2 + 3072, [128, 1024], BF16)
    gas = carve("gas", o2 + 3584, [128, 1024], BF16)
    gbs = carve("gbs", o2 + 4096, [128, 1024], BF16)
    qnT = carve("qnT", o2 + 4608, [128, 8, 128], BF16)
    qpT = carve("qpT", o2 + 5120, [128, 4, 128], BF16)
    pb = [carve(f"pb{i}", o2 + 5376 + 256 * i, [128, 512], BF16) for i in range(3)]
    ya = carve("ya", o2 + 6144, [128, 1024], BF16)
    yb = carve("yb", o2 + 6656, [128, 1024], BF16)
    MG = carve("MG", o2 + 7168, [128, 2048])
    mixb = carve("mixb", o2 + 9216, [128, 1024], BF16)
    mix = MG[:, 0:1024]
    gsg = MG[:, 1024:2048]
    sq = MG[:, 0:1536].rearrange("p (a b) -> p a b", a=8)
    for nm in ("mix", "gsg", "sq"):
        P.alias[nm] = P.alias["MG"]
    bsv = sb("bsv", [128, 8, 16])
    idxf = sb("idxf", [128, 128])
    idxi = sb("idxi", [128, 128], I32)
    gate = sb("gate", [128, 128])
    actv = sb("actv", [128, 128])
    glv = sb("glv", [128, 128])

    pf = [ps(f"pf{i}", [128, 512]) for i in range(6)]
    pt = [ps(f"pt{i}", [128, 1024], BF16) for i in range(2)]
    cnt = {"mm": 0, "s": 0, "t": 0, "w": 0, "pb": 0, "pm": 0, "stg": 0, "uv": 0, "dg": 0}

    def rot(kind, n):
        v = cnt[kind] % n
        cnt[kind] += 1
        return v

    def bank_mm():
        i = rot("mm", 2)
        return pf[i], f"pf{i}"

    def bank_s():
        i = 2 + rot("s", 2)
        return pf[i], f"pf{i}"

    def bank_t():
        i = rot("t", 2)
        return pt[i], f"pt{i}"

    def dma_sp(out, in_, r, w, final=False):
        P.dma("sp", lambda e: e.dma_start(out=out, in_=in_), r, w, final)

    def mm(out, lhsT, rhs, start, stop, r, w):
        P.op("pe", lambda e: e.matmul(out, lhsT, rhs, start=start, stop=stop), r, w)

    def tr(out, in_, idn, r, w):
        P.op("pe", lambda e: e.transpose(out, in_, idn), r, w)

    def act(out, in_, func, r, w, bias=None, scale=None, accum_out=None):
        kw = {}
        if bias is not None:
            kw["bias"] = bias
        if scale is not None:
            kw["scale"] = scale
        if accum_out is not None:
            kw["accum_out"] = accum_out
        P.op("act", lambda e: e.activation(out=out, in_=in_, func=func, **kw), r, w)

    def tt(out, in0, in1, op, r, w, eng="dve"):
        P.op(eng, lambda e: e.tensor_tensor(out=out, in0=in0, in1=in1, op=op), r, w)

    def tsc(out, in0, s1, s2, op0, op1, r, w, eng="dve", accum_out=None):
        if op1 is None:
            P.op(eng, lambda e: e.tensor_scalar(out=out, in0=in0, scalar1=s1, scalar2=None, op0=op0), r, w)
        else:
            P.op(eng, lambda e: e.tensor_scalar(out=out, in0=in0, scalar1=s1, scalar2=s2, op0=op0, op1=op1,
                                                accum_out=accum_out), r, w)

    def stt(out, in0, scalar, in1, op0, op1, r, w, accum_out=None):
        P.op("dve", lambda e: e.scalar_tensor_tensor(out=out, in0=in0, scalar=scalar, in1=in1, op0=op0, op1=op1,
                                                     accum_out=accum_out), r, w)

    def cp(out, in_, r, w, eng="dve"):
        if eng == "act":
            P.op("act", lambda e: e.copy(out=out, in_=in_), r, w)
        else:
            P.op(eng, lambda e: e.tensor_copy(out=out, in_=in_), r, w)

    def recip(out, in_, r, w):
        P.op("dve", lambda e: e.reciprocal(out=out, in_=in_), r, w)

    def red(out, in_, op, r, w):
        P.op("dve", lambda e: e.tensor_reduce(out=out, in_=in_, axis=AX.X, op=op), r, w)

    def rstd_from_ss(dst, ssum, n, r, w):
        act(dst, ssum, AF.Sqrt, tuple(r) + ("epsb",), w, bias=epsb[:, 0:1], scale=1.0 / n)
        recip(dst, dst, w, w)

    epsb = sb("epsb", [128, 1])
    P.op("dve", lambda e: e.memset(epsb[:], EPS), (), ("epsb",))

    import os
    SK = os.environ.get("DBGSKIP", "").split(",")
    if "cst" not in SK:
        dma_sp(cst[:], cst_d, (), ("cst",))
    if "gcol" not in SK:
        dma_sp(gcol[:], gcol_d, (), ("gcol",))
    if "gbt" not in SK:
        dma_sp(gbt[:], gb_d, (), ("gbt",))
    if "wa2" not in SK:
        dma_sp(wa2[:], wa2_d, (), ("wa2",))
    if "keysT" not in SK:
        dma_sp(keysT[:], keysT_d, (), ("keysT",))
    if "rope" not in SK:
        dma_sp(ropet[:], rope_d.rearrange("t p c -> p t c"), (), ("ropet",))
    if "ms" not in SK:
        cp(identb[:], IDENT, ("cst",), ("identb",))
        P.op("dve", lambda e: e.memset(gaT[:], 1.0), (), ("gaT",))
        P.op("dve", lambda e: e.memset(VC[:], 1.0), (), tuple(f"V{t}" for t in range(NKT)))
        P.op("dve", lambda e: e.memset(S0[:], 0.0), (), ("S0",))

    for name, (wd, kc_n, ncol, gc) in Wd.items():
        if stop == "P1" or (stop == "P2" and name != "ukv"):
            continue
        for kc in range(kc_n):
            for c0 in range(0, ncol, 512):
                c1 = min(ncol, c0 + 512)
                i = rot("stg", 2)
                dma_sp(stg[i][:, 0:c1 - c0], wd[kc * 128:(kc + 1) * 128, c0:c1], (), (f"stg{i}",))
                if gc is None:
                    cp(stgb[i][:, 0:c1 - c0], stg[i][:, 0:c1 - c0], (f"stg{i}",), (f"stgb{i}",),
                       eng=("act" if (cnt["stg"] % 4 < 2) else "dve"))
                else:
                    tsc(stgb[i][:, 0:c1 - c0], stg[i][:, 0:c1 - c0], gcol[:, gc + kc:gc + kc + 1], None, ALU.mult,
                        None, (f"stg{i}", "gcol"), (f"stgb{i}",))
                dma_sp(ws[name][:, kc, c0:c1], stgb[i][:, 0:c1 - c0], (f"stgb{i}",), (f"ws_{name}",))

    if stop in (None, "H"):
        for i in range(NEXP // 128):
            k = i % 2
            dma_sp(ustg[k][:], uv[i * 128:(i + 1) * 128, :], (), (f"ustg{k}",))
            cp(ustgb[k][:], ustg[k][:], (f"ustg{k}",), (f"ustgb{k}",), eng=("act", "dve", "pool")[i % 3])
            dma_sp(uvs[i * 128:(i + 1) * 128, :], ustgb[k][:], (f"ustgb{k}",), ("uvs",))

    def load_w(name, c0, c1):
        kc_n = Wd[name][1]
        i = rot("w", NWS)
        dma_sp(wslot[i][:, 0:kc_n, 0:c1 - c0], ws[name][:, :, c0:c1], (f"ws_{name}",), (f"wslot{i}",))
        return wslot[i], f"wslot{i}", kc_n

    def proj_tm(name, c0, c1, lhs, lhs_key):
        wsl, wk, kc_n = load_w(name, c0, c1)
        bk, bkey = bank_mm()
        for kc in range(kc_n):
            mm(bk[:, 0:c1 - c0], lhs[:, kc, :], wsl[:, kc, 0:c1 - c0], kc == 0, kc == kc_n - 1, (lhs_key, wk), (bkey,))
        return bk, bkey

    def transposes(dst, dst_key, src, src_key, nblk, eng="dve"):
        bk, bkey = bank_t()
        for j in range(nblk):
            tr(bk[:, j * 128:(j + 1) * 128], src[:, j * 128:(j + 1) * 128], identb[:], (src_key, "identb"), (bkey,))
        cp(dst, bk[:, 0:nblk * 128], (bkey,), (dst_key,), eng=eng)

    SCALE_ATT = 192.0 ** -0.5
    SCALE_GLA = 128.0 ** -0.5

    def rope_apply(dst, src, t, nh, r, w):
        cos = ropet[:, t, 0:32].unsqueeze(1).to_broadcast([128, nh, 32])
        sin = ropet[:, t, 32:64].unsqueeze(1).to_broadcast([128, nh, 32])
        x1 = src[:, :, 0:32]
        x2 = src[:, :, 32:64]
        a = rt[:, 0:nh, :]
        b = rt2[:, 0:nh, :]
        tt(a, x1, cos, ALU.mult, r + ("ropet",), ("rt",))
        tt(b, x2, sin, ALU.mult, r + ("ropet",), ("rt2",))
        tt(dst[:, :, 0:32], a, b, ALU.subtract, ("rt", "rt2"), w)
        tt(a, x2, cos, ALU.mult, r + ("ropet",), ("rt",))
        tt(b, x1, sin, ALU.mult, r + ("ropet",), ("rt2",))
        tt(dst[:, :, 32:64], a, b, ALU.add, ("rt", "rt2"), w)

    def dump(s, t, items):
        X = xt[0]
        o = 0
        for ap, key, n in items:
            cp(X[:ap.shape[0], o:o + n], ap, (key,), ("xt0",))
            o += n
        dma_sp(y[s, (t - 1) * 128:t * 128, :], X[:], ("xt0",), ("y",), final=True)

    def tile(s, t, par):
        is_meta = s is None
        X = xt[0]
        xk = "xt0"
        if is_meta:
            dma_sp(X[:], xm, (), (xk,))
        else:
            dma_sp(X[:], x[s, (t - 1) * 128:t * 128, :], (), (xk,))
        act(junk[:], X[:], AF.Square, (xk,), ("st1a",), accum_out=st1[:, 0:1])
        rstd_from_ss(st1[:, 0:1], st1[:, 0:1], 1024.0, ("st1a",), ("st1a",))
        tsc(nb[:], X[:], st1[:, 0:1], None, ALU.mult, None, (xk, "st1a"), ("nb",))
        transposes(nT[:].rearrange("p a b -> p (a b)"), "nT", nb, "nb", 8)

        if stop == "A":
            if not is_meta:
                dump(s, t, [(nb[:], "nb", 1024)])
            return
        bk, bkey = proj_tm("in", 0, 512, nT, "nT")
        cp(lat[:, 0:512], bk[:, 0:512], (bkey,), ("lat",), eng="act")
        bk, bkey = proj_tm("in", 512, 704, nT, "nT")
        cp(lat[:, 512:704], bk[:, 0:192], (bkey,), ("lat",), eng="act")
        wsl, wk, _ = load_w("in", C_GQ, C_GQ + 512)
        bk, bkey = bank_mm()
        for h in range(4):
            for kc in range(8):
                mm(bk[:, h * 128:(h + 1) * 128], wsl[:, kc, h * 128:(h + 1) * 128], nT[:, kc, :], kc == 0, kc == 7,
                   ("nT", wk), (bkey,))
        cp(gqT[:].rearrange("p a b -> p (a b)"), bk[:, :], (bkey,), ("gqT",), eng="act")
        wsl, wk, _ = load_w("in", C_GK, C_GK + 512)
        bk, bkey = bank_mm()
        for h in range(4):
            for kc in range(8):
                mm(bk[:, h * 128:(h + 1) * 128], wsl[:, kc, h * 128:(h + 1) * 128], nT[:, kc, :], kc == 0, kc == 7,
                   ("nT", wk), (bkey,))
        cp(gkT[:].rearrange("p a b -> p (a b)"), bk[:, :], (bkey,), ("gkT",), eng="act")
        bk, bkey = bank_mm()
        for kc in range(8):
            mm(bk[:, :], nT[:, kc, :], wsl[:, kc, :], kc == 0, kc == 7, ("nT", wk), (bkey,))
        cp(gktm[:], bk[:, :], (bkey,), ("gktm",), eng="dve")
        for j in range(2):
            bk, bkey = proj_tm("in", C_GV + j * 512, C_GV + (j + 1) * 512, nT, "nT")
            cp(gv[:, j * 512:(j + 1) * 512], bk[:, :], (bkey,), ("gv",), eng=("act" if j == 0 else "dve"))
        wsl, wk, _ = load_w("in", C_GA, C_GA + 16)
        bk, bkey = bank_mm()
        for kc in range(8):
            mm(bk[0:16, 0:128], wsl[:, kc, 0:16], nT[:, kc, :], kc == 0, kc == 7, ("nT", wk), (bkey,))
        cp(gaT[0:16, :], bk[0:16, 0:128], (bkey,), ("gaT",), eng="dve")
        if not is_meta:
            for j in range(2):
                bk, bkey = proj_tm("in", C_GG + j * 512, C_GG + (j + 1) * 512, nT, "nT")
                act(sg[:, j * 512:(j + 1) * 512], bk[:, :], AF.Silu, (bkey,), ("sg",))
            for j in range(2):
                bk, bkey = proj_tm("in", C_GTA + j * 512, C_GTA + (j + 1) * 512, nT, "nT")
                act(gas[:, j * 512:(j + 1) * 512], bk[:, :], AF.Sigmoid, (bkey,), ("gas",))
            for j in range(2):
                bk, bkey = proj_tm("in", C_GTB + j * 512, C_GTB + (j + 1) * 512, nT, "nT")
                act(gbs[:, j * 512:(j + 1) * 512], bk[:, :], AF.Sigmoid, (bkey,), ("gbs",))

        if stop == "B":
            if not is_meta:
                dump(s, t, [(lat[:, :], "lat", 704), (gv[:, 0:320], "gv", 320)])
            return
        tt(sq[:].rearrange("p a b -> p (a b)")[:, 0:704], lat[:, :], lat[:, :], ALU.mult, ("lat",), ("sq",))
        sqf = sq[:].rearrange("p a b -> p (a b)")
        red(st1[:, 1:2], sqf[:, 0:384], ALU.add, ("sq",), ("st1b",))
        red(st1[:, 2:3], sqf[:, 384:640], ALU.add, ("sq",), ("st1b",))
        red(st1[:, 3:4], sqf[:, 640:704], ALU.add, ("sq",), ("st1b",))
        rstd_from_ss(st1[:, 1:2], st1[:, 1:2], 384.0, ("st1b",), ("st1b",))
        rstd_from_ss(st1[:, 2:3], st1[:, 2:3], 256.0, ("st1b",), ("st1b",))
        rstd_from_ss(st1[:, 3:4], st1[:, 3:4], 64.0, ("st1b",), ("st1b",))
        tsc(ckn[:], lat[:, 384:640], st1[:, 2:3], None, ALU.mult, None, ("lat", "st1b"), ("ckn",))
        transposes(ckT[:].rearrange("p a b -> p (a b)"), "ckT", ckn, "ckn", 2)
        stt(kp[:], lat[:, 640:704], st1[:, 3:4], G_KP, ALU.mult, ALU.mult, ("lat", "st1b", "gbt"), ("kp",))
        kpv = kp[:].unsqueeze(1)
        kp2v = kp2[:].rearrange("p (a b) -> p a b", a=2)
        rope_apply(kp2v[:, 0:1, :], kpv, t, 1, ("kp",), ("kp2",))
        cp(kp2[:, 64:128], kp2[:, 0:64], ("kp2",), ("kp2",))
        bk, bkey = bank_t()
        tr(bk[:, 0:128], kp2[:], identb[:], ("kp2", "identb"), (bkey,))
        cp(KPT[:, t * 128:(t + 1) * 128], bk[:, 0:128], (bkey,), (f"KP{t}",))
        if stop == "C1":
            if not is_meta:
                dump(s, t, [])
            return
        for j in range(4):
            bk, bkey = proj_tm("ukv", j * 512, (j + 1) * 512, ckT, "ckT")
            bv = bk[:, :].rearrange("p (h c) -> p h c", h=2)
            ee = "act" if j % 2 == 0 else "dve"
            cp(kvs[:, 2 * j:2 * j + 2, :], bv[:, :, 0:128], (bkey,), ("kvs",), eng=ee)
            cp(VC[:, t, 2 * j:2 * j + 2, 0:128], bv[:, :, 128:256], (bkey,), (f"V{t}",), eng=ee)
        if stop == "C1a":
            if not is_meta:
                dump(s, t, [])
            return
        tt(sq[:, :, 0:128], kvs[:], kvs[:], ALU.mult, ("kvs",), ("sq",))
        red(st1[:, 8:16], sq[:, :, 0:128], ALU.add, ("sq",), ("st1c",))
        rstd_from_ss(st1[:, 8:16], st1[:, 8:16], 128.0, ("st1c",), ("st1c",))
        if stop == "C1b":
            if not is_meta:
                dump(s, t, [])
            return
        tt(kvs[:], kvs[:], st1[:, 8:16].unsqueeze(2).to_broadcast([128, 8, 128]), ALU.mult, ("kvs", "st1c"), ("kvs",))
        tt(knn[:], kvs[:], G_KN.unsqueeze(1).to_broadcast([128, 8, 128]), ALU.mult, ("kvs", "gbt"), ("knn",))
        if stop == "C1c":
            if not is_meta:
                dump(s, t, [])
            return
        bk, bkey = bank_t()
        for h in range(8):
            tr(bk[:, h * 128:(h + 1) * 128], knn[:, h, :], identb[:], ("knn", "identb"), (bkey,))
        cp(KT[:, :, t * 128:(t + 1) * 128], bk[:, :].rearrange("p (h c) -> p h c", h=8), (bkey,), (f"K{t}",), eng="act")

        if stop == "C2":
            if not is_meta:
                dump(s, t, [])
            return
        if not is_meta:
            tsc(cqn[:], lat[:, 0:384], st1[:, 1:2], None, ALU.mult, None, ("lat", "st1b"), ("cqn",))
            transposes(cqT[:].rearrange("p a b -> p (a b)"), "cqT", cqn, "cqn", 3)
            qf = qsb[:].rearrange("p a b -> p (a b)")
            for j in range(3):
                bk, bkey = proj_tm("uq", j * 512, (j + 1) * 512, cqT, "cqT")
                cp(qf[:, j * 512:(j + 1) * 512], bk[:, :], (bkey,), ("qsb",), eng=("act" if j != 1 else "dve"))
            tt(sq[:], qsb[:], qsb[:], ALU.mult, ("qsb",), ("sq",))
            red(st1[:, 16:24], sq[:, :, 0:128], ALU.add, ("sq",), ("st1d",))
            red(st1[:, 24:32], sq[:, :, 128:192], ALU.add, ("sq",), ("st1d",))
            rstd_from_ss(st1[:, 16:24], st1[:, 16:24], 128.0, ("st1d",), ("st1d",))
            rstd_from_ss(st1[:, 24:32], st1[:, 24:32], 64.0, ("st1d",), ("st1d",))
            tt(qsb[:, :, 0:128], qsb[:, :, 0:128], st1[:, 16:24].unsqueeze(2).to_broadcast([128, 8, 128]), ALU.mult,
               ("qsb", "st1d"), ("qsb",))
            tt(qnn[:], qsb[:, :, 0:128], G_QN.unsqueeze(1).to_broadcast([128, 8, 128]), ALU.mult, ("qsb", "gbt"),
               ("qnn",))
            tt(qsb[:, :, 128:192], qsb[:, :, 128:192], st1[:, 24:32].unsqueeze(2).to_broadcast([128, 8, 64]), ALU.mult,
               ("qsb", "st1d"), ("qsb",))
            tt(qpe[:], qsb[:, :, 128:192], G_QP.unsqueeze(1).to_broadcast([128, 8, 64]), ALU.mult, ("qsb", "gbt"),
               ("qpe",))
            rope_apply(qpr[:], qpe[:], t, 8, ("qpe",), ("qpr",))
            transposes(qnT[:].rearrange("p a b -> p (a b)"), "qnT", qnn[:].rearrange("p a b -> p (a b)"), "qnn", 8,
                       eng="act")
            transposes(qpT[:].rearrange("p a b -> p (a b)"), "qpT", qpr[:].rearrange("p a b -> p (a b)"), "qpr", 4,
                       eng="act")

            if stop == "C":
                dump(s, t, [(qnn[:].rearrange("p a b -> p (a b)")[:, 0:512], "qnn", 512),
                            (qpr[:].rearrange("p a b -> p (a b)"), "qpr", 512)])
                return
            kts = list(range(1, t + 1))
            for h in range(8):
                hp0 = (h % 2) * 64
                acc = pf[4 + (h % 2)]
                akey = f"pf{4 + (h % 2)}"
                bk, bkey = bank_s()
                mm(bk[0:16, 0:128], KT[:, h, 0:16], qnT[:, h, :], True, False, ("K0", "qnT"), (bkey,))
                mm(bk[0:16, 0:128], KPT[hp0:hp0 + 64, 0:16], qpT[hp0:hp0 + 64, h // 2, :], False, True,
                   ("KP0", "qpT"), (bkey,))
                mi = rot("pm", 2)
                act(pm[mi][:], bk[0:16, 0:128], AF.Exp, (bkey,), (f"pm{mi}",), scale=SCALE_ATT)
                mm(acc[:, 0:129], pm[mi][:], VC[0:16, 0, h, 0:129], True, False, (f"pm{mi}", "V0"), (akey,))
                for g0 in range(0, len(kts), 4):
                    grp = kts[g0:g0 + 4]
                    bk, bkey = bank_s()
                    for j, kt in enumerate(grp):
                        mm(bk[:, j * 128:(j + 1) * 128], KT[:, h, kt * 128:(kt + 1) * 128], qnT[:, h, :], True, False,
                           (f"K{kt}", "qnT"), (bkey,))
                        mm(bk[:, j * 128:(j + 1) * 128], KPT[hp0:hp0 + 64, kt * 128:(kt + 1) * 128],
                           qpT[hp0:hp0 + 64, h // 2, :], False, True, (f"KP{kt}", "qpT"), (bkey,))
                    pi = rot("pb", 3)
                    n = len(grp) * 128
                    act(pb[pi][:, 0:n], bk[:, 0:n], AF.Exp, (bkey,), (f"pb{pi}",), scale=SCALE_ATT)
                    if grp[-1] == t:
                        j = len(grp) - 1
                        P.op("dve", (lambda pp, jj: (lambda e: e.memset(pp[64:128, jj * 128:jj * 128 + 64], 0.0)))(
                            pb[pi], j), (), (f"pb{pi}",))
                    for j, kt in enumerate(grp):
                        mm(acc[:, 0:129], pb[pi][:, j * 128:(j + 1) * 128], VC[:, kt, h, 0:129], False, kt == t,
                           (f"pb{pi}", f"V{kt}"), (akey,))
                recip(st1[:, 32 + h:33 + h], acc[:, 128:129], (akey,), (f"st1e{h}",))
                tsc(ya[:, h * 128:(h + 1) * 128], acc[:, 0:128], st1[:, 32 + h:33 + h], None, ALU.mult, None,
                    (akey, f"st1e{h}"), ("ya",))

        if stop == "D" and not is_meta:
            dump(s, t, [(ya[:], "ya", 1024)])
            return
        tri = TRIM if is_meta else TRI
        sut = SUTM if is_meta else SUT
        bk, bkey = bank_s()
        mm(bk[:, :], gaT[:, :], wa2[:, :], True, True, ("gaT", "wa2"), (bkey,))
        act(e1[:], bk[:, :], AF.Exp, (bkey,), ("e1",), scale=-1.0)
        act(lsp[:], e1[:], AF.Ln, ("e1", "oneb"), ("lsp",), bias=oneb[:, 0:1])
        bk, bkey = bank_s()
        for h in range(4):
            mm(bk[:, h * 128:(h + 1) * 128], lsp[:, h * 128:(h + 1) * 128], tri, True, True, ("lsp", "cst"), (bkey,))
        act(Ep[:], bk[:, :], AF.Exp, (bkey,), ("Ep",))
        act(En[:], bk[:, :], AF.Exp, (bkey,), ("En",), scale=-1.0)
        bk, bkey = bank_s()
        mm(bk[:, :], sut, lsp[:], True, True, ("lsp", "cst"), (bkey,))
        act(e1[:], bk[:, :], AF.Exp, (bkey,), ("e1",))
        tt(Khat[:], gktm[:], e1[:], ALU.mult, ("gktm", "e1"), ("Khat",))
        tt(KtT[:].rearrange("p a b -> p (a b)"), gkT[:].rearrange("p a b -> p (a b)"), En[:], ALU.mult, ("gkT", "En"),
           ("KtT",))
        if not is_meta:
            stt(QtT[:].rearrange("p a b -> p (a b)"), gqT[:].rearrange("p a b -> p (a b)"), SCALE_GLA, Ep[:], ALU.mult,
                ALU.mult, ("gqT", "Ep"), ("QtT",))
            bk, bkey = bank_s()
            for h in range(4):
                mm(bk[:, h * 128:(h + 1) * 128], KtT[:, h, :], QtT[:, h, :], True, True, ("KtT", "QtT"), (bkey,))
            tt(At[:], bk[:, :], MASKUT, ALU.mult, (bkey, "cst"), ("At",))
            for h in range(4):
                acc = pf[4 + h // 2]
                akey = f"pf{4 + h // 2}"
                o = (h % 2) * 256
                mm(acc[:, o:o + 256], At[:, h * 128:(h + 1) * 128], gv[:, h * 256:(h + 1) * 256], True, False,
                   ("At", "gv"), (akey,))
                mm(acc[:, o:o + 256], QtT[:, h, :], Sb[:, h, :], False, True, ("QtT", "Sb"), (akey,))
            for h in range(4):
                acc = pf[4 + h // 2]
                akey = f"pf{4 + h // 2}"
                o = (h % 2) * 256
                act(junk[:, 0:256], acc[:, o:o + 256], AF.Square, (akey,), ("st1f",),
                    accum_out=st1[:, 40 + h:41 + h])
            rstd_from_ss(st1[:, 40:44], st1[:, 40:44], 256.0, ("st1f",), ("st1f",))
            tt(gsg[:].rearrange("p (h c) -> p h c", h=4), sg[:].rearrange("p (h c) -> p h c", h=4),
               G_GLA.unsqueeze(1).to_broadcast([128, 4, 256]), ALU.mult, ("sg", "gbt"), ("gsg",))
            for h in range(4):
                acc = pf[4 + h // 2]
                akey = f"pf{4 + h // 2}"
                o = (h % 2) * 256
                stt(yb[:, h * 256:(h + 1) * 256], acc[:, o:o + 256], st1[:, 40 + h:41 + h],
                    gsg[:, h * 256:(h + 1) * 256], ALU.mult, ALU.mult, (akey, "st1f", "gsg"), ("yb",))
        for h in range(4):
            bk, bkey = bank_s()
            mm(bk[:, 0:256], Khat[:, h * 128:(h + 1) * 128], gv[:, h * 256:(h + 1) * 256], True, True, ("Khat", "gv"),
               (bkey,))
            if is_meta:
                cp(S0[:, h, :], bk[:, 0:256], (bkey,), ("S0",))
            else:
                stt(S[:, h, :], S[:, h, :], Ep[:, h * 128 + 127:h * 128 + 128], bk[:, 0:256], ALU.mult, ALU.add,
                    ("S", "Ep", bkey), ("S",))
        if is_meta:
            return
        cp(Sb[:], S[:], ("S",), ("Sb",), eng="act")

        if stop == "E":
            dump(s, t, [(yb[:], "yb", 1024)])
            return
        transposes(yT[:].rearrange("p a b -> p (a b)"), "yT", ya, "ya", 8)
        for j in range(2):
            bk, bkey = proj_tm("oa", j * 512, (j + 1) * 512, yT, "yT")
            tt(mix[:, j * 512:(j + 1) * 512], bk[:, :], gas[:, j * 512:(j + 1) * 512], ALU.mult, (bkey, "gas"),
               ("mix",))
        transposes(yT[:].rearrange("p a b -> p (a b)"), "yT", yb, "yb", 8)
        for j in range(2):
            bk, bkey = proj_tm("ob", j * 512, (j + 1) * 512, yT, "yT")
            tt(gsg[:, j * 512:(j + 1) * 512], bk[:, :], gbs[:, j * 512:(j + 1) * 512], ALU.mult, (bkey, "gbs"),
               ("gsg",))
        tt(mixb[:], mix[:], gsg[:], ALU.add, ("mix", "gsg"), ("mixb",))
        transposes(yT[:].rearrange("p a b -> p (a b)"), "yT", mixb, "mixb", 8)
        for j in range(2):
            bk, bkey = proj_tm("out", j * 512, (j + 1) * 512, yT, "yT")
            tt(X[:, j * 512:(j + 1) * 512], bk[:, :], X[:, j * 512:(j + 1) * 512], ALU.add, (bkey, xk), (xk,))

        if stop == "F":
            dump(s, t, [(X[:], "xt0", 1024)])
            return
        act(junk[:], X[:], AF.Square, (xk,), ("st1g",), accum_out=st1[:, 48:49])
        rstd_from_ss(st1[:, 48:49], st1[:, 48:49], 1024.0, ("st1g",), ("st1g",))
        stt(n2b[:], X[:], st1[:, 48:49], G_FFN, ALU.mult, ALU.mult, (xk, "st1g", "gbt"), ("n2b",))
        transposes(yT[:].rearrange("p a b -> p (a b)"), "yT", n2b, "n2b", 8)
        for j in range(4):
            wsl, wk, _ = load_w("pq", j * 512, (j + 1) * 512)
            bk, bkey = bank_mm()
            for c in range(4):
                for kc in range(8):
                    mm(bk[:, c * 128:(c + 1) * 128], wsl[:, kc, c * 128:(c + 1) * 128], yT[:, kc, :], kc == 0, kc == 7,
                       ("yT", wk), (bkey,))
            cp(pqT[:].rearrange("p a b -> p (a b)"), bk[:, :], (bkey,), ("pqT",), eng="act")
            bk, bkey = bank_mm()
            for c in range(4):
                hp = 4 * j + c
                mm(bk[:, c * 128:(c + 1) * 128], pqT[:, c, :], keysT[:, (hp % 2) * 128:(hp % 2 + 1) * 128], True, True,
                   ("pqT", "keysT"), (bkey,))
            for c in range(4):
                hp = 4 * j + c
                sv = bk[:, c * 128:(c + 1) * 128]
                P.op("dve", (lambda hh, vv: (lambda e: e.max(out=tsv[:, hh, 0:8], in_=vv)))(hp, sv), (bkey,), ("tsv",))
                P.op("dve", (lambda hh, vv: (lambda e: e.max_index(out=tiu[:, hh, 0:8], in_max=tsv[:, hh, 0:8],
                                                                   in_values=vv)))(hp, sv), (bkey, "tsv"), ("tiu",))
                P.op("dve", (lambda hh, vv, cc: (lambda e: e.match_replace(
                    out=ss2[:, cc, :], in_to_replace=tsv[:, hh, 0:8], in_values=vv, imm_value=-1e30)))(hp, sv, c),
                    (bkey, "tsv"), ("ss2",))
                P.op("dve", (lambda hh, cc: (lambda e: e.max(out=tsv[:, hh, 8:16], in_=ss2[:, cc, :])))(hp, c),
                     ("ss2",), ("tsv",))
                P.op("dve", (lambda hh, cc: (lambda e: e.max_index(out=tiu[:, hh, 8:16], in_max=tsv[:, hh, 8:16],
                                                                   in_values=ss2[:, cc, :])))(hp, c), ("ss2", "tsv"),
                     ("tiu",))
        cp(tif[:], tiu[:], ("tiu",), ("tif",))
        ts4 = tsv[:].rearrange("p (h two) k -> p h two k", two=2)
        ti4 = tif[:].rearrange("p (h two) k -> p h two k", two=2)
        tsc(ti4[:, :, 0, :], ti4[:, :, 0, :], 128.0, None, ALU.mult, None, ("tif",), ("tif",))
        for h in range(8):
            cs = cands[:].rearrange("p (a b) -> p a b", a=16)
            ci = candi[:].rearrange("p (a b) -> p a b", a=16)
            tt(cs, ts4[:, h, 0, :].unsqueeze(2).to_broadcast([128, 16, 16]),
               ts4[:, h, 1, :].unsqueeze(1).to_broadcast([128, 16, 16]), ALU.add, ("tsv",), ("cands",))
            tt(ci, ti4[:, h, 0, :].unsqueeze(2).to_broadcast([128, 16, 16]),
               ti4[:, h, 1, :].unsqueeze(1).to_broadcast([128, 16, 16]), ALU.add, ("tif",), ("candi",))
            P.op("dve", (lambda hh: (lambda e: e.max(out=bsv[:, hh, 0:8], in_=cands[:])))(h), ("cands",), ("bsv",))
            P.op("dve", (lambda hh: (lambda e: e.match_replace(out=cands2[:], in_to_replace=bsv[:, hh, 0:8],
                                                               in_values=cands[:], imm_value=-1e30)))(h),
                 ("cands", "bsv"), ("cands2",))
            P.op("dve", (lambda hh: (lambda e: e.max(out=bsv[:, hh, 8:16], in_=cands2[:])))(h), ("cands2",), ("bsv",))
            for k in range(16):
                stt(junk2[:, 0:256], cands[:], bsv[:, h, k:k + 1], candi[:], ALU.is_equal, ALU.mult,
                    ("cands", "bsv", "candi"), ("idxf",), accum_out=idxf[:, h * 16 + k:h * 16 + k + 1])
        tsc(idxf[:], idxf[:], 16383.0, 0.0, ALU.min, ALU.max, ("idxf",), ("idxf",))
        cp(idxi[:], idxf[:], ("idxf",), ("idxi",))
        b3 = bsv[:]
        g3 = gate[:].rearrange("p (h k) -> p h k", h=8)
        tt(g3, b3, b3[:, :, 0:1].to_broadcast([128, 8, 16]), ALU.subtract, ("bsv",), ("gate",))
        act(gate[:], gate[:], AF.Exp, ("gate",), ("gate",))
        red(st1[:, 50:58], g3, ALU.add, ("gate",), ("st1h",))
        recip(st1[:, 50:58], st1[:, 50:58], ("st1h",), ("st1h",))
        tt(g3, g3, st1[:, 50:58].unsqueeze(2).to_broadcast([128, 8, 16]), ALU.mult, ("gate", "st1h"), ("gate",))
        if stop == "G":
            dump(s, t, [(idxf[:], "idxf", 128), (gate[:], "gate", 128)])
            return
        for j in range(128):
            ui = rot("uv", NUV)
            P.dma("pool", (lambda uu, jj: (lambda e: e.indirect_dma_start(
                out=uvb[uu][:, :], out_offset=None, in_=uvs,
                in_offset=bass.IndirectOffsetOnAxis(ap=idxi[:, jj:jj + 1], axis=0))))(ui, j),
                ("idxi", "uvs"), (f"uvb{ui}",))
            stt(junk2[:], uvb[ui][:, 0:1024], 1.0, n2b[:], ALU.mult, ALU.mult, (f"uvb{ui}", "n2b"), (f"actv{j}",),
                accum_out=actv[:, j:j + 1])
            act(glv[:, j:j + 1], actv[:, j:j + 1], AF.Gelu, (f"actv{j}",), (f"glv{j}",))
            di = rot("dg", ND)
            tsc(dgb[di][:], identb[:], glv[:, j:j + 1], gate[:, j:j + 1], ALU.mult, ALU.mult,
                ("identb", f"glv{j}", "gate"), (f"dgb{di}",))
            mm(pf[4][:, :], dgb[di][:], uvb[ui][:, 1024:1536], j == 0, j == 127, (f"dgb{di}", f"uvb{ui}"), ("pf4",))
            mm(pf[5][:, :], dgb[di][:], uvb[ui][:, 1536:2048], j == 0, j == 127, (f"dgb{di}", f"uvb{ui}"), ("pf5",))
        tt(X[:, 0:512], pf[4][:, :], X[:, 0:512], ALU.add, ("pf4", xk), (xk,))
        tt(X[:, 512:1024], pf[5][:, :], X[:, 512:1024], ALU.add, ("pf5", xk), (xk,))
        dma_sp(y[s, (t - 1) * 128:t * 128, :], X[:], (xk,), ("y",), final=True)

    oneb = sb("oneb", [128, 1])
    P.op("dve", lambda e: e.memset(oneb[:], 1.0), (), ("oneb",))

    if stop not in ("M0", "P1", "P2"):
        tile(None, 0, 0)
    par = 1
    for s in range(nseq):
        cp(S[:], S0[:], ("S0",), ("S",))
        cp(Sb[:], S0[:], ("S0",), ("Sb",), eng="act")
        for t in range(1, ntile + 1):
            if stop in ("M0", "P1", "P2"):
                dump(s, t, [])
                continue
            tile(s, t, par)
            par ^= 1
    P.flush()
    stack.close()
    return nc


def _host_consts():
    idx = np.arange(128)
    ident = np.eye(128, dtype=np.float32)
    s_ = idx[:, None]
    t_ = idx[None, :]
    tri = np.where(s_ <= t_, -1.0 / 16.0, 0.0).astype(np.float32)
    sut = np.where(s_ > t_, -1.0 / 16.0, 0.0).astype(np.float32)
    trim = np.where((s_ <= t_) & (s_ < 16), -1.0 / 16.0, 0.0).astype(np.float32)
    sutm = np.where((s_ > t_) & (s_ < 16), -1.0 / 16.0, 0.0).astype(np.float32)
    mut = np.where(s_ <= t_, 1.0, 0.0).astype(np.float32)
    cst = np.concatenate([ident, tri, sut, trim, sutm, mut, mut, mut, mut], axis=1)
    inv_freq = (10000.0 ** (-np.arange(32, dtype=np.float32) / 32.0)).astype(np.float32)
    rope = np.zeros((NKT, 128, 64), np.float32)
    for t in range(NKT):
        pos = (np.arange(128) if t == 0 else 16 + 128 * (t - 1) + np.arange(128)).astype(np.float32)
        ang = pos[:, None] * inv_freq[None, :]
        rope[t, :, 0:32] = np.cos(ang)
        rope[t, :, 32:64] = np.sin(ang)
    return np.ascontiguousarray(cst), rope


_CACHE = {}


def _run(inputs, nseq, ntile, ncores, stop=None):
    f = lambda a: np.ascontiguousarray(np.asarray(a, dtype=np.float32))
    x = f(inputs["x"])
    cst, rope = _host_consts()
    xm = np.zeros((128, 1024), np.float32)
    xm[0:16] = f(inputs["meta"])
    gcol = np.concatenate([f(inputs["norm_mix"])[0].reshape(8, 128).T, f(inputs["mla_q_norm"])[0].reshape(3, 128).T,
                           f(inputs["mla_kv_norm"])[0].reshape(2, 128).T], axis=1)
    gvec = np.concatenate([f(inputs["qn_nope"])[0], f(inputs["qn_pe"])[0], f(inputs["kn_nope"])[0],
                           f(inputs["kn_pe"])[0], f(inputs["gla_norm"])[0], f(inputs["norm_ffn"])[0]])
    gb = np.ascontiguousarray(np.broadcast_to(gvec[None, :], (128, gvec.shape[0])))
    wa2 = np.concatenate([f(inputs["gla_w_a2"])[0], f(inputs["gla_b_a"]), np.zeros((15, 512), np.float32)], axis=0)
    keys = f(inputs["peer_keys"])[0]
    keysT = np.ascontiguousarray(keys.transpose(2, 0, 1).reshape(128, 256))
    uvt = np.ascontiguousarray(np.concatenate([f(inputs["peer_u"])[0], f(inputs["peer_v"])[0]], axis=1))
    common = {
        "xm": xm, "w_in": f(inputs["w_in"])[0], "w_uq": f(inputs["mla_w_uq"])[0], "w_ukv": f(inputs["mla_w_ukv"])[0],
        "w_oa": f(inputs["w_o_mla"])[0], "w_ob": f(inputs["w_o_gla"])[0], "w_out": f(inputs["w_out"])[0],
        "w_pq": f(inputs["peer_w_q"])[0], "gcol": np.ascontiguousarray(gcol), "gb": gb, "wa2": np.ascontiguousarray(wa2),
        "keysT": keysT, "uv": (uvt if stop in (None, "H") else uvt[:128]), "rope": rope, "cst": cst,
    }
    key = (nseq, ntile, stop)
    if key not in _CACHE:
        _CACHE[key] = build(nseq, ntile, stop)
    nc = _CACHE[key]
    in_maps = []
    for c in range(ncores):
        m = dict(common)
        m["x"] = np.ascontiguousarray(x[c * nseq:(c + 1) * nseq])
        in_maps.append(m)
    import os
    res = run_bass_kernel_spmd(nc, in_maps, core_ids=list(range(ncores)), trace=bool(os.environ.get("DBGTRACE")))
    if os.environ.get("DBGTRACE"):
        print("EXEC_NS", res.exec_time_ns)
    return np.concatenate([np.asarray(r["y"]) for r in res.results], axis=0)


def kernel(**inputs):
    out = _run(inputs, 2, 16, NCORES)
    return out.astype(np.float32)
```

```python
import numpy as np
from contextlib import ExitStack
import concourse.bass as bass
import concourse.mybir as mybir
from concourse.bass_utils import run_bass_kernel_spmd

F32 = mybir.dt.float32
BF16 = mybir.dt.bfloat16
I32 = mybir.dt.int32
U32 = mybir.dt.uint32
AF = mybir.ActivationFunctionType
ALU = mybir.AluOpType
AX = mybir.AxisListType

EPS = 1e-6
NCORES = 8
ENGS = ("pe", "dve", "act", "pool", "sp")
EPOCH = 30000


class Prog:
    def __init__(self, nc, stack):
        self.nc = nc
        self.stack = stack
        self.q = {e: [] for e in ENGS}
        self.cnt = {e: 0 for e in ENGS}
        self.known = {e: {} for e in ENGS}
        self.sems = {}
        self.st = {}
        self.ring = {"sp": [("dma", "sp", i) for i in range(12)], "pool": [("dma", "pool", i) for i in range(16)],
                     "act": [("dma", "act", i) for i in range(4)]}
        self.ring_pos = {k: 0 for k in self.ring}
        self.ring_ev = {}
        self.ring_uses = {}
        self.final = []
        self.alias = {}

    def _x(self, keys):
        out = []
        for k in keys:
            out.extend(self.alias.get(k, (k,)))
        return out

    def _sem(self, key):
        if key not in self.sems:
            name = "s_" + "_".join(str(k) for k in key)
            self.sems[key] = self.stack.enter_context(self.nc.semaphore(name))
        return self.sems[key]

    def _deps(self, r, w):
        r = self._x(r)
        w = self._x(w)
        deps = []
        for k in r:
            s = self.st.get(k)
            if s:
                deps.extend(s[0].values())
        for k in w:
            s = self.st.get(k)
            if s:
                deps.extend(s[0].values())
                deps.extend(s[1].values())
        return deps

    def _wait(self, eng, deps):
        for (sk, val) in deps:
            if eng == "pe" and sk[0] == "pe":
                continue
            if self.known[eng].get(sk, 0) >= val:
                continue
            self.known[eng][sk] = val
            self._sem(sk)
            self.q[eng].append(("wait", sk, val))

    def _record(self, ev, r, w):
        r = self._x(r)
        w = self._x(w)
        for k in w:
            s = self.st.setdefault(k, [{}, {}])
            s[0][ev[0]] = ev
            s[1] = {}
        for k in r:
            s = self.st.setdefault(k, [{}, {}])
            s[1][ev[0]] = ev

    def op(self, eng, fn, r=(), w=()):
        self._wait(eng, self._deps(r, w))
        self.cnt[eng] += 1
        c = self.cnt[eng]
        ep = (c - 1) // EPOCH
        ev = ((eng, ep), c - ep * EPOCH)
        self._sem(ev[0])
        self.q[eng].append(("op", fn, ev[0], 1))
        self._record(ev, r, w)
        return ev

    def dma(self, eng, fn, r=(), w=(), final=False):
        ring = self.ring[eng]
        sk = ring[self.ring_pos[eng] % len(ring)]
        self.ring_pos[eng] += 1
        deps = self._deps(r, w)
        if sk in self.ring_ev:
            deps.append(self.ring_ev[sk])
        self._wait(eng, deps)
        self.ring_uses[sk] = self.ring_uses.get(sk, 0) + 1
        ev = (sk, 16 * self.ring_uses[sk])
        self.ring_ev[sk] = ev
        self._sem(sk)
        self.q[eng].append(("op", fn, sk, 16))
        self._record(ev, r, w)
        if final:
            self.final.append(ev)
        return ev

    def flush(self):
        nc = self.nc
        self._wait("sp", self.final)
        self._wait("sp", list(self.ring_ev.values()))
        print("PROG counts", self.cnt, {k: len(v) for k, v in self.q.items()}, flush=True)
        q = self.q
        sems = self.sems

        def run(name, e):
            for it in q[name]:
                if it[0] == "wait":
                    e.wait_ge(sems[it[1]], it[2])
                else:
                    ins = it[1](e)
                    ins.then_inc(sems[it[2]], it[3])

        with nc.Block() as block:
            @block.tensor
            def _(e):
                run("pe", e)

            @block.vector
            def _(e):
                run("dve", e)

            @block.scalar
            def _(e):
                run("act", e)

            @block.gpsimd
            def _(e):
                run("pool", e)

            @block.sync
            def _(e):
                run("sp", e)


C_CQ, C_CKV, C_KPE, C_GQ, C_GK, C_GV, C_GA, C_GG, C_GTA, C_GTB, C_END = (
    0, 384, 640, 704, 1216, 1728, 2752, 2768, 3792, 4816, 5840)
NKT = 17


def build(nseq, ntile, stop=None):
    nc = bass.Bass("TRN2", target_bir_lowering=False)
    stack = ExitStack()
    P = Prog(nc, stack)

    def din(name, shape, dt=F32):
        return nc.dram_tensor(name, list(shape), dt, kind="ExternalInput").ap()

    x = din("x", [nseq, 2048, 1024])
    xm = din("xm", [128, 1024])
    Wd = {
        "in": (din("w_in", [1024, 5840]), 8, 5840, 0),
        "uq": (din("w_uq", [384, 1536]), 3, 1536, 8),
        "ukv": (din("w_ukv", [256, 2048]), 2, 2048, 11),
        "oa": (din("w_oa", [1024, 1024]), 8, 1024, None),
        "ob": (din("w_ob", [1024, 1024]), 8, 1024, None),
        "out": (din("w_out", [1024, 1024]), 8, 1024, None),
        "pq": (din("w_pq", [1024, 2048]), 8, 2048, None),
    }
    gcol_d = din("gcol", [128, 13])
    gb_d = din("gb", [128, 1664])
    wa2_d = din("wa2", [32, 512])
    keysT_d = din("keysT", [128, 256])
    NEXP = 16384 if stop in (None, "H") else 128
    uv = din("uv", [NEXP, 2048])
    rope_d = din("rope", [NKT, 128, 64])
    cst_d = din("cst", [128, 1152])
    y = nc.dram_tensor("y", [nseq, 2048, 1024], F32, kind="ExternalOutput").ap()
    uvs = nc.dram_tensor("uvs", [NEXP, 2048], BF16, kind="Internal").ap()
    ws = {k: nc.dram_tensor("ws_" + k, [128, v[1], v[2]], BF16, kind="Internal").ap() for k, v in Wd.items()}

    def sb(name, shape, dt=F32):
        return stack.enter_context(nc.sbuf_tensor("sb_" + name, list(shape), dt))

    def ps(name, shape, dt=F32):
        return stack.enter_context(nc.psum_tensor("ps_" + name, list(shape), dt))

    cst = sb("cst", [128, 1152])
    IDENT = cst[:, 0:128]
    TRI = cst[:, 128:256]
    SUT = cst[:, 256:384]
    TRIM = cst[:, 384:512]
    SUTM = cst[:, 512:640]
    MASKUT = cst[:, 640:1152]
    identb = sb("identb", [128, 128], BF16)
    gcol = sb("gcol", [128, 13])
    gbt = sb("gbt", [128, 1664])
    G_QN, G_QP, G_KN, G_KP, G_GLA, G_FFN = (gbt[:, 0:128], gbt[:, 128:192], gbt[:, 192:320], gbt[:, 320:384],
                                          gbt[:, 384:640], gbt[:, 640:1664])
    wa2 = sb("wa2", [32, 512])
    keysT = sb("keysT", [128, 256])
    ropet = sb("ropet", [128, NKT, 64])

    KT = sb("KT", [128, 8, NKT * 128], BF16)
    KPT = sb("KPT", [128, NKT * 128], BF16)
    VC = sb("VC", [128, NKT, 8, 132], BF16)
    S = sb("S", [128, 4, 256])
    S0 = sb("S0", [128, 4, 256])
    Sb = sb("Sb", [128, 4, 256], BF16)

    NWS = 3
    wslot = [sb(f"wslot{i}", [128, 8, 512], BF16) for i in range(NWS)]
    NUV = 12
    ND = 8

    AW = 16896
    arena = sb("arena", [128, AW])

    def carve(name, off, shape, dt=F32):
        per = 1
        for d in shape[1:]:
            per *= d
        words = per if dt in (F32, I32, U32) else per // 2
        assert off + words <= AW, name
        v = arena[:, off:off + words]
        if dt != F32:
            v = v.bitcast(dt)
        if len(shape) == 3:
            v = v.rearrange("p (a b) -> p a b", a=shape[1])
        P.alias[name] = tuple(("ar", g) for g in range(off // 128, (off + words - 1) // 128 + 1))
        return v

    lat = carve("lat", 0, [128, 704])
    qsb = carve("qsb", 704, [128, 8, 192])
    kvs = carve("kvs", 2240, [128, 8, 128])
    knn = carve("knn", 3264, [128, 8, 128], BF16)
    qnn = carve("qnn", 3776, [128, 8, 128], BF16)
    qpe = carve("qpe", 4288, [128, 8, 64])
    qpr = carve("qpr", 4800, [128, 8, 64], BF16)
    cqn = carve("cqn", 5056, [128, 384], BF16)
    ckn = carve("ckn", 5248, [128, 256], BF16)
    cqT = carve("cqT", 5376, [128, 3, 128], BF16)
    ckT = carve("ckT", 5568, [128, 2, 128], BF16)
    kp = carve("kp", 5696, [128, 64])
    kp2 = carve("kp2", 5760, [128, 128], BF16)
    rt = carve("rt", 5824, [128, 8, 32])
    rt2 = carve("rt2", 6080, [128, 8, 32])
    e1 = carve("e1", 0, [128, 512])
    lsp = carve("lsp", 512, [128, 512])
    Ep = carve("Ep", 1024, [128, 512])
    En = carve("En", 1536, [128, 512])
    QtT = carve("QtT", 2048, [128, 4, 128], BF16)
    KtT = carve("KtT", 2304, [128, 4, 128], BF16)
    Khat = carve("Khat", 2560, [128, 512], BF16)
    At = carve("At", 2816, [128, 512], BF16)
    n2b = carve("n2b", 0, [128, 1024], BF16)
    pqT = carve("pqT", 512, [128, 4, 128])
    ss2 = carve("ss2", 1024, [128, 4, 128])
    tsv = carve("tsv", 1536, [128, 16, 16])
    tiu = carve("tiu", 1792, [128, 16, 16], U32)
    tif = carve("tif", 2048, [128, 16, 16])
    cands = carve("cands", 2304, [128, 256])
    cands2 = carve("cands2", 2560, [128, 256])
    candi = carve("candi", 2816, [128, 256])
    dgb = [carve(f"dgb{i}", 3072 + 64 * i, [128, 128], BF16) for i in range(ND)]
    uvb = [carve(f"uvb{i}", 3584 + 1024 * i, [128, 2048], BF16) for i in range(NUV)]
    stg = [carve(f"stg{i}", 512 * i, [128, 512]) for i in range(2)]
    stgb = [carve(f"stgb{i}", 1024 + 256 * i, [128, 512], BF16) for i in range(2)]

    ustg = [carve(f"ustg{i}", 2048 * i, [128, 2048]) for i in range(2)]
    ustgb = [carve(f"ustgb{i}", 4096 + 1024 * i, [128, 2048], BF16) for i in range(2)]

    xt = [sb("xt0", [128, 1024])]
    junk = sb("junk", [128, 1024], BF16)
    junk2 = sb("junk2", [128, 1024], BF16)
    st1 = sb("st1", [128, 64])
    gaT = sb("gaT", [32, 128])
    pm = [sb(f"pm{i}", [16, 128], BF16) for i in range(2)]
    yT = sb("yT", [128, 8, 128], BF16)
    o2 = 7168
    nb = carve("nb", o2, [128, 1024], BF16)
    nT = carve("nT", o2 + 512, [128, 8, 128], BF16)
    gqT = carve("gqT", o2 + 1024, [128, 4, 128])
    gkT = carve("gkT", o2 + 1536, [128, 4, 128])
    gktm = carve("gktm", o2 + 2048, [128, 512])
    gv = carve("gv", o2 + 2560, [128, 1024], BF16)
    sg = carve("sg", o2 + 3072, [128, 1024], BF16)
    gas = carve("gas", o2 + 3584, [128, 1024], BF16)
    gbs = carve("gbs", o2 + 4096, [128, 1024], BF16)
    qnT = carve("qnT", o2 + 4608, [128, 8, 128], BF16)
    qpT = carve("qpT", o2 + 5120, [128, 4, 128], BF16)
    pb = [carve(f"pb{i}", o2 + 5376 + 256 * i, [128, 512], BF16) for i in range(3)]
    ya = carve("ya", o2 + 6144, [128, 1024], BF16)
    yb = carve("yb", o2 + 6656, [128, 1024], BF16)
    MG = carve("MG", o2 + 7168, [128, 2048])
    mixb = carve("mixb", o2 + 9216, [128, 1024], BF16)
    mix = MG[:, 0:1024]
    gsg = MG[:, 1024:2048]
    sq = MG[:, 0:1536].rearrange("p (a b) -> p a b", a=8)
    for nm in ("mix", "gsg", "sq"):
        P.alias[nm] = P.alias["MG"]
    bsv = sb("bsv", [128, 8, 16])
    idxf = sb("idxf", [128, 128])
    idxi = sb("idxi", [128, 128], I32)
    gate = sb("gate", [128, 128])
    actv = sb("actv", [128, 128])
    glv = sb("glv", [128, 128])

    pf = [ps(f"pf{i}", [128, 512]) for i in range(6)]
    pt = [ps(f"pt{i}", [128, 1024], BF16) for i in range(2)]
    cnt = {"mm": 0, "s": 0, "t": 0, "w": 0, "pb": 0, "pm": 0, "stg": 0, "uv": 0, "dg": 0}

    def rot(kind, n):
        v = cnt[kind] % n
        cnt[kind] += 1
        return v

    def bank_mm():
        i = rot("mm", 2)
        return pf[i], f"pf{i}"

    def bank_s():
        i = 2 + rot("s", 2)
        return pf[i], f"pf{i}"

    def bank_t():
        i = rot("t", 2)
        return pt[i], f"pt{i}"

    def dma_sp(out, in_, r, w, final=False):
        P.dma("sp", lambda e: e.dma_start(out=out, in_=in_), r, w, final)

    def mm(out, lhsT, rhs, start, stop, r, w):
        P.op("pe", lambda e: e.matmul(out, lhsT, rhs, start=start, stop=stop), r, w)

    def tr(out, in_, idn, r, w):
        P.op("pe", lambda e: e.transpose(out, in_, idn), r, w)

    def act(out, in_, func, r, w, bias=None, scale=None, accum_out=None):
        kw = {}
        if bias is not None:
            kw["bias"] = bias
        if scale is not None:
            kw["scale"] = scale
        if accum_out is not None:
            kw["accum_out"] = accum_out
        P.op("act", lambda e: e.activation(out=out, in_=in_, func=func, **kw), r, w)

    def tt(out, in0, in1, op, r, w, eng="dve"):
        P.op(eng, lambda e: e.tensor_tensor(out=out, in0=in0, in1=in1, op=op), r, w)

    def tsc(out, in0, s1, s2, op0, op1, r, w, eng="dve", accum_out=None):
        if op1 is None:
            P.op(eng, lambda e: e.tensor_scalar(out=out, in0=in0, scalar1=s1, scalar2=None, op0=op0), r, w)
        else:
            P.op(eng, lambda e: e.tensor_scalar(out=out, in0=in0, scalar1=s1, scalar2=s2, op0=op0, op1=op1,
                                                accum_out=accum_out), r, w)

    def stt(out, in0, scalar, in1, op0, op1, r, w, accum_out=None):
        P.op("dve", lambda e: e.scalar_tensor_tensor(out=out, in0=in0, scalar=scalar, in1=in1, op0=op0, op1=op1,
                                                     accum_out=accum_out), r, w)

    def cp(out, in_, r, w, eng="dve"):
        if eng == "act":
            P.op("act", lambda e: e.copy(out=out, in_=in_), r, w)
        else:
            P.op(eng, lambda e: e.tensor_copy(out=out, in_=in_), r, w)

    def recip(out, in_, r, w):
        P.op("dve", lambda e: e.reciprocal(out=out, in_=in_), r, w)

    def red(out, in_, op, r, w):
        P.op("dve", lambda e: e.tensor_reduce(out=out, in_=in_, axis=AX.X, op=op), r, w)

    def rstd_from_ss(dst, ssum, n, r, w):
        act(dst, ssum, AF.Sqrt, tuple(r) + ("epsb",), w, bias=epsb[:, 0:1], scale=1.0 / n)
        recip(dst, dst, w, w)

    epsb = sb("epsb", [128, 1])
    P.op("dve", lambda e: e.memset(epsb[:], EPS), (), ("epsb",))

    import os
    SK = os.environ.get("DBGSKIP", "").split(",")
    if "cst" not in SK:
        dma_sp(cst[:], cst_d, (), ("cst",))
    if "gcol" not in SK:
        dma_sp(gcol[:], gcol_d, (), ("gcol",))
    if "gbt" not in SK:
        dma_sp(gbt[:], gb_d, (), ("gbt",))
    if "wa2" not in SK:
        dma_sp(wa2[:], wa2_d, (), ("wa2",))
    if "keysT" not in SK:
        dma_sp(keysT[:], keysT_d, (), ("keysT",))
    if "rope" not in SK:
        dma_sp(ropet[:], rope_d.rearrange("t p c -> p t c"), (), ("ropet",))
    if "ms" not in SK:
        cp(identb[:], IDENT, ("cst",), ("identb",))
        P.op("dve", lambda e: e.memset(gaT[:], 1.0), (), ("gaT",))
        P.op("dve", lambda e: e.memset(VC[:], 1.0), (), tuple(f"V{t}" for t in range(NKT)))
        P.op("dve", lambda e: e.memset(S0[:], 0.0), (), ("S0",))

    for name, (wd, kc_n, ncol, gc) in Wd.items():
        if stop == "P1" or (stop == "P2" and name != "ukv"):
            continue
        for kc in range(kc_n):
            for c0 in range(0, ncol, 512):
                c1 = min(ncol, c0 + 512)
                i = rot("stg", 2)
                dma_sp(stg[i][:, 0:c1 - c0], wd[kc * 128:(kc + 1) * 128, c0:c1], (), (f"stg{i}",))
                if gc is None:
                    cp(stgb[i][:, 0:c1 - c0], stg[i][:, 0:c1 - c0], (f"stg{i}",), (f"stgb{i}",),
                       eng=("act" if (cnt["stg"] % 4 < 2) else "dve"))
                else:
                    tsc(stgb[i][:, 0:c1 - c0], stg[i][:, 0:c1 - c0], gcol[:, gc + kc:gc + kc + 1], None, ALU.mult,
                        None, (f"stg{i}", "gcol"), (f"stgb{i}",))
                dma_sp(ws[name][:, kc, c0:c1], stgb[i][:, 0:c1 - c0], (f"stgb{i}",), (f"ws_{name}",))

    if stop in (None, "H"):
        for i in range(NEXP // 128):
            k = i % 2
            dma_sp(ustg[k][:], uv[i * 128:(i + 1) * 128, :], (), (f"ustg{k}",))
            cp(ustgb[k][:], ustg[k][:], (f"ustg{k}",), (f"ustgb{k}",), eng=("act", "dve", "pool")[i % 3])
            dma_sp(uvs[i * 128:(i + 1) * 128, :], ustgb[k][:], (f"ustgb{k}",), ("uvs",))

    def load_w(name, c0, c1):
        kc_n = Wd[name][1]
        i = rot("w", NWS)
        dma_sp(wslot[i][:, 0:kc_n, 0:c1 - c0], ws[name][:, :, c0:c1], (f"ws_{name}",), (f"wslot{i}",))
        return wslot[i], f"wslot{i}", kc_n

    def proj_tm(name, c0, c1, lhs, lhs_key):
        wsl, wk, kc_n = load_w(name, c0, c1)
        bk, bkey = bank_mm()
        for kc in range(kc_n):
            mm(bk[:, 0:c1 - c0], lhs[:, kc, :], wsl[:, kc, 0:c1 - c0], kc == 0, kc == kc_n - 1, (lhs_key, wk), (bkey,))
        return bk, bkey

    def transposes(dst, dst_key, src, src_key, nblk, eng="dve"):
        bk, bkey = bank_t()
        for j in range(nblk):
            tr(bk[:, j * 128:(j + 1) * 128], src[:, j * 128:(j + 1) * 128], identb[:], (src_key, "identb"), (bkey,))
        cp(dst, bk[:, 0:nblk * 128], (bkey,), (dst_key,), eng=eng)

    SCALE_ATT = 192.0 ** -0.5
    SCALE_GLA = 128.0 ** -0.5

    def rope_apply(dst, src, t, nh, r, w):
        cos = ropet[:, t, 0:32].unsqueeze(1).to_broadcast([128, nh, 32])
        sin = ropet[:, t, 32:64].unsqueeze(1).to_broadcast([128, nh, 32])
        x1 = src[:, :, 0:32]
        x2 = src[:, :, 32:64]
        a = rt[:, 0:nh, :]
        b = rt2[:, 0:nh, :]
        tt(a, x1, cos, ALU.mult, r + ("ropet",), ("rt",))
        tt(b, x2, sin, ALU.mult, r + ("ropet",), ("rt2",))
        tt(dst[:, :, 0:32], a, b, ALU.subtract, ("rt", "rt2"), w)
        tt(a, x2, cos, ALU.mult, r + ("ropet",), ("rt",))
        tt(b, x1, sin, ALU.mult, r + ("ropet",), ("rt2",))
        tt(dst[:, :, 32:64], a, b, ALU.add, ("rt", "rt2"), w)

    def dump(s, t, items):
        X = xt[0]
        o = 0
        for ap, key, n in items:
            cp(X[:ap.shape[0], o:o + n], ap, (key,), ("xt0",))
            o += n
        dma_sp(y[s, (t - 1) * 128:t * 128, :], X[:], ("xt0",), ("y",), final=True)

    def tile(s, t, par):
        is_meta = s is None
        X = xt[0]
        xk = "xt0"
        if is_meta:
            dma_sp(X[:], xm, (), (xk,))
        else:
            dma_sp(X[:], x[s, (t - 1) * 128:t * 128, :], (), (xk,))
        act(junk[:], X[:], AF.Square, (xk,), ("st1a",), accum_out=st1[:, 0:1])
        rstd_from_ss(st1[:, 0:1], st1[:, 0:1], 1024.0, ("st1a",), ("st1a",))
        tsc(nb[:], X[:], st1[:, 0:1], None, ALU.mult, None, (xk, "st1a"), ("nb",))
        transposes(nT[:].rearrange("p a b -> p (a b)"), "nT", nb, "nb", 8)

        if stop == "A":
            if not is_meta:
                dump(s, t, [(nb[:], "nb", 1024)])
            return
        bk, bkey = proj_tm("in", 0, 512, nT, "nT")
        cp(lat[:, 0:512], bk[:, 0:512], (bkey,), ("lat",), eng="act")
        bk, bkey = proj_tm("in", 512, 704, nT, "nT")
        cp(lat[:, 512:704], bk[:, 0:192], (bkey,), ("lat",), eng="act")
        wsl, wk, _ = load_w("in", C_GQ, C_GQ + 512)
        bk, bkey = bank_mm()
        for h in range(4):
            for kc in range(8):
                mm(bk[:, h * 128:(h + 1) * 128], wsl[:, kc, h * 128:(h + 1) * 128], nT[:, kc, :], kc == 0, kc == 7,
                   ("nT", wk), (bkey,))
        cp(gqT[:].rearrange("p a b -> p (a b)"), bk[:, :], (bkey,), ("gqT",), eng="act")
        wsl, wk, _ = load_w("in", C_GK, C_GK + 512)
        bk, bkey = bank_mm()
        for h in range(4):
            for kc in range(8):
                mm(bk[:, h * 128:(h + 1) * 128], wsl[:, kc, h * 128:(h + 1) * 128], nT[:, kc, :], kc == 0, kc == 7,
                   ("nT", wk), (bkey,))
        cp(gkT[:].rearrange("p a b -> p (a b)"), bk[:, :], (bkey,), ("gkT",), eng="act")
        bk, bkey = bank_mm()
        for kc in range(8):
            mm(bk[:, :], nT[:, kc, :], wsl[:, kc, :], kc == 0, kc == 7, ("nT", wk), (bkey,))
        cp(gktm[:], bk[:, :], (bkey,), ("gktm",), eng="dve")
        for j in range(2):
            bk, bkey = proj_tm("in", C_GV + j * 512, C_GV + (j + 1) * 512, nT, "nT")
            cp(gv[:, j * 512:(j + 1) * 512], bk[:, :], (bkey,), ("gv",), eng=("act" if j == 0 else "dve"))
        wsl, wk, _ = load_w("in", C_GA, C_GA + 16)
        bk, bkey = bank_mm()
        for kc in range(8):
            mm(bk[0:16, 0:128], wsl[:, kc, 0:16], nT[:, kc, :], kc == 0, kc == 7, ("nT", wk), (bkey,))
        cp(gaT[0:16, :], bk[0:16, 0:128], (bkey,), ("gaT",), eng="dve")
        if not is_meta:
            for j in range(2):
                bk, bkey = proj_tm("in", C_GG + j * 512, C_GG + (j + 1) * 512, nT, "nT")
                act(sg[:, j * 512:(j + 1) * 512], bk[:, :], AF.Silu, (bkey,), ("sg",))
            for j in range(2):
                bk, bkey = proj_tm("in", C_GTA + j * 512, C_GTA + (j + 1) * 512, nT, "nT")
                act(gas[:, j * 512:(j + 1) * 512], bk[:, :], AF.Sigmoid, (bkey,), ("gas",))
            for j in range(2):
                bk, bkey = proj_tm("in", C_GTB + j * 512, C_GTB + (j + 1) * 512, nT, "nT")
                act(gbs[:, j * 512:(j + 1) * 512], bk[:, :], AF.Sigmoid, (bkey,), ("gbs",))

        if stop == "B":
            if not is_meta:
                dump(s, t, [(lat[:, :], "lat", 704), (gv[:, 0:320], "gv", 320)])
            return
        tt(sq[:].rearrange("p a b -> p (a b)")[:, 0:704], lat[:, :], lat[:, :], ALU.mult, ("lat",), ("sq",))
        sqf = sq[:].rearrange("p a b -> p (a b)")
        red(st1[:, 1:2], sqf[:, 0:384], ALU.add, ("sq",), ("st1b",))
        red(st1[:, 2:3], sqf[:, 384:640], ALU.add, ("sq",), ("st1b",))
        red(st1[:, 3:4], sqf[:, 640:704], ALU.add, ("sq",), ("st1b",))
        rstd_from_ss(st1[:, 1:2], st1[:, 1:2], 384.0, ("st1b",), ("st1b",))
        rstd_from_ss(st1[:, 2:3], st1[:, 2:3], 256.0, ("st1b",), ("st1b",))
        rstd_from_ss(st1[:, 3:4], st1[:, 3:4], 64.0, ("st1b",), ("st1b",))
        tsc(ckn[:], lat[:, 384:640], st1[:, 2:3], None, ALU.mult, None, ("lat", "st1b"), ("ckn",))
        transposes(ckT[:].rearrange("p a b -> p (a b)"), "ckT", ckn, "ckn", 2)
        stt(kp[:], lat[:, 640:704], st1[:, 3:4], G_KP, ALU.mult, ALU.mult, ("lat", "st1b", "gbt"), ("kp",))
        kpv = kp[:].unsqueeze(1)
        kp2v = kp2[:].rearrange("p (a b) -> p a b", a=2)
        rope_apply(kp2v[:, 0:1, :], kpv, t, 1, ("kp",), ("kp2",))
        cp(kp2[:, 64:128], kp2[:, 0:64], ("kp2",), ("kp2",))
        bk, bkey = bank_t()
        tr(bk[:, 0:128], kp2[:], identb[:], ("kp2", "identb"), (bkey,))
        cp(KPT[:, t * 128:(t + 1) * 128], bk[:, 0:128], (bkey,), (f"KP{t}",))
        if stop == "C1":
            if not is_meta:
                dump(s, t, [])
            return
        for j in range(4):
            bk, bkey = proj_tm("ukv", j * 512, (j + 1) * 512, ckT, "ckT")
            bv = bk[:, :].rearrange("p (h c) -> p h c", h=2)
            ee = "act" if j % 2 == 0 else "dve"
            cp(kvs[:, 2 * j:2 * j + 2, :], bv[:, :, 0:128], (bkey,), ("kvs",), eng=ee)
            cp(VC[:, t, 2 * j:2 * j + 2, 0:128], bv[:, :, 128:256], (bkey,), (f"V{t}",), eng=ee)
        if stop == "C1a":
            if not is_meta:
                dump(s, t, [])
            return
        tt(sq[:, :, 0:128], kvs[:], kvs[:], ALU.mult, ("kvs",), ("sq",))
        red(st1[:, 8:16], sq[:, :, 0:128], ALU.add, ("sq",), ("st1c",))
        rstd_from_ss(st1[:, 8:16], st1[:, 8:16], 128.0, ("st1c",), ("st1c",))
        if stop == "C1b":
            if not is_meta:
                dump(s, t, [])
            return
        tt(kvs[:], kvs[:], st1[:, 8:16].unsqueeze(2).to_broadcast([128, 8, 128]), ALU.mult, ("kvs", "st1c"), ("kvs",))
        tt(knn[:], kvs[:], G_KN.unsqueeze(1).to_broadcast([128, 8, 128]), ALU.mult, ("kvs", "gbt"), ("knn",))
        if stop == "C1c":
            if not is_meta:
                dump(s, t, [])
            return
        bk, bkey = bank_t()
        for h in range(8):
            tr(bk[:, h * 128:(h + 1) * 128], knn[:, h, :], identb[:], ("knn", "identb"), (bkey,))
        cp(KT[:, :, t * 128:(t + 1) * 128], bk[:, :].rearrange("p (h c) -> p h c", h=8), (bkey,), (f"K{t}",), eng="act")

        if stop == "C2":
            if not is_meta:
                dump(s, t, [])
            return
        if not is_meta:
            tsc(cqn[:], lat[:, 0:384], st1[:, 1:2], None, ALU.mult, None, ("lat", "st1b"), ("cqn",))
            transposes(cqT[:].rearrange("p a b -> p (a b)"), "cqT", cqn, "cqn", 3)
            qf = qsb[:].rearrange("p a b -> p (a b)")
            for j in range(3):
                bk, bkey = proj_tm("uq", j * 512, (j + 1) * 512, cqT, "cqT")
                cp(qf[:, j * 512:(j + 1) * 512], bk[:, :], (bkey,), ("qsb",), eng=("act" if j != 1 else "dve"))
            tt(sq[:], qsb[:], qsb[:], ALU.mult, ("qsb",), ("sq",))
            red(st1[:, 16:24], sq[:, :, 0:128], ALU.add, ("sq",), ("st1d",))
            red(st1[:, 24:32], sq[:, :, 128:192], ALU.add, ("sq",), ("st1d",))
            rstd_from_ss(st1[:, 16:24], st1[:, 16:24], 128.0, ("st1d",), ("st1d",))
            rstd_from_ss(st1[:, 24:32], st1[:, 24:32], 64.0, ("st1d",), ("st1d",))
            tt(qsb[:, :, 0:128], qsb[:, :, 0:128], st1[:, 16:24].unsqueeze(2).to_broadcast([128, 8, 128]), ALU.mult,
               ("qsb", "st1d"), ("qsb",))
            tt(qnn[:], qsb[:, :, 0:128], G_QN.unsqueeze(1).to_broadcast([128, 8, 128]), ALU.mult, ("qsb", "gbt"),
               ("qnn",))
            tt(qsb[:, :, 128:192], qsb[:, :, 128:192], st1[:, 24:32].unsqueeze(2).to_broadcast([128, 8, 64]), ALU.mult,
               ("qsb", "st1d"), ("qsb",))
            tt(qpe[:], qsb[:, :, 128:192], G_QP.unsqueeze(1).to_broadcast([128, 8, 64]), ALU.mult, ("qsb", "gbt"),
               ("qpe",))
            rope_apply(qpr[:], qpe[:], t, 8, ("qpe",), ("qpr",))
            transposes(qnT[:].rearrange("p a b -> p (a b)"), "qnT", qnn[:].rearrange("p a b -> p (a b)"), "qnn", 8,
                       eng="act")
            transposes(qpT[:].rearrange("p a b -> p (a b)"), "qpT", qpr[:].rearrange("p a b -> p (a b)"), "qpr", 4,
                       eng="act")

            if stop == "C":
                dump(s, t, [(qnn[:].rearrange("p a b -> p (a b)")[:, 0:512], "qnn", 512),
                            (qpr[:].rearrange("p a b -> p (a b)"), "qpr", 512)])
                return
            kts = list(range(1, t + 1))
            for h in range(8):
                hp0 = (h % 2) * 64
                acc = pf[4 + (h % 2)]
                akey = f"pf{4 + (h % 2)}"
                bk, bkey = bank_s()
                mm(bk[0:16, 0:128], KT[:, h, 0:16], qnT[:, h, :], True, False, ("K0", "qnT"), (bkey,))
                mm(bk[0:16, 0:128], KPT[hp0:hp0 + 64, 0:16], qpT[hp0:hp0 + 64, h // 2, :], False, True,
                   ("KP0", "qpT"), (bkey,))
                mi = rot("pm", 2)
                act(pm[mi][:], bk[0:16, 0:128], AF.Exp, (bkey,), (f"pm{mi}",), scale=SCALE_ATT)
                mm(acc[:, 0:129], pm[mi][:], VC[0:16, 0, h, 0:129], True, False, (f"pm{mi}", "V0"), (akey,))
                for g0 in range(0, len(kts), 4):
                    grp = kts[g0:g0 + 4]
                    bk, bkey = bank_s()
                    for j, kt in enumerate(grp):
                        mm(bk[:, j * 128:(j + 1) * 128], KT[:, h, kt * 128:(kt + 1) * 128], qnT[:, h, :], True, False,
                           (f"K{kt}", "qnT"), (bkey,))
                        mm(bk[:, j * 128:(j + 1) * 128], KPT[hp0:hp0 + 64, kt * 128:(kt + 1) * 128],
                           qpT[hp0:hp0 + 64, h // 2, :], False, True, (f"KP{kt}", "qpT"), (bkey,))
                    pi = rot("pb", 3)
                    n = len(grp) * 128
                    act(pb[pi][:, 0:n], bk[:, 0:n], AF.Exp, (bkey,), (f"pb{pi}",), scale=SCALE_ATT)
                    if grp[-1] == t:
                        j = len(grp) - 1
                        P.op("dve", (lambda pp, jj: (lambda e: e.memset(pp[64:128, jj * 128:jj * 128 + 64], 0.0)))(
                            pb[pi], j), (), (f"pb{pi}",))
                    for j, kt in enumerate(grp):
                        mm(acc[:, 0:129], pb[pi][:, j * 128:(j + 1) * 128], VC[:, kt, h, 0:129], False, kt == t,
                           (f"pb{pi}", f"V{kt}"), (akey,))
                recip(st1[:, 32 + h:33 + h], acc[:, 128:129], (akey,), (f"st1e{h}",))
                tsc(ya[:, h * 128:(h + 1) * 128], acc[:, 0:128], st1[:, 32 + h:33 + h], None, ALU.mult, None,
                    (akey, f"st1e{h}"), ("ya",))

        if stop == "D" and not is_meta:
            dump(s, t, [(ya[:], "ya", 1024)])
            return
        tri = TRIM if is_meta else TRI
        sut = SUTM if is_meta else SUT
        bk, bkey = bank_s()
        mm(bk[:, :], gaT[:, :], wa2[:, :], True, True, ("gaT", "wa2"), (bkey,))
        act(e1[:], bk[:, :], AF.Exp, (bkey,), ("e1",), scale=-1.0)
        act(lsp[:], e1[:], AF.Ln, ("e1", "oneb"), ("lsp",), bias=oneb[:, 0:1])
        bk, bkey = bank_s()
        for h in range(4):
            mm(bk[:, h * 128:(h + 1) * 128], lsp[:, h * 128:(h + 1) * 128], tri, True, True, ("lsp", "cst"), (bkey,))
        act(Ep[:], bk[:, :], AF.Exp, (bkey,), ("Ep",))
        act(En[:], bk[:, :], AF.Exp, (bkey,), ("En",), scale=-1.0)
        bk, bkey = bank_s()
        mm(bk[:, :], sut, lsp[:], True, True, ("lsp", "cst"), (bkey,))
        act(e1[:], bk[:, :], AF.Exp, (bkey,), ("e1",))
        tt(Khat[:], gktm[:], e1[:], ALU.mult, ("gktm", "e1"), ("Khat",))
        tt(KtT[:].rearrange("p a b -> p (a b)"), gkT[:].rearrange("p a b -> p (a b)"), En[:], ALU.mult, ("gkT", "En"),
           ("KtT",))
        if not is_meta:
            stt(QtT[:].rearrange("p a b -> p (a b)"), gqT[:].rearrange("p a b -> p (a b)"), SCALE_GLA, Ep[:], ALU.mult,
                ALU.mult, ("gqT", "Ep"), ("QtT",))
            bk, bkey = bank_s()
            for h in range(4):
                mm(bk[:, h * 128:(h + 1) * 128], KtT[:, h, :], QtT[:, h, :], True, True, ("KtT", "QtT"), (bkey,))
            tt(At[:], bk[:, :], MASKUT, ALU.mult, (bkey, "cst"), ("At",))
            for h in range(4):
                acc = pf[4 + h // 2]
                akey = f"pf{4 + h // 2}"
                o = (h % 2) * 256
                mm(acc[:, o:o + 256], At[:, h * 128:(h + 1) * 128], gv[:, h * 256:(h + 1) * 256], True, False,
                   ("At", "gv"), (akey,))
                mm(acc[:, o:o + 256], QtT[:, h, :], Sb[:, h, :], False, True, ("QtT", "Sb"), (akey,))
            for h in range(4):
                acc = pf[4 + h // 2]
                akey = f"pf{4 + h // 2}"
                o = (h % 2) * 256
                act(junk[:, 0:256], acc[:, o:o + 256], AF.Square, (akey,), ("st1f",),
                    accum_out=st1[:, 40 + h:41 + h])
            rstd_from_ss(st1[:, 40:44], st1[:, 40:44], 256.0, ("st1f",), ("st1f",))
            tt(gsg[:].rearrange("p (h c) -> p h c", h=4), sg[:].rearrange("p (h c) -> p h c", h=4),
               G_GLA.unsqueeze(1).to_broadcast([128, 4, 256]), ALU.mult, ("sg", "gbt"), ("gsg",))
            for h in range(4):
                acc = pf[4 + h // 2]
                akey = f"pf{4 + h // 2}"
                o = (h % 2) * 256
                stt(yb[:, h * 256:(h + 1) * 256], acc[:, o:o + 256], st1[:, 40 + h:41 + h],
                    gsg[:, h * 256:(h + 1) * 256], ALU.mult, ALU.mult, (akey, "st1f", "gsg"), ("yb",))
        for h in range(4):
            bk, bkey = bank_s()
            mm(bk[:, 0:256], Khat[:, h * 128:(h + 1) * 128], gv[:, h * 256:(h + 1) * 256], True, True, ("Khat", "gv"),
               (bkey,))
            if is_meta:
                cp(S0[:, h, :], bk[:, 0:256], (bkey,), ("S0",))
            else:
                stt(S[:, h, :], S[:, h, :], Ep[:, h * 128 + 127:h * 128 + 128], bk[:, 0:256], ALU.mult, ALU.add,
                    ("S", "Ep", bkey), ("S",))
        if is_meta:
            return
        cp(Sb[:], S[:], ("S",), ("Sb",), eng="act")

        if stop == "E":
            dump(s, t, [(yb[:], "yb", 1024)])
            return
        transposes(yT[:].rearrange("p a b -> p (a b)"), "yT", ya, "ya", 8)
        for j in range(2):
            bk, bkey = proj_tm("oa", j * 512, (j + 1) * 512, yT, "yT")
            tt(mix[:, j * 512:(j + 1) * 512], bk[:, :], gas[:, j * 512:(j + 1) * 512], ALU.mult, (bkey, "gas"),
               ("mix",))
        transposes(yT[:].rearrange("p a b -> p (a b)"), "yT", yb, "yb", 8)
        for j in range(2):
            bk, bkey = proj_tm("ob", j * 512, (j + 1) * 512, yT, "yT")
            tt(gsg[:, j * 512:(j + 1) * 512], bk[:, :], gbs[:, j * 512:(j + 1) * 512], ALU.mult, (bkey, "gbs"),
               ("gsg",))
        tt(mixb[:], mix[:], gsg[:], ALU.add, ("mix", "gsg"), ("mixb",))
        transposes(yT[:].rearrange("p a b -> p (a b)"), "yT", mixb, "mixb", 8)
        for j in range(2):
            bk, bkey = proj_tm("out", j * 512, (j + 1) * 512, yT, "yT")
            tt(X[:, j * 512:(j + 1) * 512], bk[:, :], X[:, j * 512:(j + 1) * 512], ALU.add, (bkey, xk), (xk,))

        if stop == "F":
            dump(s, t, [(X[:], "xt0", 1024)])
            return
        act(junk[:], X[:], AF.Square, (xk,), ("st1g",), accum_out=st1[:, 48:49])
        rstd_from_ss(st1[:, 48:49], st1[:, 48:49], 1024.0, ("st1g",), ("st1g",))
        stt(n2b[:], X[:], st1[:, 48:49], G_FFN, ALU.mult, ALU.mult, (xk, "st1g", "gbt"), ("n2b",))
        transposes(yT[:].rearrange("p a b -> p (a b)"), "yT", n2b, "n2b", 8)
        for j in range(4):
            wsl, wk, _ = load_w("pq", j * 512, (j + 1) * 512)
            bk, bkey = bank_mm()
            for c in range(4):
                for kc in range(8):
                    mm(bk[:, c * 128:(c + 1) * 128], wsl[:, kc, c * 128:(c + 1) * 128], yT[:, kc, :], kc == 0, kc == 7,
                       ("yT", wk), (bkey,))
            cp(pqT[:].rearrange("p a b -> p (a b)"), bk[:, :], (bkey,), ("pqT",), eng="act")
            bk, bkey = bank_mm()
            for c in range(4):
                hp = 4 * j + c
                mm(bk[:, c * 128:(c + 1) * 128], pqT[:, c, :], keysT[:, (hp % 2) * 128:(hp % 2 + 1) * 128], True, True,
                   ("pqT", "keysT"), (bkey,))
            for c in range(4):
                hp = 4 * j + c
                sv = bk[:, c * 128:(c + 1) * 128]
                P.op("dve", (lambda hh, vv: (lambda e: e.max(out=tsv[:, hh, 0:8], in_=vv)))(hp, sv), (bkey,), ("tsv",))
                P.op("dve", (lambda hh, vv: (lambda e: e.max_index(out=tiu[:, hh, 0:8], in_max=tsv[:, hh, 0:8],
                                                                   in_values=vv)))(hp, sv), (bkey, "tsv"), ("tiu",))
                P.op("dve", (lambda hh, vv, cc: (lambda e: e.match_replace(
                    out=ss2[:, cc, :], in_to_replace=tsv[:, hh, 0:8], in_values=vv, imm_value=-1e30)))(hp, sv, c),
                    (bkey, "tsv"), ("ss2",))
                P.op("dve", (lambda hh, cc: (lambda e: e.max(out=tsv[:, hh, 8:16], in_=ss2[:, cc, :])))(hp, c),
                     ("ss2",), ("tsv",))
                P.op("dve", (lambda hh, cc: (lambda e: e.max_index(out=tiu[:, hh, 8:16], in_max=tsv[:, hh, 8:16],
                                                                   in_values=ss2[:, cc, :])))(hp, c), ("ss2", "tsv"),
                     ("tiu",))
        cp(tif[:], tiu[:], ("tiu",), ("tif",))
        ts4 = tsv[:].rearrange("p (h two) k -> p h two k", two=2)
        ti4 = tif[:].rearrange("p (h two) k -> p h two k", two=2)
        tsc(ti4[:, :, 0, :], ti4[:, :, 0, :], 128.0, None, ALU.mult, None, ("tif",), ("tif",))
        for h in range(8):
            cs = cands[:].rearrange("p (a b) -> p a b", a=16)
            ci = candi[:].rearrange("p (a b) -> p a b", a=16)
            tt(cs, ts4[:, h, 0, :].unsqueeze(2).to_broadcast([128, 16, 16]),
               ts4[:, h, 1, :].unsqueeze(1).to_broadcast([128, 16, 16]), ALU.add, ("tsv",), ("cands",))
            tt(ci, ti4[:, h, 0, :].unsqueeze(2).to_broadcast([128, 16, 16]),
               ti4[:, h, 1, :].unsqueeze(1).to_broadcast([128, 16, 16]), ALU.add, ("tif",), ("candi",))
            P.op("dve", (lambda hh: (lambda e: e.max(out=bsv[:, hh, 0:8], in_=cands[:])))(h), ("cands",), ("bsv",))
            P.op("dve", (lambda hh: (lambda e: e.match_replace(out=cands2[:], in_to_replace=bsv[:, hh, 0:8],
                                                               in_values=cands[:], imm_value=-1e30)))(h),
                 ("cands", "bsv"), ("cands2",))
            P.op("dve", (lambda hh: (lambda e: e.max(out=bsv[:, hh, 8:16], in_=cands2[:])))(h), ("cands2",), ("bsv",))
            for k in range(16):
                stt(junk2[:, 0:256], cands[:], bsv[:, h, k:k + 1], candi[:], ALU.is_equal, ALU.mult,
                    ("cands", "bsv", "candi"), ("idxf",), accum_out=idxf[:, h * 16 + k:h * 16 + k + 1])
        tsc(idxf[:], idxf[:], 16383.0, 0.0, ALU.min, ALU.max, ("idxf",), ("idxf",))
        cp(idxi[:], idxf[:], ("idxf",), ("idxi",))
        b3 = bsv[:]
        g3 = gate[:].rearrange("p (h k) -> p h k", h=8)
        tt(g3, b3, b3[:, :, 0:1].to_broadcast([128, 8, 16]), ALU.subtract, ("bsv",), ("gate",))
        act(gate[:], gate[:], AF.Exp, ("gate",), ("gate",))
        red(st1[:, 50:58], g3, ALU.add, ("gate",), ("st1h",))
        recip(st1[:, 50:58], st1[:, 50:58], ("st1h",), ("st1h",))
        tt(g3, g3, st1[:, 50:58].unsqueeze(2).to_broadcast([128, 8, 16]), ALU.mult, ("gate", "st1h"), ("gate",))
        if stop == "G":
            dump(s, t, [(idxf[:], "idxf", 128), (gate[:], "gate", 128)])
            return
        for j in range(128):
            ui = rot("uv", NUV)
            P.dma("pool", (lambda uu, jj: (lambda e: e.indirect_dma_start(
                out=uvb[uu][:, :], out_offset=None, in_=uvs,
                in_offset=bass.IndirectOffsetOnAxis(ap=idxi[:, jj:jj + 1], axis=0))))(ui, j),
                ("idxi", "uvs"), (f"uvb{ui}",))
            stt(junk2[:], uvb[ui][:, 0:1024], 1.0, n2b[:], ALU.mult, ALU.mult, (f"uvb{ui}", "n2b"), (f"actv{j}",),
                accum_out=actv[:, j:j + 1])
            act(glv[:, j:j + 1], actv[:, j:j + 1], AF.Gelu, (f"actv{j}",), (f"glv{j}",))
            di = rot("dg", ND)
            tsc(dgb[di][:], identb[:], glv[:, j:j + 1], gate[:, j:j + 1], ALU.mult, ALU.mult,
                ("identb", f"glv{j}", "gate"), (f"dgb{di}",))
            mm(pf[4][:, :], dgb[di][:], uvb[ui][:, 1024:1536], j == 0, j == 127, (f"dgb{di}", f"uvb{ui}"), ("pf4",))
            mm(pf[5][:, :], dgb[di][:], uvb[ui][:, 1536:2048], j == 0, j == 127, (f"dgb{di}", f"uvb{ui}"), ("pf5",))
        tt(X[:, 0:512], pf[4][:, :], X[:, 0:512], ALU.add, ("pf4", xk), (xk,))
        tt(X[:, 512:1024], pf[5][:, :], X[:, 512:1024], ALU.add, ("pf5", xk), (xk,))
        dma_sp(y[s, (t - 1) * 128:t * 128, :], X[:], (xk,), ("y",), final=True)

    oneb = sb("oneb", [128, 1])
    P.op("dve", lambda e: e.memset(oneb[:], 1.0), (), ("oneb",))

    if stop not in ("M0", "P1", "P2"):
        tile(None, 0, 0)
    par = 1
    for s in range(nseq):
        cp(S[:], S0[:], ("S0",), ("S",))
        cp(Sb[:], S0[:], ("S0",), ("Sb",), eng="act")
        for t in range(1, ntile + 1):
            if stop in ("M0", "P1", "P2"):
                dump(s, t, [])
                continue
            tile(s, t, par)
            par ^= 1
    P.flush()
    stack.close()
    return nc


def _host_consts():
    idx = np.arange(128)
    ident = np.eye(128, dtype=np.float32)
    s_ = idx[:, None]
    t_ = idx[None, :]
    tri = np.where(s_ <= t_, -1.0 / 16.0, 0.0).astype(np.float32)
    sut = np.where(s_ > t_, -1.0 / 16.0, 0.0).astype(np.float32)
    trim = np.where((s_ <= t_) & (s_ < 16), -1.0 / 16.0, 0.0).astype(np.float32)
    sutm = np.where((s_ > t_) & (s_ < 16), -1.0 / 16.0, 0.0).astype(np.float32)
    mut = np.where(s_ <= t_, 1.0, 0.0).astype(np.float32)
    cst = np.concatenate([ident, tri, sut, trim, sutm, mut, mut, mut, mut], axis=1)
    inv_freq = (10000.0 ** (-np.arange(32, dtype=np.float32) / 32.0)).astype(np.float32)
    rope = np.zeros((NKT, 128, 64), np.float32)
    for t in range(NKT):
        pos = (np.arange(128) if t == 0 else 16 + 128 * (t - 1) + np.arange(128)).astype(np.float32)
        ang = pos[:, None] * inv_freq[None, :]
        rope[t, :, 0:32] = np.cos(ang)
        rope[t, :, 32:64] = np.sin(ang)
    return np.ascontiguousarray(cst), rope


_CACHE = {}


def _run(inputs, nseq, ntile, ncores, stop=None):
    f = lambda a: np.ascontiguousarray(np.asarray(a, dtype=np.float32))
    x = f(inputs["x"])
    cst, rope = _host_consts()
    xm = np.zeros((128, 1024), np.float32)
    xm[0:16] = f(inputs["meta"])
    gcol = np.concatenate([f(inputs["norm_mix"])[0].reshape(8, 128).T, f(inputs["mla_q_norm"])[0].reshape(3, 128).T,
                           f(inputs["mla_kv_norm"])[0].reshape(2, 128).T], axis=1)
    gvec = np.concatenate([f(inputs["qn_nope"])[0], f(inputs["qn_pe"])[0], f(inputs["kn_nope"])[0],
                           f(inputs["kn_pe"])[0], f(inputs["gla_norm"])[0], f(inputs["norm_ffn"])[0]])
    gb = np.ascontiguousarray(np.broadcast_to(gvec[None, :], (128, gvec.shape[0])))
    wa2 = np.concatenate([f(inputs["gla_w_a2"])[0], f(inputs["gla_b_a"]), np.zeros((15, 512), np.float32)], axis=0)
    keys = f(inputs["peer_keys"])[0]
    keysT = np.ascontiguousarray(keys.transpose(2, 0, 1).reshape(128, 256))
    uvt = np.ascontiguousarray(np.concatenate([f(inputs["peer_u"])[0], f(inputs["peer_v"])[0]], axis=1))
    common = {
        "xm": xm, "w_in": f(inputs["w_in"])[0], "w_uq": f(inputs["mla_w_uq"])[0], "w_ukv": f(inputs["mla_w_ukv"])[0],
        "w_oa": f(inputs["w_o_mla"])[0], "w_ob": f(inputs["w_o_gla"])[0], "w_out": f(inputs["w_out"])[0],
        "w_pq": f(inputs["peer_w_q"])[0], "gcol": np.ascontiguousarray(gcol), "gb": gb, "wa2": np.ascontiguousarray(wa2),
        "keysT": keysT, "uv": (uvt if stop in (None, "H") else uvt[:128]), "rope": rope, "cst": cst,
    }
    key = (nseq, ntile, stop)
    if key not in _CACHE:
        _CACHE[key] = build(nseq, ntile, stop)
    nc = _CACHE[key]
    in_maps = []
    for c in range(ncores):
        m = dict(common)
        m["x"] = np.ascontiguousarray(x[c * nseq:(c + 1) * nseq])
        in_maps.append(m)
    import os
    res = run_bass_kernel_spmd(nc, in_maps, core_ids=list(range(ncores)), trace=bool(os.environ.get("DBGTRACE")))
    if os.environ.get("DBGTRACE"):
        print("EXEC_NS", res.exec_time_ns)
    return np.concatenate([np.asarray(r["y"]) for r in res.results], axis=0)


def kernel(**inputs):
    out = _run(inputs, 2, 16, NCORES)
    return out.astype(np.float32)
```

```python
import numpy as np
from contextlib import ExitStack
import concourse.bass as bass
import concourse.mybir as mybir
from concourse.bass_utils import run_bass_kernel_spmd

F32 = mybir.dt.float32
BF16 = mybir.dt.bfloat16
I32 = mybir.dt.int32
U32 = mybir.dt.uint32
AF = mybir.ActivationFunctionType
ALU = mybir.AluOpType
AX = mybir.AxisListType

EPS = 1e-6
NCORES = 8
ENGS = ("pe", "dve", "act", "pool", "sp")
EPOCH = 30000


class Prog:
    def __init__(self, nc, stack):
        self.nc = nc
        self.stack = stack
        self.q = {e: [] for e in ENGS}
        self.cnt = {e: 0 for e in ENGS}
        self.known = {e: {} for e in ENGS}
        self.sems = {}
        self.st = {}
        self.ring = {"sp": [("dma", "sp", i) for i in range(12)], "pool": [("dma", "pool", i) for i in range(16)],
                     "act": [("dma", "act", i) for i in range(4)]}
        self.ring_pos = {k: 0 for k in self.ring}
        self.ring_ev = {}
        self.ring_uses = {}
        self.final = []
        self.alias = {}

    def _x(self, keys):
        out = []
        for k in keys:
            out.extend(self.alias.get(k, (k,)))
        return out

    def _sem(self, key):
        if key not in self.sems:
            name = "s_" + "_".join(str(k) for k in key)
            self.sems[key] = self.stack.enter_context(self.nc.semaphore(name))
        return self.sems[key]

    def _deps(self, r, w):
        r = self._x(r)
        w = self._x(w)
        deps = []
        for k in r:
            s = self.st.get(k)
            if s:
                deps.extend(s[0].values())
        for k in w:
            s = self.st.get(k)
            if s:
                deps.extend(s[0].values())
                deps.extend(s[1].values())
        return deps

    def _wait(self, eng, deps):
        for (sk, val) in deps:
            if eng == "pe" and sk[0] == "pe":
                continue
            if self.known[eng].get(sk, 0) >= val:
                continue
            self.known[eng][sk] = val
            self._sem(sk)
            self.q[eng].append(("wait", sk, val))

    def _record(self, ev, r, w):
        r = self._x(r)
        w = self._x(w)
        for k in w:
            s = self.st.setdefault(k, [{}, {}])
            s[0][ev[0]] = ev
            s[1] = {}
        for k in r:
            s = self.st.setdefault(k, [{}, {}])
            s[1][ev[0]] = ev

    def op(self, eng, fn, r=(), w=()):
        self._wait(eng, self._deps(r, w))
        self.cnt[eng] += 1
        c = self.cnt[eng]
        ep = (c - 1) // EPOCH
        ev = ((eng, ep), c - ep * EPOCH)
        self._sem(ev[0])
        self.q[eng].append(("op", fn, ev[0], 1))
        self._record(ev, r, w)
        return ev

    def dma(self, eng, fn, r=(), w=(), final=False):
        ring = self.ring[eng]
        sk = ring[self.ring_pos[eng] % len(ring)]
        self.ring_pos[eng] += 1
        deps = self._deps(r, w)
        if sk in self.ring_ev:
            deps.append(self.ring_ev[sk])
        self._wait(eng, deps)
        self.ring_uses[sk] = self.ring_uses.get(sk, 0) + 1
        ev = (sk, 16 * self.ring_uses[sk])
        self.ring_ev[sk] = ev
        self._sem(sk)
        self.q[eng].append(("op", fn, sk, 16))
        self._record(ev, r, w)
        if final:
            self.final.append(ev)
        return ev

    def flush(self):
        nc = self.nc
        self._wait("sp", self.final)
        self._wait("sp", list(self.ring_ev.values()))
        print("PROG counts", self.cnt, {k: len(v) for k, v in self.q.items()}, flush=True)
        q = self.q
        sems = self.sems

        def run(name, e):
            for it in q[name]:
                if it[0] == "wait":
                    e.wait_ge(sems[it[1]], it[2])
                else:
                    ins = it[1](e)
                    ins.then_inc(sems[it[2]], it[3])

        with nc.Block() as block:
            @block.tensor
            def _(e):
                run("pe", e)

            @block.vector
            def _(e):
                run("dve", e)

            @block.scalar
            def _(e):
                run("act", e)

            @block.gpsimd
            def _(e):
                run("pool", e)

            @block.sync
            def _(e):
                run("sp", e)


C_CQ, C_CKV, C_KPE, C_GQ, C_GK, C_GV, C_GA, C_GG, C_GTA, C_GTB, C_END = (
    0, 384, 640, 704, 1216, 1728, 2752, 2768, 3792, 4816, 5840)
NKT = 17


def build(nseq, ntile, stop=None):
    nc = bass.Bass("TRN2", target_bir_lowering=False)
    stack = ExitStack()
    P = Prog(nc, stack)

    def din(name, shape, dt=F32):
        return nc.dram_tensor(name, list(shape), dt, kind="ExternalInput").ap()

    x = din("x", [nseq, 2048, 1024])
    xm = din("xm", [128, 1024])
    Wd = {
        "in": (din("w_in", [1024, 5840]), 8, 5840, 0),
        "uq": (din("w_uq", [384, 1536]), 3, 1536, 8),
        "ukv": (din("w_ukv", [256, 2048]), 2, 2048, 11),
        "oa": (din("w_oa", [1024, 1024]), 8, 1024, None),
        "ob": (din("w_ob", [1024, 1024]), 8, 1024, None),
        "out": (din("w_out", [1024, 1024]), 8, 1024, None),
        "pq": (din("w_pq", [1024, 2048]), 8, 2048, None),
    }
    gcol_d = din("gcol", [128, 13])
    gb_d = din("gb", [128, 1664])
    wa2_d = din("wa2", [32, 512])
    keysT_d = din("keysT", [128, 256])
    NEXP = 16384 if stop in (None, "H") else 128
    uv = din("uv", [NEXP, 2048])
    rope_d = din("rope", [NKT, 128, 64])
    cst_d = din("cst", [128, 1152])
    y = nc.dram_tensor("y", [nseq, 2048, 1024], F32, kind="ExternalOutput").ap()
    uvs = nc.dram_tensor("uvs", [NEXP, 2048], BF16, kind="Internal").ap()
    ws = {k: nc.dram_tensor("ws_" + k, [128, v[1], v[2]], BF16, kind="Internal").ap() for k, v in Wd.items()}

    def sb(name, shape, dt=F32):
        return stack.enter_context(nc.sbuf_tensor("sb_" + name, list(shape), dt))

    def ps(name, shape, dt=F32):
        return stack.enter_context(nc.psum_tensor("ps_" + name, list(shape), dt))

    cst = sb("cst", [128, 1152])
    IDENT = cst[:, 0:128]
    TRI = cst[:, 128:256]
    SUT = cst[:, 256:384]
    TRIM = cst[:, 384:512]
    SUTM = cst[:, 512:640]
    MASKUT = cst[:, 640:1152]
    identb = sb("identb", [128, 128], BF16)
    gcol = sb("gcol", [128, 13])
    gbt = sb("gbt", [128, 1664])
    G_QN, G_QP, G_KN, G_KP, G_GLA, G_FFN = (gbt[:, 0:128], gbt[:, 128:192], gbt[:, 192:320], gbt[:, 320:384],
                                          gbt[:, 384:640], gbt[:, 640:1664])
    wa2 = sb("wa2", [32, 512])
    keysT = sb("keysT", [128, 256])
    ropet = sb("ropet", [128, NKT, 64])

    KT = sb("KT", [128, 8, NKT * 128], BF16)
    KPT = sb("KPT", [128, NKT * 128], BF16)
    VC = sb("VC", [128, NKT, 8, 132], BF16)
    S = sb("S", [128, 4, 256])
    S0 = sb("S0", [128, 4, 256])
    Sb = sb("Sb", [128, 4, 256], BF16)

    NWS = 3
    wslot = [sb(f"wslot{i}", [128, 8, 512], BF16) for i in range(NWS)]
    NUV = 12
    ND = 8

    AW = 16896
    arena = sb("arena", [128, AW])

    def carve(name, off, shape, dt=F32):
        per = 1
        for d in shape[1:]:
            per *= d
        words = per if dt in (F32, I32, U32) else per // 2
        assert off + words <= AW, name
        v = arena[:, off:off + words]
        if dt != F32:
            v = v.bitcast(dt)
        if len(shape) == 3:
            v = v.rearrange("p (a b) -> p a b", a=shape[1])
        P.alias[name] = tuple(("ar", g) for g in range(off // 128, (off + words - 1) // 128 + 1))
        return v

    lat = carve("lat", 0, [128, 704])
    qsb = carve("qsb", 704, [128, 8, 192])
    kvs = carve("kvs", 2240, [128, 8, 128])
    knn = carve("knn", 3264, [128, 8, 128], BF16)
    qnn = carve("qnn", 3776, [128, 8, 128], BF16)
    qpe = carve("qpe", 4288, [128, 8, 64])
    qpr = carve("qpr", 4800, [128, 8, 64], BF16)
    cqn = carve("cqn", 5056, [128, 384], BF16)
    ckn = carve("ckn", 5248, [128, 256], BF16)
    cqT = carve("cqT", 5376, [128, 3, 128], BF16)
    ckT = carve("ckT", 5568, [128, 2, 128], BF16)
    kp = carve("kp", 5696, [128, 64])
    kp2 = carve("kp2", 5760, [128, 128], BF16)
    rt = carve("rt", 5824, [128, 8, 32])
    rt2 = carve("rt2", 6080, [128, 8, 32])
    e1 = carve("e1", 0, [128, 512])
    lsp = carve("lsp", 512, [128, 512])
    Ep = carve("Ep", 1024, [128, 512])
    En = carve("En", 1536, [128, 512])
    QtT = carve("QtT", 2048, [128, 4, 128], BF16)
    KtT = carve("KtT", 2304, [128, 4, 128], BF16)
    Khat = carve("Khat", 2560, [128, 512], BF16)
    At = carve("At", 2816, [128, 512], BF16)
    n2b = carve("n2b", 0, [128, 1024], BF16)
    pqT = carve("pqT", 512, [128, 4, 128])
    ss2 = carve("ss2", 1024, [128, 4, 128])
    tsv = carve("tsv", 1536, [128, 16, 16])
    tiu = carve("tiu", 1792, [128, 16, 16], U32)
    tif = carve("tif", 2048, [128, 16, 16])
    cands = carve("cands", 2304, [128, 256])
    cands2 = carve("cands2", 2560, [128, 256])
    candi = carve("candi", 2816, [128, 256])
    dgb = [carve(f"dgb{i}", 3072 + 64 * i, [128, 128], BF16) for i in range(ND)]
    uvb = [carve(f"uvb{i}", 3584 + 1024 * i, [128, 2048], BF16) for i in range(NUV)]
    stg = [carve(f"stg{i}", 512 * i, [128, 512]) for i in range(2)]
    stgb = [carve(f"stgb{i}", 1024 + 256 * i, [128, 512], BF16) for i in range(2)]

    ustg = [carve(f"ustg{i}", 2048 * i, [128, 2048]) for i in range(2)]
    ustgb = [carve(f"ustgb{i}", 4096 + 1024 * i, [128, 2048], BF16) for i in range(2)]

    xt = [sb("xt0", [128, 1024])]
    junk = sb("junk", [128, 1024], BF16)
    junk2 = sb("junk2", [128, 1024], BF16)
    st1 = sb("st1", [128, 64])
    gaT = sb("gaT", [32, 128])
    pm = [sb(f"pm{i}", [16, 128], BF16) for i in range(2)]
    yT = sb("yT", [128, 8, 128], BF16)
    o2 = 7168
    nb = carve("nb", o2, [128, 1024], BF16)
    nT = carve("nT", o2 + 512, [128, 8, 128], BF16)
    gqT = carve("gqT", o2 + 1024, [128, 4, 128])
    gkT = carve("gkT", o2 + 1536, [128, 4, 128])
    gktm = carve("gktm", o2 + 2048, [128, 512])
    gv = carve("gv", o2 + 2560, [128, 1024], BF16)
    sg = carve("sg", o2 + 3072, [128, 1024], BF16)
    gas = carve("gas", o2 + 3584, [128, 1024], BF16)
    gbs = carve("gbs", o2 + 4096, [128, 1024], BF16)
    qnT = carve("qnT", o2 + 4608, [128, 8, 128], BF16)
    qpT = carve("qpT", o2 + 5120, [128, 4, 128], BF16)
    pb = [carve(f"pb{i}", o2 + 5376 + 256 * i, [128, 512], BF16) for i in range(3)]
    ya = carve("ya", o2 + 6144, [128, 1024], BF16)
    yb = carve("yb", o2 + 6656, [128, 1024], BF16)
    MG = carve("MG", o2 + 7168, [128, 2048])
    mixb = carve("mixb", o2 + 9216, [128, 1024], BF16)
    mix = MG[:, 0:1024]
    gsg = MG[:, 1024:2048]
    sq = MG[:, 0:1536].rearrange("p (a b) -> p a b", a=8)
    for nm in ("mix", "gsg", "sq"):
        P.alias[nm] = P.alias["MG"]
    bsv = sb("bsv", [128, 8, 16])
    idxf = sb("idxf", [128, 128])
    idxi = sb("idxi", [128, 128], I32)
    gate = sb("gate", [128, 128])
    actv = sb("actv", [128, 128])
    glv = sb("glv", [128, 128])

    pf = [ps(f"pf{i}", [128, 512]) for i in range(6)]
    pt = [ps(f"pt{i}", [128, 1024], BF16) for i in range(2)]
    cnt = {"mm": 0, "s": 0, "t": 0, "w": 0, "pb": 0, "pm": 0, "stg": 0, "uv": 0, "dg": 0}

    def rot(kind, n):
        v = cnt[kind] % n
        cnt[kind] += 1
        return v

    def bank_mm():
        i = rot("mm", 2)
        return pf[i], f"pf{i}"

    def bank_s():
        i = 2 + rot("s", 2)
        return pf[i], f"pf{i}"

    def bank_t():
        i = rot("t", 2)
        return pt[i], f"pt{i}"

    def dma_sp(out, in_, r, w, final=False):
        P.dma("sp", lambda e: e.dma_start(out=out, in_=in_), r, w, final)

    def mm(out, lhsT, rhs, start, stop, r, w):
        P.op("pe", lambda e: e.matmul(out, lhsT, rhs, start=start, stop=stop), r, w)

    def tr(out, in_, idn, r, w):
        P.op("pe", lambda e: e.transpose(out, in_, idn), r, w)

    def act(out, in_, func, r, w, bias=None, scale=None, accum_out=None):
        kw = {}
        if bias is not None:
            kw["bias"] = bias
        if scale is not None:
            kw["scale"] = scale
        if accum_out is not None:
            kw["accum_out"] = accum_out
        P.op("act", lambda e: e.activation(out=out, in_=in_, func=func, **kw), r, w)

    def tt(out, in0, in1, op, r, w, eng="dve"):
        P.op(eng, lambda e: e.tensor_tensor(out=out, in0=in0, in1=in1, op=op), r, w)

    def tsc(out, in0, s1, s2, op0, op1, r, w, eng="dve", accum_out=None):
        if op1 is None:
            P.op(eng, lambda e: e.tensor_scalar(out=out, in0=in0, scalar1=s1, scalar2=None, op0=op0), r, w)
        else:
            P.op(eng, lambda e: e.tensor_scalar(out=out, in0=in0, scalar1=s1, scalar2=s2, op0=op0, op1=op1,
                                                accum_out=accum_out), r, w)

    def stt(out, in0, scalar, in1, op0, op1, r, w, accum_out=None):
        P.op("dve", lambda e: e.scalar_tensor_tensor(out=out, in0=in0, scalar=scalar, in1=in1, op0=op0, op1=op1,
                                                     accum_out=accum_out), r, w)

    def cp(out, in_, r, w, eng="dve"):
        if eng == "act":
            P.op("act", lambda e: e.copy(out=out, in_=in_), r, w)
        else:
            P.op(eng, lambda e: e.tensor_copy(out=out, in_=in_), r, w)

    def recip(out, in_, r, w):
        P.op("dve", lambda e: e.reciprocal(out=out, in_=in_), r, w)

    def red(out, in_, op, r, w):
        P.op("dve", lambda e: e.tensor_reduce(out=out, in_=in_, axis=AX.X, op=op), r, w)

    def rstd_from_ss(dst, ssum, n, r, w):
        act(dst, ssum, AF.Sqrt, tuple(r) + ("epsb",), w, bias=epsb[:, 0:1], scale=1.0 / n)
        recip(dst, dst, w, w)

    epsb = sb("epsb", [128, 1])
    P.op("dve", lambda e: e.memset(epsb[:], EPS), (), ("epsb",))

    import os
    SK = os.environ.get("DBGSKIP", "").split(",")
    if "cst" not in SK:
        dma_sp(cst[:], cst_d, (), ("cst",))
    if "gcol" not in SK:
        dma_sp(gcol[:], gcol_d, (), ("gcol",))
    if "gbt" not in SK:
        dma_sp(gbt[:], gb_d, (), ("gbt",))
    if "wa2" not in SK:
        dma_sp(wa2[:], wa2_d, (), ("wa2",))
    if "keysT" not in SK:
        dma_sp(keysT[:], keysT_d, (), ("keysT",))
    if "rope" not in SK:
        dma_sp(ropet[:], rope_d.rearrange("t p c -> p t c"), (), ("ropet",))
    if "ms" not in SK:
        cp(identb[:], IDENT, ("cst",), ("identb",))
        P.op("dve", lambda e: e.memset(gaT[:], 1.0), (), ("gaT",))
        P.op("dve", lambda e: e.memset(VC[:], 1.0), (), tuple(f"V{t}" for t in range(NKT)))
        P.op("dve", lambda e: e.memset(S0[:], 0.0), (), ("S0",))

    for name, (wd, kc_n, ncol, gc) in Wd.items():
        if stop == "P1" or (stop == "P2" and name != "ukv"):
            continue
        for kc in range(kc_n):
            for c0 in range(0, ncol, 512):
                c1 = min(ncol, c0 + 512)
                i = rot("stg", 2)
                dma_sp(stg[i][:, 0:c1 - c0], wd[kc * 128:(kc + 1) * 128, c0:c1], (), (f"stg{i}",))
                if gc is None:
                    cp(stgb[i][:, 0:c1 - c0], stg[i][:, 0:c1 - c0], (f"stg{i}",), (f"stgb{i}",),
                       eng=("act" if (cnt["stg"] % 4 < 2) else "dve"))
                else:
                    tsc(stgb[i][:, 0:c1 - c0], stg[i][:, 0:c1 - c0], gcol[:, gc + kc:gc + kc + 1], None, ALU.mult,
                        None, (f"stg{i}", "gcol"), (f"stgb{i}",))
                dma_sp(ws[name][:, kc, c0:c1], stgb[i][:, 0:c1 - c0], (f"stgb{i}",), (f"ws_{name}",))

    if stop in (None, "H"):
        for i in range(NEXP // 128):
            k = i % 2
            dma_sp(ustg[k][:], uv[i * 128:(i + 1) * 128, :], (), (f"ustg{k}",))
            cp(ustgb[k][:], ustg[k][:], (f"ustg{k}",), (f"ustgb{k}",), eng=("act", "dve", "pool")[i % 3])
            dma_sp(uvs[i * 128:(i + 1) * 128, :], ustgb[k][:], (f"ustgb{k}",), ("uvs",))

    def load_w(name, c0, c1):
        kc_n = Wd[name][1]
        i = rot("w", NWS)
        dma_sp(wslot[i][:, 0:kc_n, 0:c1 - c0], ws[name][:, :, c0:c1], (f"ws_{name}",), (f"wslot{i}",))
        return wslot[i], f"wslot{i}", kc_n

    def proj_tm(name, c0, c1, lhs, lhs_key):
        wsl, wk, kc_n = load_w(name, c0, c1)
        bk, bkey = bank_mm()
        for kc in range(kc_n):
            mm(bk[:, 0:c1 - c0], lhs[:, kc, :], wsl[:, kc, 0:c1 - c0], kc == 0, kc == kc_n - 1, (lhs_key, wk), (bkey,))
        return bk, bkey

    def transposes(dst, dst_key, src, src_key, nblk, eng="dve"):
        bk, bkey = bank_t()
        for j in range(nblk):
            tr(bk[:, j * 128:(j + 1) * 128], src[:, j * 128:(j + 1) * 128], identb[:], (src_key, "identb"), (bkey,))
        cp(dst, bk[:, 0:nblk * 128], (bkey,), (dst_key,), eng=eng)

    SCALE_ATT = 192.0 ** -0.5
    SCALE_GLA = 128.0 ** -0.5

    def rope_apply(dst, src, t, nh, r, w):
        cos = ropet[:, t, 0:32].unsqueeze(1).to_broadcast([128, nh, 32])
        sin = ropet[:, t, 32:64].unsqueeze(1).to_broadcast([128, nh, 32])
        x1 = src[:, :, 0:32]
        x2 = src[:, :, 32:64]
        a = rt[:, 0:nh, :]
        b = rt2[:, 0:nh, :]
        tt(a, x1, cos, ALU.mult, r + ("ropet",), ("rt",))
        tt(b, x2, sin, ALU.mult, r + ("ropet",), ("rt2",))
        tt(dst[:, :, 0:32], a, b, ALU.subtract, ("rt", "rt2"), w)
        tt(a, x2, cos, ALU.mult, r + ("ropet",), ("rt",))
        tt(b, x1, sin, ALU.mult, r + ("ropet",), ("rt2",))
        tt(dst[:, :, 32:64], a, b, ALU.add, ("rt", "rt2"), w)

    def dump(s, t, items):
        X = xt[0]
        o = 0
        for ap, key, n in items:
            cp(X[:ap.shape[0], o:o + n], ap, (key,), ("xt0",))
            o += n
        dma_sp(y[s, (t - 1) * 128:t * 128, :], X[:], ("xt0",), ("y",), final=True)

    def tile(s, t, par):
        is_meta = s is None
        X = xt[0]
        xk = "xt0"
        if is_meta:
            dma_sp(X[:], xm, (), (xk,))
        else:
            dma_sp(X[:], x[s, (t - 1) * 128:t * 128, :], (), (xk,))
        act(junk[:], X[:], AF.Square, (xk,), ("st1a",), accum_out=st1[:, 0:1])
        rstd_from_ss(st1[:, 0:1], st1[:, 0:1], 1024.0, ("st1a",), ("st1a",))
        tsc(nb[:], X[:], st1[:, 0:1], None, ALU.mult, None, (xk, "st1a"), ("nb",))
        transposes(nT[:].rearrange("p a b -> p (a b)"), "nT", nb, "nb", 8)

        if stop == "A":
            if not is_meta:
                dump(s, t, [(nb[:], "nb", 1024)])
            return
        bk, bkey = proj_tm("in", 0, 512, nT, "nT")
        cp(lat[:, 0:512], bk[:, 0:512], (bkey,), ("lat",), eng="act")
        bk, bkey = proj_tm("in", 512, 704, nT, "nT")
        cp(lat[:, 512:704], bk[:, 0:192], (bkey,), ("lat",), eng="act")
        wsl, wk, _ = load_w("in", C_GQ, C_GQ + 512)
        bk, bkey = bank_mm()
        for h in range(4):
            for kc in range(8):
                mm(bk[:, h * 128:(h + 1) * 128], wsl[:, kc, h * 128:(h + 1) * 128], nT[:, kc, :], kc == 0, kc == 7,
                   ("nT", wk), (bkey,))
        cp(gqT[:].rearrange("p a b -> p (a b)"), bk[:, :], (bkey,), ("gqT",), eng="act")
        wsl, wk, _ = load_w("in", C_GK, C_GK + 512)
        bk, bkey = bank_mm()
        for h in range(4):
            for kc in range(8):
                mm(bk[:, h * 128:(h + 1) * 128], wsl[:, kc, h * 128:(h + 1) * 128], nT[:, kc, :], kc == 0, kc == 7,
                   ("nT", wk), (bkey,))
        cp(gkT[:].rearrange("p a b -> p (a b)"), bk[:, :], (bkey,), ("gkT",), eng="act")
        bk, bkey = bank_mm()
        for kc in range(8):
            mm(bk[:, :], nT[:, kc, :], wsl[:, kc, :], kc == 0, kc == 7, ("nT", wk), (bkey,))
        cp(gktm[:], bk[:, :], (bkey,), ("gktm",), eng="dve")
        for j in range(2):
            bk, bkey = proj_tm("in", C_GV + j * 512, C_GV + (j + 1) * 512, nT, "nT")
            cp(gv[:, j * 512:(j + 1) * 512], bk[:, :], (bkey,), ("gv",), eng=("act" if j == 0 else "dve"))
        wsl, wk, _ = load_w("in", C_GA, C_GA + 16)
        bk, bkey = bank_mm()
        for kc in range(8):
            mm(bk[0:16, 0:128], wsl[:, kc, 0:16], nT[:, kc, :], kc == 0, kc == 7, ("nT", wk), (bkey,))
        cp(gaT[0:16, :], bk[0:16, 0:128], (bkey,), ("gaT",), eng="dve")
        if not is_meta:
            for j in range(2):
                bk, bkey = proj_tm("in", C_GG + j * 512, C_GG + (j + 1) * 512, nT, "nT")
                act(sg[:, j * 512:(j + 1) * 512], bk[:, :], AF.Silu, (bkey,), ("sg",))
            for j in range(2):
                bk, bkey = proj_tm("in", C_GTA + j * 512, C_GTA + (j + 1) * 512, nT, "nT")
                act(gas[:, j * 512:(j + 1) * 512], bk[:, :], AF.Sigmoid, (bkey,), ("gas",))
            for j in range(2):
                bk, bkey = proj_tm("in", C_GTB + j * 512, C_GTB + (j + 1) * 512, nT, "nT")
                act(gbs[:, j * 512:(j + 1) * 512], bk[:, :], AF.Sigmoid, (bkey,), ("gbs",))

        if stop == "B":
            if not is_meta:
                dump(s, t, [(lat[:, :], "lat", 704), (gv[:, 0:320], "gv", 320)])
            return
        tt(sq[:].rearrange("p a b -> p (a b)")[:, 0:704], lat[:, :], lat[:, :], ALU.mult, ("lat",), ("sq",))
        sqf = sq[:].rearrange("p a b -> p (a b)")
        red(st1[:, 1:2], sqf[:, 0:384], ALU.add, ("sq",), ("st1b",))
        red(st1[:, 2:3], sqf[:, 384:640], ALU.add, ("sq",), ("st1b",))
        red(st1[:, 3:4], sqf[:, 640:704], ALU.add, ("sq",), ("st1b",))
        rstd_from_ss(st1[:, 1:2], st1[:, 1:2], 384.0, ("st1b",), ("st1b",))
        rstd_from_ss(st1[:, 2:3], st1[:, 2:3], 256.0, ("st1b",), ("st1b",))
        rstd_from_ss(st1[:, 3:4], st1[:, 3:4], 64.0, ("st1b",), ("st1b",))
        tsc(ckn[:], lat[:, 384:640], st1[:, 2:3], None, ALU.mult, None, ("lat", "st1b"), ("ckn",))
        transposes(ckT[:].rearrange("p a b -> p (a b)"), "ckT", ckn, "ckn", 2)
        stt(kp[:], lat[:, 640:704], st1[:, 3:4], G_KP, ALU.mult, ALU.mult, ("lat", "st1b", "gbt"), ("kp",))
        kpv = kp[:].unsqueeze(1)
        kp2v = kp2[:].rearrange("p (a b) -> p a b", a=2)
        rope_apply(kp2v[:, 0:1, :], kpv, t, 1, ("kp",), ("kp2",))
        cp(kp2[:, 64:128], kp2[:, 0:64], ("kp2",), ("kp2",))
        bk, bkey = bank_t()
        tr(bk[:, 0:128], kp2[:], identb[:], ("kp2", "identb"), (bkey,))
        cp(KPT[:, t * 128:(t + 1) * 128], bk[:, 0:128], (bkey,), (f"KP{t}",))
        if stop == "C1":
            if not is_meta:
                dump(s, t, [])
            return
        for j in range(4):
            bk, bkey = proj_tm("ukv", j * 512, (j + 1) * 512, ckT, "ckT")
            bv = bk[:, :].rearrange("p (h c) -> p h c", h=2)
            ee = "act" if j % 2 == 0 else "dve"
            cp(kvs[:, 2 * j:2 * j + 2, :], bv[:, :, 0:128], (bkey,), ("kvs",), eng=ee)
            cp(VC[:, t, 2 * j:2 * j + 2, 0:128], bv[:, :, 128:256], (bkey,), (f"V{t}",), eng=ee)
        if stop == "C1a":
            if not is_meta:
                dump(s, t, [])
            return
        tt(sq[:, :, 0:128], kvs[:], kvs[:], ALU.mult, ("kvs",), ("sq",))
        red(st1[:, 8:16], sq[:, :, 0:128], ALU.add, ("sq",), ("st1c",))
        rstd_from_ss(st1[:, 8:16], st1[:, 8:16], 128.0, ("st1c",), ("st1c",))
        if stop == "C1b":
            if not is_meta:
                dump(s, t, [])
            return
        tt(kvs[:], kvs[:], st1[:, 8:16].unsqueeze(2).to_broadcast([128, 8, 128]), ALU.mult, ("kvs", "st1c"), ("kvs",))
        tt(knn[:], kvs[:], G_KN.unsqueeze(1).to_broadcast([128, 8, 128]), ALU.mult, ("kvs", "gbt"), ("knn",))
        if stop == "C1c":
            if not is_meta:
                dump(s, t, [])
            return
        bk, bkey = bank_t()
        for h in range(8):
            tr(bk[:, h * 128:(h + 1) * 128], knn[:, h, :], identb[:], ("knn", "identb"), (bkey,))
        cp(KT[:, :, t * 128:(t + 1) * 128], bk[:, :].rearrange("p (h c) -> p h c", h=8), (bkey,), (f"K{t}",), eng="act")

        if stop == "C2":
            if not is_meta:
                dump(s, t, [])
            return
        if not is_meta:
            tsc(cqn[:], lat[:, 0:384], st1[:, 1:2], None, ALU.mult, None, ("lat", "st1b"), ("cqn",))
            transposes(cqT[:].rearrange("p a b -> p (a b)"), "cqT", cqn, "cqn", 3)
            qf = qsb[:].rearrange("p a b -> p (a b)")
            for j in range(3):
                bk, bkey = proj_tm("uq", j * 512, (j + 1) * 512, cqT, "cqT")
                cp(qf[:, j * 512:(j + 1) * 512], bk[:, :], (bkey,), ("qsb",), eng=("act" if j != 1 else "dve"))
            tt(sq[:], qsb[:], qsb[:], ALU.mult, ("qsb",), ("sq",))
            red(st1[:, 16:24], sq[:, :, 0:128], ALU.add, ("sq",), ("st1d",))
            red(st1[:, 24:32], sq[:, :, 128:192], ALU.add, ("sq",), ("st1d",))
            rstd_from_ss(st1[:, 16:24], st1[:, 16:24], 128.0, ("st1d",), ("st1d",))
            rstd_from_ss(st1[:, 24:32], st1[:, 24:32], 64.0, ("st1d",), ("st1d",))
            tt(qsb[:, :, 0:128], qsb[:, :, 0:128], st1[:, 16:24].unsqueeze(2).to_broadcast([128, 8, 128]), ALU.mult,
               ("qsb", "st1d"), ("qsb",))
            tt(qnn[:], qsb[:, :, 0:128], G_QN.unsqueeze(1).to_broadcast([128, 8, 128]), ALU.mult, ("qsb", "gbt"),
               ("qnn",))
            tt(qsb[:, :, 128:192], qsb[:, :, 128:192], st1[:, 24:32].unsqueeze(2).to_broadcast([128, 8, 64]), ALU.mult,
               ("qsb", "st1d"), ("qsb",))
            tt(qpe[:], qsb[:, :, 128:192], G_QP.unsqueeze(1).to_broadcast([128, 8, 64]), ALU.mult, ("qsb", "gbt"),
               ("qpe",))
            rope_apply(qpr[:], qpe[:], t, 8, ("qpe",), ("qpr",))
            transposes(qnT[:].rearrange("p a b -> p (a b)"), "qnT", qnn[:].rearrange("p a b -> p (a b)"), "qnn", 8,
                       eng="act")
            transposes(qpT[:].rearrange("p a b -> p (a b)"), "qpT", qpr[:].rearrange("p a b -> p (a b)"), "qpr", 4,
                       eng="act")

            if stop == "C":
                dump(s, t, [(qnn[:].rearrange("p a b -> p (a b)")[:, 0:512], "qnn", 512),
                            (qpr[:].rearrange("p a b -> p (a b)"), "qpr", 512)])
                return
            kts = list(range(1, t + 1))
            for h in range(8):
                hp0 = (h % 2) * 64
                acc = pf[4 + (h % 2)]
                akey = f"pf{4 + (h % 2)}"
                bk, bkey = bank_s()
                mm(bk[0:16, 0:128], KT[:, h, 0:16], qnT[:, h, :], True, False, ("K0", "qnT"), (bkey,))
                mm(bk[0:16, 0:128], KPT[hp0:hp0 + 64, 0:16], qpT[hp0:hp0 + 64, h // 2, :], False, True,
                   ("KP0", "qpT"), (bkey,))
                mi = rot("pm", 2)
                act(pm[mi][:], bk[0:16, 0:128], AF.Exp, (bkey,), (f"pm{mi}",), scale=SCALE_ATT)
                mm(acc[:, 0:129], pm[mi][:], VC[0:16, 0, h, 0:129], True, False, (f"pm{mi}", "V0"), (akey,))
                for g0 in range(0, len(kts), 4):
                    grp = kts[g0:g0 + 4]
                    bk, bkey = bank_s()
                    for j, kt in enumerate(grp):
                        mm(bk[:, j * 128:(j + 1) * 128], KT[:, h, kt * 128:(kt + 1) * 128], qnT[:, h, :], True, False,
                           (f"K{kt}", "qnT"), (bkey,))
                        mm(bk[:, j * 128:(j + 1) * 128], KPT[hp0:hp0 + 64, kt * 128:(kt + 1) * 128],
                           qpT[hp0:hp0 + 64, h // 2, :], False, True, (f"KP{kt}", "qpT"), (bkey,))
                    pi = rot("pb", 3)
                    n = len(grp) * 128
                    act(pb[pi][:, 0:n], bk[:, 0:n], AF.Exp, (bkey,), (f"pb{pi}",), scale=SCALE_ATT)
                    if grp[-1] == t:
                        j = len(grp) - 1
                        P.op("dve", (lambda pp, jj: (lambda e: e.memset(pp[64:128, jj * 128:jj * 128 + 64], 0.0)))(
                            pb[pi], j), (), (f"pb{pi}",))
                    for j, kt in enumerate(grp):
                        mm(acc[:, 0:129], pb[pi][:, j * 128:(j + 1) * 128], VC[:, kt, h, 0:129], False, kt == t,
                           (f"pb{pi}", f"V{kt}"), (akey,))
                recip(st1[:, 32 + h:33 + h], acc[:, 128:129], (akey,), (f"st1e{h}",))
                tsc(ya[:, h * 128:(h + 1) * 128], acc[:, 0:128], st1[:, 32 + h:33 + h], None, ALU.mult, None,
                    (akey, f"st1e{h}"), ("ya",))

        if stop == "D" and not is_meta:
            dump(s, t, [(ya[:], "ya", 1024)])
            return
        tri = TRIM if is_meta else TRI
        sut = SUTM if is_meta else SUT
        bk, bkey = bank_s()
        mm(bk[:, :], gaT[:, :], wa2[:, :], True, True, ("gaT", "wa2"), (bkey,))
        act(e1[:], bk[:, :], AF.Exp, (bkey,), ("e1",), scale=-1.0)
        act(lsp[:], e1[:], AF.Ln, ("e1", "oneb"), ("lsp",), bias=oneb[:, 0:1])
        bk, bkey = bank_s()
        for h in range(4):
            mm(bk[:, h * 128:(h + 1) * 128], lsp[:, h * 128:(h + 1) * 128], tri, True, True, ("lsp", "cst"), (bkey,))
        act(Ep[:], bk[:, :], AF.Exp, (bkey,), ("Ep",))
        act(En[:], bk[:, :], AF.Exp, (bkey,), ("En",), scale=-1.0)
        bk, bkey = bank_s()
        mm(bk[:, :], sut, lsp[:], True, True, ("lsp", "cst"), (bkey,))
        act(e1[:], bk[:, :], AF.Exp, (bkey,), ("e1",))
        tt(Khat[:], gktm[:], e1[:], ALU.mult, ("gktm", "e1"), ("Khat",))
        tt(KtT[:].rearrange("p a b -> p (a b)"), gkT[:].rearrange("p a b -> p (a b)"), En[:], ALU.mult, ("gkT", "En"),
           ("KtT",))
        if not is_meta:
            stt(QtT[:].rearrange("p a b -> p (a b)"), gqT[:].rearrange("p a b -> p (a b)"), SCALE_GLA, Ep[:], ALU.mult,
                ALU.mult, ("gqT", "Ep"), ("QtT",))
            bk, bkey = bank_s()
            for h in range(4):
                mm(bk[:, h * 128:(h + 1) * 128], KtT[:, h, :], QtT[:, h, :], True, True, ("KtT", "QtT"), (bkey,))
            tt(At[:], bk[:, :], MASKUT, ALU.mult, (bkey, "cst"), ("At",))
            for h in range(4):
                acc = pf[4 + h // 2]
                akey = f"pf{4 + h // 2}"
                o = (h % 2) * 256
                mm(acc[:, o:o + 256], At[:, h * 128:(h + 1) * 128], gv[:, h * 256:(h + 1) * 256], True, False,
                   ("At", "gv"), (akey,))
                mm(acc[:, o:o + 256], QtT[:, h, :], Sb[:, h, :], False, True, ("QtT", "Sb"), (akey,))
            for h in range(4):
                acc = pf[4 + h // 2]
                akey = f"pf{4 + h // 2}"
                o = (h % 2) * 256
                act(junk[:, 0:256], acc[:, o:o + 256], AF.Square, (akey,), ("st1f",),
                    accum_out=st1[:, 40 + h:41 + h])
            rstd_from_ss(st1[:, 40:44], st1[:, 40:44], 256.0, ("st1f",), ("st1f",))
            tt(gsg[:].rearrange("p (h c) -> p h c", h=4), sg[:].rearrange("p (h c) -> p h c", h=4),
               G_GLA.unsqueeze(1).to_broadcast([128, 4, 256]), ALU.mult, ("sg", "gbt"), ("gsg",))
            for h in range(4):
                acc = pf[4 + h // 2]
                akey = f"pf{4 + h // 2}"
                o = (h % 2) * 256
                stt(yb[:, h * 256:(h + 1) * 256], acc[:, o:o + 256], st1[:, 40 + h:41 + h],
                    gsg[:, h * 256:(h + 1) * 256], ALU.mult, ALU.mult, (akey, "st1f", "gsg"), ("yb",))
        for h in range(4):
            bk, bkey = bank_s()
            mm(bk[:, 0:256], Khat[:, h * 128:(h + 1) * 128], gv[:, h * 256:(h + 1) * 256], True, True, ("Khat", "gv"),
               (bkey,))
            if is_meta:
                cp(S0[:, h, :], bk[:, 0:256], (bkey,), ("S0",))
            else:
                stt(S[:, h, :], S[:, h, :], Ep[:, h * 128 + 127:h * 128 + 128], bk[:, 0:256], ALU.mult, ALU.add,
                    ("S", "Ep", bkey), ("S",))
        if is_meta:
            return
        cp(Sb[:], S[:], ("S",), ("Sb",), eng="act")

        if stop == "E":
            dump(s, t, [(yb[:], "yb", 1024)])
            return
        transposes(yT[:].rearrange("p a b -> p (a b)"), "yT", ya, "ya", 8)
        for j in range(2):
            bk, bkey = proj_tm("oa", j * 512, (j + 1) * 512, yT, "yT")
            tt(mix[:, j * 512:(j + 1) * 512], bk[:, :], gas[:, j * 512:(j + 1) * 512], ALU.mult, (bkey, "gas"),
               ("mix",))
        transposes(yT[:].rearrange("p a b -> p (a b)"), "yT", yb, "yb", 8)
        for j in range(2):
            bk, bkey = proj_tm("ob", j * 512, (j + 1) * 512, yT, "yT")
            tt(gsg[:, j * 512:(j + 1) * 512], bk[:, :], gbs[:, j * 512:(j + 1) * 512], ALU.mult, (bkey, "gbs"),
               ("gsg",))
        tt(mixb[:], mix[:], gsg[:], ALU.add, ("mix", "gsg"), ("mixb",))
        transposes(yT[:].rearrange("p a b -> p (a b)"), "yT", mixb, "mixb", 8)
        for j in range(2):
            bk, bkey = proj_tm("out", j * 512, (j + 1) * 512, yT, "yT")
            tt(X[:, j * 512:(j + 1) * 512], bk[:, :], X[:, j * 512:(j + 1) * 512], ALU.add, (bkey, xk), (xk,))

        if stop == "F":
            dump(s, t, [(X[:], "xt0", 1024)])
            return
        act(junk[:], X[:], AF.Square, (xk,), ("st1g",), accum_out=st1[:, 48:49])
        rstd_from_ss(st1[:, 48:49], st1[:, 48:49], 1024.0, ("st1g",), ("st1g",))
        stt(n2b[:], X[:], st1[:, 48:49], G_FFN, ALU.mult, ALU.mult, (xk, "st1g", "gbt"), ("n2b",))
        transposes(yT[:].rearrange("p a b -> p (a b)"), "yT", n2b, "n2b", 8)
        for j in range(4):
            wsl, wk, _ = load_w("pq", j * 512, (j + 1) * 512)
            bk, bkey = bank_mm()
            for c in range(4):
                for kc in range(8):
                    mm(bk[:, c * 128:(c + 1) * 128], wsl[:, kc, c * 128:(c + 1) * 128], yT[:, kc, :], kc == 0, kc == 7,
                       ("yT", wk), (bkey,))
            cp(pqT[:].rearrange("p a b -> p (a b)"), bk[:, :], (bkey,), ("pqT",), eng="act")
            bk, bkey = bank_mm()
            for c in range(4):
                hp = 4 * j + c
                mm(bk[:, c * 128:(c + 1) * 128], pqT[:, c, :], keysT[:, (hp % 2) * 128:(hp % 2 + 1) * 128], True, True,
                   ("pqT", "keysT"), (bkey,))
            for c in range(4):
                hp = 4 * j + c
                sv = bk[:, c * 128:(c + 1) * 128]
                P.op("dve", (lambda hh, vv: (lambda e: e.max(out=tsv[:, hh, 0:8], in_=vv)))(hp, sv), (bkey,), ("tsv",))
                P.op("dve", (lambda hh, vv: (lambda e: e.max_index(out=tiu[:, hh, 0:8], in_max=tsv[:, hh, 0:8],
                                                                   in_values=vv)))(hp, sv), (bkey, "tsv"), ("tiu",))
                P.op("dve", (lambda hh, vv, cc: (lambda e: e.match_replace(
                    out=ss2[:, cc, :], in_to_replace=tsv[:, hh, 0:8], in_values=vv, imm_value=-1e30)))(hp, sv, c),
                    (bkey, "tsv"), ("ss2",))
                P.op("dve", (lambda hh, cc: (lambda e: e.max(out=tsv[:, hh, 8:16], in_=ss2[:, cc, :])))(hp, c),
                     ("ss2",), ("tsv",))
                P.op("dve", (lambda hh, cc: (lambda e: e.max_index(out=tiu[:, hh, 8:16], in_max=tsv[:, hh, 8:16],
                                                                   in_values=ss2[:, cc, :])))(hp, c), ("ss2", "tsv"),
                     ("tiu",))
        cp(tif[:], tiu[:], ("tiu",), ("tif",))
        ts4 = tsv[:].rearrange("p (h two) k -> p h two k", two=2)
        ti4 = tif[:].rearrange("p (h two) k -> p h two k", two=2)
        tsc(ti4[:, :, 0, :], ti4[:, :, 0, :], 128.0, None, ALU.mult, None, ("tif",), ("tif",))
        for h in range(8):
            cs = cands[:].rearrange("p (a b) -> p a b", a=16)
            ci = candi[:].rearrange("p (a b) -> p a b", a=16)
            tt(cs, ts4[:, h, 0, :].unsqueeze(2).to_broadcast([128, 16, 16]),
               ts4[:, h, 1, :].unsqueeze(1).to_broadcast([128, 16, 16]), ALU.add, ("tsv",), ("cands",))
            tt(ci, ti4[:, h, 0, :].unsqueeze(2).to_broadcast([128, 16, 16]),
               ti4[:, h, 1, :].unsqueeze(1).to_broadcast([128, 16, 16]), ALU.add, ("tif",), ("candi",))
            P.op("dve", (lambda hh: (lambda e: e.max(out=bsv[:, hh, 0:8], in_=cands[:])))(h), ("cands",), ("bsv",))
            P.op("dve", (lambda hh: (lambda e: e.match_replace(out=cands2[:], in_to_replace=bsv[:, hh, 0:8],
                                                               in_values=cands[:], imm_value=-1e30)))(h),
                 ("cands", "bsv"), ("cands2",))
            P.op("dve", (lambda hh: (lambda e: e.max(out=bsv[:, hh, 8:16], in_=cands2[:])))(h), ("cands2",), ("bsv",))
            for k in range(16):
                stt(junk2[:, 0:256], cands[:], bsv[:, h, k:k + 1], candi[:], ALU.is_equal, ALU.mult,
                    ("cands", "bsv", "candi"), (f"ix{h}_{k}",), accum_out=idxf[:, h * 16 + k:h * 16 + k + 1])
        tsc(idxf[:], idxf[:], 16383.0, 0.0, ALU.min, ALU.max,
            tuple(f"ix{h}_{k}" for h in range(8) for k in range(16)) + ("idxf",), ("idxf",))
        cp(idxi[:], idxf[:], ("idxf",), ("idxi",))
        b3 = bsv[:]
        g3 = gate[:].rearrange("p (h k) -> p h k", h=8)
        tt(g3, b3, b3[:, :, 0:1].to_broadcast([128, 8, 16]), ALU.subtract, ("bsv",), ("gate",))
        act(gate[:], gate[:], AF.Exp, ("gate",), ("gate",))
        red(st1[:, 50:58], g3, ALU.add, ("gate",), ("st1h",))
        recip(st1[:, 50:58], st1[:, 50:58], ("st1h",), ("st1h",))
        tt(g3, g3, st1[:, 50:58].unsqueeze(2).to_broadcast([128, 8, 16]), ALU.mult, ("gate", "st1h"), ("gate",))
        if stop == "G":
            dump(s, t, [(idxf[:], "idxf", 128), (gate[:], "gate", 128)])
            return
        for j in range(128):
            ui = rot("uv", NUV)
            P.dma("pool", (lambda uu, jj: (lambda e: e.indirect_dma_start(
                out=uvb[uu][:, :], out_offset=None, in_=uvs,
                in_offset=bass.IndirectOffsetOnAxis(ap=idxi[:, jj:jj + 1], axis=0))))(ui, j),
                ("idxi", "uvs"), (f"uvb{ui}",))
            stt(junk2[:], uvb[ui][:, 0:1024], 1.0, n2b[:], ALU.mult, ALU.mult, (f"uvb{ui}", "n2b"), (f"actv{j}",),
                accum_out=actv[:, j:j + 1])
            act(glv[:, j:j + 1], actv[:, j:j + 1], AF.Gelu, (f"actv{j}",), (f"glv{j}",))
            di = rot("dg", ND)
            tsc(dgb[di][:], identb[:], glv[:, j:j + 1], gate[:, j:j + 1], ALU.mult, ALU.mult,
                ("identb", f"glv{j}", "gate"), (f"dgb{di}",))
            mm(pf[4][:, :], dgb[di][:], uvb[ui][:, 1024:1536], j == 0, j == 127, (f"dgb{di}", f"uvb{ui}"), ("pf4",))
            mm(pf[5][:, :], dgb[di][:], uvb[ui][:, 1536:2048], j == 0, j == 127, (f"dgb{di}", f"uvb{ui}"), ("pf5",))
        tt(X[:, 0:512], pf[4][:, :], X[:, 0:512], ALU.add, ("pf4", xk), (xk,))
        tt(X[:, 512:1024], pf[5][:, :], X[:, 512:1024], ALU.add, ("pf5", xk), (xk,))
        dma_sp(y[s, (t - 1) * 128:t * 128, :], X[:], (xk,), ("y",), final=True)

    oneb = sb("oneb", [128, 1])
    P.op("dve", lambda e: e.memset(oneb[:], 1.0), (), ("oneb",))

    if stop not in ("M0", "P1", "P2"):
        tile(None, 0, 0)
    par = 1
    for s in range(nseq):
        cp(S[:], S0[:], ("S0",), ("S",))
        cp(Sb[:], S0[:], ("S0",), ("Sb",), eng="act")
        for t in range(1, ntile + 1):
            if stop in ("M0", "P1", "P2"):
                dump(s, t, [])
                continue
            tile(s, t, par)
            par ^= 1
    P.flush()
    stack.close()
    return nc


def _host_consts():
    idx = np.arange(128)
    ident = np.eye(128, dtype=np.float32)
    s_ = idx[:, None]
    t_ = idx[None, :]
    tri = np.where(s_ <= t_, -1.0 / 16.0, 0.0).astype(np.float32)
    sut = np.where(s_ > t_, -1.0 / 16.0, 0.0).astype(np.float32)
    trim = np.where((s_ <= t_) & (s_ < 16), -1.0 / 16.0, 0.0).astype(np.float32)
    sutm = np.where((s_ > t_) & (s_ < 16), -1.0 / 16.0, 0.0).astype(np.float32)
    mut = np.where(s_ <= t_, 1.0, 0.0).astype(np.float32)
    cst = np.concatenate([ident, tri, sut, trim, sutm, mut, mut, mut, mut], axis=1)
    inv_freq = (10000.0 ** (-np.arange(32, dtype=np.float32) / 32.0)).astype(np.float32)
    rope = np.zeros((NKT, 128, 64), np.float32)
    for t in range(NKT):
        pos = (np.arange(128) if t == 0 else 16 + 128 * (t - 1) + np.arange(128)).astype(np.float32)
        ang = pos[:, None] * inv_freq[None, :]
        rope[t, :, 0:32] = np.cos(ang)
        rope[t, :, 32:64] = np.sin(ang)
    return np.ascontiguousarray(cst), rope


_CACHE = {}


def _run(inputs, nseq, ntile, ncores, stop=None):
    f = lambda a: np.ascontiguousarray(np.asarray(a, dtype=np.float32))
    x = f(inputs["x"])
    cst, rope = _host_consts()
    xm = np.zeros((128, 1024), np.float32)
    xm[0:16] = f(inputs["meta"])
    gcol = np.concatenate([f(inputs["norm_mix"])[0].reshape(8, 128).T, f(inputs["mla_q_norm"])[0].reshape(3, 128).T,
                           f(inputs["mla_kv_norm"])[0].reshape(2, 128).T], axis=1)
    gvec = np.concatenate([f(inputs["qn_nope"])[0], f(inputs["qn_pe"])[0], f(inputs["kn_nope"])[0],
                           f(inputs["kn_pe"])[0], f(inputs["gla_norm"])[0], f(inputs["norm_ffn"])[0]])
    gb = np.ascontiguousarray(np.broadcast_to(gvec[None, :], (128, gvec.shape[0])))
    wa2 = np.concatenate([f(inputs["gla_w_a2"])[0], f(inputs["gla_b_a"]), np.zeros((15, 512), np.float32)], axis=0)
    keys = f(inputs["peer_keys"])[0]
    keysT = np.ascontiguousarray(keys.transpose(2, 0, 1).reshape(128, 256))
    uvt = np.ascontiguousarray(np.concatenate([f(inputs["peer_u"])[0], f(inputs["peer_v"])[0]], axis=1))
    common = {
        "xm": xm, "w_in": f(inputs["w_in"])[0], "w_uq": f(inputs["mla_w_uq"])[0], "w_ukv": f(inputs["mla_w_ukv"])[0],
        "w_oa": f(inputs["w_o_mla"])[0], "w_ob": f(inputs["w_o_gla"])[0], "w_out": f(inputs["w_out"])[0],
        "w_pq": f(inputs["peer_w_q"])[0], "gcol": np.ascontiguousarray(gcol), "gb": gb, "wa2": np.ascontiguousarray(wa2),
        "keysT": keysT, "uv": (uvt if stop in (None, "H") else uvt[:128]), "rope": rope, "cst": cst,
    }
    key = (nseq, ntile, stop)
    if key not in _CACHE:
        _CACHE[key] = build(nseq, ntile, stop)
    nc = _CACHE[key]
    in_maps = []
    for c in range(ncores):
        m = dict(common)
        m["x"] = np.ascontiguousarray(x[c * nseq:(c + 1) * nseq])
        in_maps.append(m)
    import os
    res = run_bass_kernel_spmd(nc, in_maps, core_ids=list(range(ncores)), trace=bool(os.environ.get("DBGTRACE")))
    if os.environ.get("DBGTRACE"):
        print("EXEC_NS", res.exec_time_ns)
    return np.concatenate([np.asarray(r["y"]) for r in res.results], axis=0)


def kernel(**inputs):
    out = _run(inputs, 2, 16, NCORES)
    return out.astype(np.float32)
```
